# Optimizing a Trainium2 kernel written in Bass

```python
import math
import jax, jax.numpy as jnp
from jax import lax
import numpy as np

D_MODEL = 2048
BATCH = 4
SEQ = 2048
DEPTH = 4

N_A_LAYERS = DEPTH // 2
N_B_LAYERS = DEPTH - N_A_LAYERS
HG_EXPAND = 128
HG_HEADS = D_MODEL // HG_EXPAND
HG_DK = HG_EXPAND
HG_DV = D_MODEL // HG_HEADS
HG_CHUNK = 64
ATT_HEAD_DIM = 64
ATT_Q_HEADS = D_MODEL // ATT_HEAD_DIM
ATT_KV_HEADS = ATT_Q_HEADS // 8
ATT_GROUP = ATT_Q_HEADS // ATT_KV_HEADS
WINDOW = 128
ATT_BLOCK = WINDOW
N_BUCKETS = 32
REL_MAX_DISTANCE = 128
N_GROUPS = 4
EXPERTS_PER_GROUP = 8
N_EXPERTS = N_GROUPS * EXPERTS_PER_GROUP
TOP_K = 2
D_EXPERT = D_MODEL // 4
MOE_BLOCK = 128
DEEPNORM_ALPHA = (2 * DEPTH) ** 0.25
DEEPNORM_BETA = (8 * DEPTH) ** -0.25
LN_EPS = 1e-5
RMS_EPS = 1e-6
NEG_BIG = -1e30
MIN_FORGET = 1e-30

kernel_name = "yoco_hgrn2_swa_sink_hmoe_deepnorm"

F32 = jnp.float32


def _layernorm(x, g, b):
    xf = x.astype(F32)
    mu = jnp.mean(xf, -1, keepdims=True)
    var = jnp.mean(jnp.square(xf - mu), -1, keepdims=True)
    y = (xf - mu) * lax.rsqrt(var + LN_EPS) * g.astype(F32) + b.astype(F32)
    return y.astype(x.dtype)


def _hgrn2(x, w_in, lower_bound, norm_g, w_out):
    bsz, seq, _ = x.shape
    n_chunks = seq // HG_CHUNK
    q, f, i, g = jnp.split(x @ w_in, 4, axis=-1)
    f = f.astype(F32)
    lb = lower_bound
    sig = jax.nn.sigmoid(f)
    forget = lb + (1.0 - lb) * sig
    log_forget = jnp.log(jnp.maximum(forget, MIN_FORGET))
    k = (1.0 - lb) * (1.0 - sig)

    def to_chunks(t):
        return t.astype(F32).reshape(bsz, n_chunks, HG_CHUNK, HG_HEADS, -1).transpose(1, 0, 3, 2, 4)

    qc, kc, vc, gc = to_chunks(q), to_chunks(k), to_chunks(i), to_chunks(log_forget)
    causal = jnp.tril(jnp.ones((HG_CHUNK, HG_CHUNK), bool))

    def chunk_step(state, inp):
        q_c, k_c, v_c, g_c = inp
        b = jnp.cumsum(g_c, axis=2)
        b_last = b[:, :, -1:, :]
        o_inter = jnp.einsum('bhtk,bhkv->bhtv', q_c * jnp.exp(b), state)
        rel = jnp.where(causal[None, None, :, :, None],
                        b[:, :, :, None, :] - b[:, :, None, :, :], NEG_BIG)
        scores = jnp.einsum('bhtk,bhsk,bhtsk->bhts', q_c, k_c, jnp.exp(rel))
        o_intra = jnp.einsum('bhts,bhsv->bhtv', scores, v_c)
        new_state = (jnp.exp(b_last[:, :, 0, :, None]) * state
                     + jnp.einsum('bhsk,bhsv->bhkv', k_c * jnp.exp(b_last - b), v_c))
        return new_state, o_inter + o_intra

    s0 = jnp.zeros((bsz, HG_HEADS, HG_DK, HG_DV), F32)
    _, o = lax.scan(chunk_step, s0, (qc, kc, vc, gc))
    o = o.transpose(1, 0, 3, 2, 4).reshape(bsz, seq, HG_HEADS, HG_DV)
    o = o * lax.rsqrt(jnp.mean(jnp.square(o), -1, keepdims=True) + RMS_EPS)
    o = o * norm_g.astype(F32).reshape(HG_HEADS, HG_DV)
    o = o.reshape(bsz, seq, D_MODEL) * jax.nn.silu(g.astype(F32))
    return o.astype(x.dtype) @ w_out


def _t5_bucket(dist):
    n = jnp.clip(dist, 0, REL_MAX_DISTANCE - 1)
    max_exact = N_BUCKETS // 2
    large = max_exact + (jnp.log(jnp.maximum(n, max_exact).astype(F32) / max_exact)
                         / math.log(REL_MAX_DISTANCE / max_exact)
                         * (N_BUCKETS - max_exact)).astype(jnp.int32)
    large = jnp.minimum(large, N_BUCKETS - 1)
    return jnp.where(n < max_exact, n, large)


def _band_bias_and_mask(rel_bias, seq):
    n_blocks = seq // ATT_BLOCK
    qi = jnp.arange(ATT_BLOCK)[:, None]
    kj = jnp.arange(2 * ATT_BLOCK)[None, :]
    dist = qi + ATT_BLOCK - kj
    in_window = (dist >= 0) & (dist < WINDOW)
    bias = rel_bias.astype(F32)[_t5_bucket(dist)]
    bias = bias.transpose(2, 0, 1).reshape(1, ATT_KV_HEADS, ATT_GROUP, 1, ATT_BLOCK, 2 * ATT_BLOCK)
    blk = jnp.arange(n_blocks)[:, None, None]
    mask = in_window[None] & ((blk > 0) | (kj[None] >= ATT_BLOCK))
    return bias, mask


def _shared_kv(x, w_kv):
    bsz, seq, _ = x.shape
    n_blocks = seq // ATT_BLOCK
    k, v = jnp.split(x @ w_kv, 2, axis=-1)

    def windows(t):
        t = t.reshape(bsz, seq, ATT_KV_HEADS, ATT_HEAD_DIM)
        t = jnp.pad(t, ((0, 0), (ATT_BLOCK, 0), (0, 0), (0, 0)))
        t = t.reshape(bsz, n_blocks + 1, ATT_BLOCK, ATT_KV_HEADS, ATT_HEAD_DIM)
        return jnp.concatenate([t[:, :-1], t[:, 1:]], axis=2)

    return windows(k), windows(v)


def _swa_sink(x, k_win, v_win, w_q, sinks, w_out, bias, mask):
    bsz, seq, _ = x.shape
    n_blocks = seq // ATT_BLOCK
    q = (x @ w_q).reshape(bsz, n_blocks, ATT_BLOCK, ATT_KV_HEADS, ATT_GROUP, ATT_HEAD_DIM)
    s = jnp.einsum('bnqhgd,bnkhd->bhgnqk', q, k_win).astype(F32) * (ATT_HEAD_DIM ** -0.5) + bias
    s = jnp.where(mask, s, NEG_BIG)
    sink = sinks.astype(F32).reshape(1, ATT_KV_HEADS, ATT_GROUP, 1, 1, 1)
    m = jnp.maximum(jnp.max(s, -1, keepdims=True), sink)
    p = jnp.exp(s - m)
    w = p / (jnp.sum(p, -1, keepdims=True) + jnp.exp(sink - m))
    o = jnp.einsum('bhgnqk,bnkhd->bnqhgd', w.astype(x.dtype), v_win)
    return o.reshape(bsz, seq, ATT_Q_HEADS * ATT_HEAD_DIM) @ w_out


def _hier_moe(x, w_rg, b_rg, w_re, b_re, w_gate, w_up, w_down):
    bsz, seq, d = x.shape
    n_tok = bsz * seq
    xf = x.reshape(n_tok, d)
    g_logits = (xf @ w_rg).astype(F32) + b_rg.astype(F32)
    g_prob = jax.nn.softmax(g_logits, -1)
    grp = jnp.argmax(g_logits, -1).astype(jnp.int32)
    p_grp = jnp.take_along_axis(g_prob, grp[:, None], -1)
    e_logits = ((xf @ w_re).astype(F32) + b_re.astype(F32)).reshape(n_tok, N_GROUPS, EXPERTS_PER_GROUP)
    e_in_grp = jnp.take_along_axis(e_logits, grp[:, None, None], 1)[:, 0]
    top_v, top_i = lax.top_k(e_in_grp, TOP_K)
    gates = p_grp * jax.nn.softmax(top_v, -1)
    expert = grp[:, None] * EXPERTS_PER_GROUP + top_i.astype(jnp.int32)

    n_asg = n_tok * TOP_K
    e_flat = expert.reshape(-1)
    g_flat = gates.reshape(-1)
    tok_flat = jnp.repeat(jnp.arange(n_tok, dtype=jnp.int32), TOP_K)
    order = jnp.argsort(e_flat)
    e_sorted = e_flat[order]
    counts = jnp.zeros((N_EXPERTS,), jnp.int32).at[e_flat].add(1)
    starts = jnp.cumsum(counts) - counts
    padded = (counts + MOE_BLOCK - 1) // MOE_BLOCK * MOE_BLOCK
    pad_ends = jnp.cumsum(padded)
    pad_starts = pad_ends - padded
    rank = jnp.arange(n_asg, dtype=jnp.int32) - starts[e_sorted]
    dest = pad_starts[e_sorted] + rank
    n_blocks = -(-n_asg // MOE_BLOCK) + N_EXPERTS
    n_slots = n_blocks * MOE_BLOCK
    slot_tok = jnp.full((n_slots,), n_tok, jnp.int32).at[dest].set(tok_flat[order])
    slot_gate = jnp.zeros((n_slots,), F32).at[dest].set(g_flat[order])
    blk_start = jnp.arange(n_blocks, dtype=jnp.int32) * MOE_BLOCK
    blk_expert = jnp.minimum(jnp.searchsorted(pad_ends, blk_start, side='right'), N_EXPERTS - 1)
    x_pad = jnp.concatenate([xf, jnp.zeros((1, d), xf.dtype)], 0)
    xs = x_pad[slot_tok].reshape(n_blocks, MOE_BLOCK, d)

    def expert_block(args):
        xb, e = args
        h = jax.nn.silu(xb @ w_gate[e]) * (xb @ w_up[e])
        return h @ w_down[e]

    ys = lax.map(expert_block, (xs, blk_expert)).reshape(n_slots, d)
    out = jnp.zeros((n_tok + 1, d), F32).at[slot_tok].add(ys.astype(F32) * slot_gate[:, None])[:n_tok]
    return out.reshape(bsz, seq, d).astype(x.dtype)


def setup_inputs(seed: int = 0) -> dict:
    key = jax.random.key(seed)
    ks = jax.random.split(key, 24)
    d = D_MODEL
    kvw = ATT_KV_HEADS * ATT_HEAD_DIM

    def nrm(k, shape, fan_in, scale=1.0):
        return jax.random.normal(k, shape, F32) * (scale * fan_in ** -0.5)

    x = jax.random.normal(ks[0], (BATCH, SEQ, d), F32)
    a_w_in = nrm(ks[1], (N_A_LAYERS, d, 4 * d), d)
    a_w_in = a_w_in.at[:, :, 2 * d:3 * d].multiply(DEEPNORM_BETA)
    a_lower_bound = jax.random.normal(ks[2], (N_A_LAYERS, d), F32)
    a_norm_g = 1.0 + 0.02 * jax.random.normal(ks[3], (N_A_LAYERS, d), F32)
    a_w_out = nrm(ks[4], (N_A_LAYERS, d, d), d, DEEPNORM_BETA)
    b_w_kv = nrm(ks[5], (d, 2 * kvw), d)
    b_w_kv = b_w_kv.at[:, kvw:].multiply(DEEPNORM_BETA)
    b_w_q = nrm(ks[6], (N_B_LAYERS, d, ATT_Q_HEADS * ATT_HEAD_DIM), d)
    b_sinks = jax.random.normal(ks[7], (N_B_LAYERS, ATT_Q_HEADS), F32)
    b_w_out = nrm(ks[8], (N_B_LAYERS, ATT_Q_HEADS * ATT_HEAD_DIM, d), d, DEEPNORM_BETA)
    rel_bias = 0.5 * jax.random.normal(ks[9], (N_BUCKETS, ATT_Q_HEADS), F32)
    moe_w_rg = nrm(ks[10], (DEPTH, d, N_GROUPS), d)
    moe_b_rg = 0.01 * jax.random.normal(ks[11], (DEPTH, N_GROUPS), F32)
    moe_w_re = nrm(ks[12], (DEPTH, d, N_EXPERTS), d)
    moe_b_re = 0.01 * jax.random.normal(ks[13], (DEPTH, N_EXPERTS), F32)
    moe_w_gate = nrm(ks[14], (DEPTH, N_EXPERTS, d, D_EXPERT), d)
    moe_w_up = nrm(ks[15], (DEPTH, N_EXPERTS, d, D_EXPERT), d)
    moe_w_down = nrm(ks[16], (DEPTH, N_EXPERTS, D_EXPERT, d), D_EXPERT, DEEPNORM_BETA)
    ln_g = 1.0 + 0.02 * jax.random.normal(ks[17], (2 * DEPTH, d), F32)
    ln_b = 0.02 * jax.random.normal(ks[18], (2 * DEPTH, d), F32)
    return {"x": x, "a_w_in": a_w_in, "a_lower_bound": a_lower_bound, "a_norm_g": a_norm_g,
            "a_w_out": a_w_out, "b_w_kv": b_w_kv, "b_w_q": b_w_q, "b_sinks": b_sinks,
            "b_w_out": b_w_out, "rel_bias": rel_bias, "moe_w_rg": moe_w_rg, "moe_b_rg": moe_b_rg,
            "moe_w_re": moe_w_re, "moe_b_re": moe_b_re, "moe_w_gate": moe_w_gate,
            "moe_w_up": moe_w_up, "moe_w_down": moe_w_down, "ln_g": ln_g, "ln_b": ln_b}


def reference(x, a_w_in, a_lower_bound, a_norm_g, a_w_out, b_w_kv, b_w_q, b_sinks, b_w_out,
              rel_bias, moe_w_rg, moe_b_rg, moe_w_re, moe_b_re, moe_w_gate, moe_w_up,
              moe_w_down, ln_g, ln_b):
    lb_sm = jax.nn.softmax(a_lower_bound.astype(F32), axis=0)
    lower_bounds = jnp.cumsum(lb_sm, axis=0) - lb_sm[0]
    att_bias, att_mask = _band_bias_and_mask(rel_bias, x.shape[1])
    k_win = None
    v_win = None
    for layer in range(DEPTH):
        if layer < N_A_LAYERS:
            h = _hgrn2(x, a_w_in[layer], lower_bounds[layer], a_norm_g[layer], a_w_out[layer])
        else:
            if layer == N_A_LAYERS:
                k_win, v_win = _shared_kv(x, b_w_kv)
            j = layer - N_A_LAYERS
            h = _swa_sink(x, k_win, v_win, b_w_q[j], b_sinks[j], b_w_out[j], att_bias, att_mask)
        x = _layernorm(DEEPNORM_ALPHA * x + h, ln_g[2 * layer], ln_b[2 * layer])
        f = _hier_moe(x, moe_w_rg[layer], moe_b_rg[layer], moe_w_re[layer], moe_b_re[layer],
                      moe_w_gate[layer], moe_w_up[layer], moe_w_down[layer])
        x = _layernorm(DEEPNORM_ALPHA * x + f, ln_g[2 * layer + 1], ln_b[2 * layer + 1])
    return x
```

```python
import math
import os
HGL = int(os.environ.get('HGL', '99'))
HGH = int(os.environ.get('HGH', '16'))
HGV = int(os.environ.get('HGV', '3'))
from contextlib import ExitStack

import numpy as np
import concourse.bass as bass
import concourse.mybir as mybir
from concourse.bass_utils import run_bass_kernel_spmd

F32 = mybir.dt.float32
BF16 = mybir.dt.bfloat16
ALU = mybir.AluOpType
AF = mybir.ActivationFunctionType
AX = mybir.AxisListType

D = 2048
NT = 8
SEG = NT * 128
NKC = 16
CAP = 128
NE = 32
ALPHA = 8 ** 0.25
LN_EPS = 1e-5
RMS_EPS = 1e-6
NEG = -1e30
SAME_ENGINE_SYNC = True


class T:
    __slots__ = ("name", "w", "r", "excl")

    def __init__(self, name, excl=False):
        self.name = name
        self.w = None
        self.r = {}
        self.excl = excl


class Sched:
    def __init__(self, nc, es):
        self.nc = nc
        self.eng = {"pe": nc.tensor, "act": nc.scalar, "dve": nc.vector, "pool": nc.gpsimd, "sp": nc.sync}
        self.sem = {}
        self.cnt = {}
        for k in ["pe", "act", "dve", "pool", "sp", "pool_d", "sp_d"]:
            self.sem[k] = es.enter_context(nc.semaphore("sem_" + k))
            self.cnt[k] = 0
        self.waited = {k: {} for k in self.eng}
        self.n_ins = 0

    def _deps(self, e, r, w):
        deps = {}
        for t in r:
            if t.w is not None:
                k, v = t.w
                deps[k] = max(deps.get(k, 0), v)
        for t in w:
            if t.w is not None:
                k, v = t.w
                deps[k] = max(deps.get(k, 0), v)
            for k, v in t.r.items():
                deps[k] = max(deps.get(k, 0), v)
        for k, v in deps.items():
            if k == e and (e == "pe" or not SAME_ENGINE_SYNC):
                continue
            if self.waited[e].get(k, 0) < v:
                self.eng[e].wait_ge(self.sem[k], v)
                self.waited[e][k] = v

    def op(self, e, fn, r=(), w=()):
        xr = tuple(t for t in r if t.excl)
        if xr:
            r = tuple(t for t in r if not t.excl)
            w = tuple(w) + xr
        self._deps(e, r, w)
        ins = fn(self.eng[e])
        ins.then_inc(self.sem[e], 1)
        self.cnt[e] += 1
        self.n_ins += 1
        v = self.cnt[e]
        for t in r:
            t.r[e] = v
        for t in w:
            t.w = (e, v)
            t.r = {}

    def dma(self, q, out, in_, r=(), w=()):
        self._deps(q, r, w)
        dk = q + "_d"
        self.eng[q].dma_start(out=out, in_=in_).then_inc(self.sem[dk], 16)
        self.cnt[dk] += 16
        self.n_ins += 1
        v = self.cnt[dk]
        for t in r:
            t.r[dk] = v
        for t in w:
            t.w = (dk, v)
            t.r = {}

    def wait_all(self, e, tiles):
        self._deps(e, tiles, tiles)


def _t5_bucket_np(dist):
    n = np.clip(dist, 0, 127)
    max_exact = 16
    large = max_exact + (np.log(np.maximum(n, max_exact).astype(np.float32) / np.float32(max_exact))
                         / np.float32(math.log(128 / max_exact)) * np.float32(32 - max_exact)).astype(np.int32)
    large = np.minimum(large, 31)
    return np.where(n < max_exact, n, large)


def host_consts():
    c = {}
    t = np.arange(128)
    ch = t // 64
    same = ch[:, None] == ch[None, :]
    mid = ch * 64 + 31
    L1 = same * ((t[:, None] <= t[None, :]).astype(np.float32) - (t[:, None] <= mid[None, :]).astype(np.float32))
    L2 = same * (t[:, None] > t[None, :]).astype(np.float32)
    sel = np.zeros((128, 4), np.float32)
    sel[:, 0] = t <= 31
    sel[:, 1] = t < 64
    sel[:, 2] = (t >= 64) & (t <= 95)
    sel[:, 3] = t >= 64
    maskT = (same & (t[:, None] <= t[None, :])).astype(np.float32)
    ustrict = (t[:, None] < t[None, :]).astype(np.float32)
    c["c_ident"] = np.eye(128, dtype=np.float32)
    c["c_L1"] = L1.astype(np.float32)
    c["c_L2"] = L2.astype(np.float32)
    c["c_sel"] = sel
    c["c_maskT"] = maskT
    c["c_ustrict"] = ustrict
    c["c_ones"] = np.ones((128, 128), np.float32)
    c["c_iota"] = np.tile(np.arange(CAP, dtype=np.float32)[None, :], (128, 1))
    qi = np.arange(128)[:, None]
    kj = np.arange(256)[None, :]
    dist = qi + 128 - kj
    inwin = (dist >= 0) & (dist < 128)
    c["c_maskc"] = np.where(inwin, 0.0, NEG).astype(np.float32)
    c["_bucket"] = _t5_bucket_np(dist)
    return c


class Prog:
    def __init__(self, debug_stop=None):
        self.debug_stop = debug_stop
        self.nc = bass.Bass("TRN2", target_bir_lowering=False)
        self.es = ExitStack()
        self.in_shapes = {}
        self.in_aps = {}

    def dram_in(self, name, shape):
        self.in_shapes[name] = list(shape)
        return None

    def din(self, name):
        if name not in self.in_aps:
            self.in_aps[name] = self.nc.dram_tensor(name, self.in_shapes[name], F32, kind="ExternalInput").ap()
        return self.in_aps[name]

    def sb(self, name, shape, dt=F32):
        return self.es.enter_context(self.nc.sbuf_tensor("s_" + name, list(shape), dt))

    def build(self):
        nc = self.nc
        es = self.es
        with es:
            self.S = Sched(nc, es)
            self._declare()
            self._program()
        return nc

    def _declare(self):
        nc = self.nc
        di = self.dram_in
        di("xp", [SEG, D]); di("xm", [SEG, D])
        di("a_w_in", [2, D, 4 * D]); di("a_lower_bound", [2, D]); di("a_norm_g", [2, D]); di("a_w_out", [2, D, D])
        di("b_w_kv", [D, 512]); di("b_w_q", [2, D, D]); di("b_sinks", [2, 32]); di("b_w_out", [2, D, D])
        di("bias_tab", [32, 128, 256])
        di("moe_w_rg", [4, D, 4]); di("moe_b_rg", [4, 4]); di("moe_w_re", [4, D, 32]); di("moe_b_re", [4, 32])
        di("moe_w_gate", [4, NE, D, 512]); di("moe_w_up", [4, NE, D, 512]); di("moe_w_down", [4, NE, 512, D])
        di("ln_g", [8, D]); di("ln_b", [8, D]); di("flag", [128, 1]); di("halo_mask", [128, 128])
        for k, shp in [("c_ident", [128, 128]), ("c_L1", [128, 128]), ("c_L2", [128, 128]), ("c_sel", [128, 4]),
                       ("c_maskT", [128, 128]), ("c_ustrict", [128, 128]), ("c_ones", [128, 128]),
                       ("c_iota", [128, CAP]), ("c_maskc", [128, 256])]:
            di(k, shp)
        self.y = nc.dram_tensor("y", [SEG, D], F32, kind="ExternalOutput").ap()
        self.d_state = [nc.dram_tensor(f"d_state{l}", [128, 16 * 128], F32, kind="Internal").ap() for l in range(2)]
        self.d_halo = nc.dram_tensor("d_halo", [128, D], F32, kind="Internal").ap()

        sb = self.sb
        self.X = sb("X", [128, NT, D])
        self.XTraw = sb("XTraw", [128, 8192])
        self.XT = self.XTraw[:].bitcast(BF16).rearrange("p (k t) -> p k t", k=NKC)
        self.OT = sb("OT", [128, NKC, SEG], BF16)
        self.tX = [T(f"X{i}") for i in range(NT)]
        self.tXT = [T(f"XT{i}") for i in range(NT)]
        self.tOT = [T(f"OT{i}") for i in range(NT)]
        self.NB = 3
        self.WB = [sb(f"WB{i}", [128, NKC, 512], BF16) for i in range(self.NB)]
        self.tWB = [T(f"WB{i}") for i in range(self.NB)]
        self.wb_next = 0
        self.c_ident = sb("c_ident", [128, 128])
        self.c_identb = sb("c_identb", [128, 128], BF16)
        self.c_maskc = sb("c_maskc", [128, 256])
        self.c_flag = sb("c_flag", [128, 1])
        self.c_halo = sb("c_halo", [128, 128])
        self.tC = T("consts")
        self.lnst = sb("lnst", [128, 8])
        self.tlnst = T("lnst")
        self.ARN = 7300
        self.AR = sb("arena", [128, self.ARN])
        self.ar_off = 0
        self.PSS = self.es.enter_context(nc.psum_tensor("pss", [128, 1024], F32))
        self.PSG = [self.es.enter_context(nc.psum_tensor(f"psg{i}", [128, 512], F32)) for i in range(2)]
        self.PSBv = [self.PSS[:, 0:512], self.PSS[:, 512:1024], self.PSG[0][:], self.PSG[1][:]]
        self.PSF = [self.es.enter_context(nc.psum_tensor(f"psf{i}", [128, 512], F32)) for i in range(2)]
        self.PST = [self.es.enter_context(nc.psum_tensor(f"pst{i}", [128, 1024], BF16)) for i in range(2)]
        self.tPSB = [T(f"psb{i}", True) for i in range(4)]
        self.tPSF = [T(f"psf{i}", True) for i in range(2)]
        self.tPST = [T(f"pst{i}", True) for i in range(2)]

    def barrier(self):
        S = self.S
        for e in S.eng:
            for k in S.sem:
                if k == e:
                    continue
                if S.cnt[k] > S.waited[e].get(k, 0):
                    S.eng[e].wait_ge(S.sem[k], S.cnt[k])
                    S.waited[e][k] = S.cnt[k]

    def phase(self):
        self.barrier()
        self.ar_off = 0

    def arf(self, cols, shape=None):
        o = self.ar_off
        self.ar_off += cols
        assert self.ar_off <= self.ARN, f"arena overflow {self.ar_off}"
        v = self.AR[:, o:o + cols]
        return v

    def arb(self, cols):
        assert cols % 2 == 0
        return self.arf(cols // 2).bitcast(BF16)

    def wload(self, pieces):
        i = self.wb_next
        self.wb_next = (i + 1) % self.NB
        buf, t = self.WB[i], self.tWB[i]
        for dst_fn, src in pieces:
            self.S.dma("pool", dst_fn(buf), src, r=(), w=(t,))
        return buf, t

    def load_consts(self):
        S = self.S
        for k in ["c_ident", "c_maskc"]:
            S.dma("sp", getattr(self, k)[:], self.din(k)[:, :], w=(self.tC,))
        S.dma("sp", self.c_flag[:], self.din("flag")[:, :], w=(self.tC,))
        S.dma("sp", self.c_halo[:], self.din("halo_mask")[:, :], w=(self.tC,))
        S.op("dve", lambda e: e.tensor_copy(out=self.c_identb[:], in_=self.c_ident[:]), r=(self.tC,), w=(self.tC,))

    def load_x(self, src):
        for i in range(NT):
            self.S.dma("sp", self.X[:, i, :], src[i * 128:(i + 1) * 128, :], w=(self.tX[i],))

    def make_xt(self):
        S = self.S
        n = 0
        for i in range(NT):
            for g in range(4):
                pb = n % 2
                n += 1
                ps, tps = self.PSF[pb], self.tPSF[pb]
                for j in range(4):
                    kc = g * 4 + j
                    S.op("pe", lambda e, kc=kc, j=j, ps=ps, i=i: e.transpose(
                        ps[:, j * 128:(j + 1) * 128], self.X[:, i, kc * 128:(kc + 1) * 128], self.c_ident[:]),
                        r=(self.tX[i], self.tC), w=(tps,))
                dst = self.XT[:, g * 4:(g + 1) * 4, i * 128:(i + 1) * 128]
                src = ps[:].rearrange("p (j t) -> p j t", j=4)
                if n % 2:
                    S.op("act", lambda e, dst=dst, src=src: e.copy(out=dst, in_=src), r=(tps,), w=(self.tXT[i],))
                else:
                    S.op("dve", lambda e, dst=dst, src=src: e.tensor_copy(out=dst, in_=src), r=(tps,), w=(self.tXT[i],))

    def rsqrt_small(self, out, in_, scale, eps, rt, wt):
        S = self.S
        S.op("act", lambda e: e.activation(out=out, in_=in_, func=AF.Ln, bias=self.epsb(eps), scale=scale), r=rt, w=wt)
        S.op("act", lambda e: e.activation(out=out, in_=out, func=AF.Exp, scale=-0.5), r=wt, w=wt)

    def epsb(self, eps):
        return self.c_eps[eps][:]

    def layernorm(self, idx):
        S = self.S
        self.barrier()
        vg = self.XTraw[:, 0:D]
        vb = self.XTraw[:, D:2 * D]
        tv = T("lnvec")
        S.dma("sp", vg, self.din("ln_g")[idx:idx + 1, :].partition_broadcast(128), w=(tv,))
        S.dma("sp", vb, self.din("ln_b")[idx:idx + 1, :].partition_broadcast(128), w=(tv,))
        st, tst = self.lnst, self.tlnst
        junk = self.OT[:, 0:2, :]
        tj = T("lnjunk")
        for i in range(NT):
            xi = self.X[:, i, :]
            xi3 = xi.rearrange("p (a b) -> p a b", a=2)
            S.op("act", lambda e, xi3=xi3: e.activation(out=junk, in_=xi3, func=AF.Copy, accum_out=st[:, 0:1]),
                 r=(self.tX[i],), w=(tst, tj))
            S.op("act", lambda e, xi3=xi3: e.activation(out=junk, in_=xi3, func=AF.Square, accum_out=st[:, 1:2]),
                 r=(self.tX[i],), w=(tst, tj))
            S.op("dve", lambda e: e.tensor_scalar(out=st[:, 2:4], in0=st[:, 0:2], scalar1=1.0 / D, scalar2=None, op0=ALU.mult),
                 r=(tst,), w=(tst,))
            S.op("dve", lambda e: e.tensor_tensor(out=st[:, 4:5], in0=st[:, 2:3], in1=st[:, 2:3], op=ALU.mult), r=(tst,), w=(tst,))
            S.op("dve", lambda e: e.tensor_tensor(out=st[:, 5:6], in0=st[:, 3:4], in1=st[:, 4:5], op=ALU.subtract), r=(tst,), w=(tst,))
            self.rsqrt_small(st[:, 6:7], st[:, 5:6], 1.0, LN_EPS, (tst,), (tst,))
            S.op("dve", lambda e: e.scalar_tensor_tensor(out=st[:, 7:8], in0=st[:, 2:3], scalar=-1.0, in1=st[:, 6:7], op0=ALU.mult, op1=ALU.mult),
                 r=(tst,), w=(tst,))
            S.op("act", lambda e, xi=xi: e.activation(out=xi, in_=xi, func=AF.Identity, bias=st[:, 7:8], scale=st[:, 6:7]),
                 r=(tst, self.tX[i]), w=(self.tX[i],))
            S.op("dve", lambda e, xi=xi: e.tensor_tensor(out=xi, in0=xi, in1=vg, op=ALU.mult), r=(tv, self.tX[i]), w=(self.tX[i],))
            S.op("dve", lambda e, xi=xi: e.tensor_tensor(out=xi, in0=xi, in1=vb, op=ALU.add), r=(tv, self.tX[i]), w=(self.tX[i],))
        self.barrier()

    def out_proj(self, w_dram):
        S = self.S
        n = 0
        for nch in range(4):
            buf, tb = self.wload([(lambda b: b[:], w_dram[:, nch * 512:(nch + 1) * 512].rearrange("(kc p) n -> p kc n", p=128))])
            for i in range(NT):
                pb = n % 2
                n += 1
                ps, tps = self.PSF[pb], self.tPSF[pb]
                for kc in range(NKC):
                    S.op("pe", lambda e, kc=kc, ps=ps, i=i, buf=buf: e.matmul(
                        ps[:], lhsT=self.OT[:, kc, i * 128:(i + 1) * 128], rhs=buf[:, kc, :], start=(kc == 0), stop=(kc == NKC - 1)),
                        r=(self.tOT[i], tb), w=(tps,))
                xs = self.X[:, i, nch * 512:(nch + 1) * 512]
                S.op("dve", lambda e, xs=xs, ps=ps: e.scalar_tensor_tensor(out=xs, in0=xs, scalar=ALPHA, in1=ps[:], op0=ALU.mult, op1=ALU.add),
                     r=(tps, self.tX[i]), w=(self.tX[i],))

    def hgrn(self, l, init_mode):
        S = self.S
        self.phase()
        f = self.arf
        St = f(2048).rearrange("p (h v) -> p h v", h=16)
        tSt = [T(f"St{h}") for h in range(16)]
        cL1, cL2, cMT, csel = f(128), f(128), f(128), f(4)
        tc = T("hc")
        for dst, k in [(cL1, "c_L1"), (cL2, "c_L2"), (cMT, "c_maskT"), (csel, "c_sel")]:
            S.dma("sp", dst, self.din(k)[:, :], w=(tc,))
        if init_mode == "zero":
            S.op("dve", lambda e: e.memset(St, 0.0), w=tuple(tSt))
        else:
            S.dma("sp", St, self.d_state[l].rearrange("p (h v) -> p h v", h=16), w=tuple(tSt))
            S.op("dve", lambda e: e.tensor_scalar(out=St, in0=St, scalar1=self.c_flag[:, 0:1], scalar2=None, op0=ALU.mult),
                 r=tuple(tSt) + (self.tC,), w=tuple(tSt))
        a0, a1, lb, oml, ng = f(128), f(128), f(128), f(128), f(128)
        tv = T("hvec")
        e1, t1, fg, kk, logf = f(128), f(128), f(128), f(128), f(128)
        eq, ek, ekl, ebT = f(128), f(128), f(128), f(4)
        sg, gg = f(128), f(128)
        ss = f(4)
        qt, kt, kh, vv = self.arb(128), self.arb(128), self.arb(128), self.arb(128)
        QF, KTs, sTm, Sp0, Sp1, on = self.arb(128), self.arb(128), self.arb(128), self.arb(128), self.arb(128), self.arb(128)
        QZ = self.arb(256).rearrange("p (a b) -> p a b", a=2)
        junk = self.arb(128)
        tw = {k: T("hw_" + k) for k in ["e1", "t1", "fg", "kk", "logf", "eq", "ek", "ekl", "ebT", "sg", "gg", "ss", "qt", "kt", "kh", "vv",
                                        "QF", "KTs", "sTm", "Sp0", "Sp1", "on", "QZ", "junk"]}
        S.op("dve", lambda e: e.memset(QZ, 0.0), w=(tw["QZ"],))
        w_in = self.din("a_w_in")
        alb = self.din("a_lower_bound")
        ang = self.din("a_norm_g")
        for h in range(HGH):
            hs = slice(h * 128, (h + 1) * 128)
            buf, tb = self.wload([(lambda b, p=p: b[:, :, p * 128:(p + 1) * 128],
                                   w_in[l, :, p * D + h * 128:p * D + (h + 1) * 128].rearrange("(kc p) n -> p kc n", p=128))
                                  for p in range(4)])
            S.dma("sp", ng, ang[l:l + 1, hs].partition_broadcast(128), w=(tv,))
            if l == 0:
                S.op("dve", lambda e: e.memset(lb, 0.0), w=(tv,))
            else:
                S.dma("sp", a0, alb[0:1, hs].partition_broadcast(128), w=(tv,))
                S.dma("sp", a1, alb[1:2, hs].partition_broadcast(128), w=(tv,))
                S.op("dve", lambda e: e.tensor_tensor(out=lb, in0=a0, in1=a1, op=ALU.subtract), r=(tv,), w=(tv,))
                S.op("act", lambda e: e.activation(out=lb, in_=lb, func=AF.Exp), r=(tv,), w=(tv,))
                S.op("dve", lambda e: e.tensor_scalar(out=lb, in0=lb, scalar1=1.0, scalar2=None, op0=ALU.add), r=(tv,), w=(tv,))
                S.op("dve", lambda e: e.reciprocal(out=lb, in_=lb), r=(tv,), w=(tv,))
            S.op("dve", lambda e: e.tensor_scalar(out=oml, in0=lb, scalar1=-1.0, scalar2=1.0, op0=ALU.mult, op1=ALU.add), r=(tv,), w=(tv,))
            for i in range(NT):
                ts = slice(i * 128, (i + 1) * 128)
                pp, tpp = self.PSF[i % 2], self.tPSF[i % 2]
                for kc in range(NKC):
                    S.op("pe", lambda e, kc=kc, pp=pp, ts=ts, buf=buf: e.matmul(pp[:], lhsT=self.XT[:, kc, ts], rhs=buf[:, kc, :],
                                                                                 start=(kc == 0), stop=(kc == NKC - 1)),
                         r=(self.tXT[i], tb), w=(tpp,))
                if HGL < 1:
                    continue
                pq, pf, pi_, pg = pp[:, 0:128], pp[:, 128:256], pp[:, 256:384], pp[:, 384:512]
                S.op("act", lambda e: e.activation(out=e1, in_=pf, func=AF.Exp, scale=-1.0), r=(tpp,), w=(tw["e1"],))
                S.op("dve", lambda e: e.tensor_scalar(out=e1, in0=e1, scalar1=1.0, scalar2=None, op0=ALU.add), r=(tw["e1"],), w=(tw["e1"],))
                S.op("dve", lambda e: e.reciprocal(out=e1, in_=e1), r=(tw["e1"],), w=(tw["e1"],))
                S.op("dve", lambda e: e.tensor_tensor(out=t1, in0=e1, in1=oml, op=ALU.mult), r=(tw["e1"], tv), w=(tw["t1"],))
                S.op("dve", lambda e: e.tensor_tensor(out=fg, in0=t1, in1=lb, op=ALU.add), r=(tw["t1"], tv), w=(tw["fg"],))
                S.op("dve", lambda e: e.tensor_scalar(out=fg, in0=fg, scalar1=1e-30, scalar2=None, op0=ALU.max), r=(tw["fg"],), w=(tw["fg"],))
                S.op("act", lambda e: e.activation(out=logf, in_=fg, func=AF.Ln), r=(tw["fg"],), w=(tw["logf"],))
                S.op("dve", lambda e: e.tensor_tensor(out=kk, in0=oml, in1=t1, op=ALU.subtract), r=(tw["t1"], tv), w=(tw["kk"],))
                if HGL < 2:
                    continue
                pc, tpc = self.PSBv[0], self.tPSB[0]
                S.op("pe", lambda e: e.matmul(pc[:, 0:128], lhsT=cL1, rhs=logf, start=True, stop=True), r=(tc, tw["logf"]), w=(tpc,))
                if HGV >= 2:
                    S.op("pe", lambda e: e.matmul(pc[:, 128:256], lhsT=cL2, rhs=logf, start=True, stop=True), r=(tc, tw["logf"]), w=(tpc,))
                if HGV >= 3:
                    S.op("pe", lambda e: e.matmul(pc[:, 256:260], lhsT=logf, rhs=csel, start=True, stop=True), r=(tc, tw["logf"]), w=(tpc,))
                if HGL < 3 or os.environ.get('HGX') == '1':
                    continue
                S.op("act", lambda e: e.activation(out=eq, in_=pc[:, 0:128], func=AF.Exp), r=(tpc,), w=(tw["eq"],))
                S.op("act", lambda e: e.activation(out=ek, in_=pc[:, 0:128], func=AF.Exp, scale=-1.0), r=(tpc,), w=(tw["ek"],))
                S.op("act", lambda e: e.activation(out=ekl, in_=pc[:, 128:256], func=AF.Exp), r=(tpc,), w=(tw["ekl"],))
                S.op("act", lambda e: e.activation(out=ebT, in_=pc[:, 256:260], func=AF.Exp), r=(tpc,), w=(tw["ebT"],))
                if os.environ.get('HGX') == '2':
                    continue
                S.op("dve", lambda e: e.tensor_tensor(out=qt, in0=pq, in1=eq, op=ALU.mult), r=(tpp, tw["eq"]), w=(tw["qt"],))
                S.op("dve", lambda e: e.tensor_tensor(out=kt, in0=kk, in1=ek, op=ALU.mult), r=(tw["kk"], tw["ek"]), w=(tw["kt"],))
                S.op("dve", lambda e: e.tensor_tensor(out=kh, in0=kk, in1=ekl, op=ALU.mult), r=(tw["kk"], tw["ekl"]), w=(tw["kh"],))
                if os.environ.get('HGX') == '3':
                    continue
                S.op("dve", lambda e: e.tensor_copy(out=vv, in_=pi_), r=(tpp,), w=(tw["vv"],))
                if os.environ.get('HGX') == '4':
                    continue
                S.op("act", lambda e: e.activation(out=sg, in_=pg, func=AF.Exp, scale=-1.0), r=(tpp,), w=(tw["sg"],))
                S.op("dve", lambda e: e.tensor_scalar(out=sg, in0=sg, scalar1=1.0, scalar2=None, op0=ALU.add), r=(tw["sg"],), w=(tw["sg"],))
                S.op("dve", lambda e: e.reciprocal(out=sg, in_=sg), r=(tw["sg"],), w=(tw["sg"],))
                S.op("dve", lambda e: e.tensor_tensor(out=gg, in0=pg, in1=sg, op=ALU.mult), r=(tpp, tw["sg"]), w=(tw["gg"],))
                S.op("dve", lambda e: e.tensor_tensor(out=gg, in0=gg, in1=ng, op=ALU.mult), r=(tw["gg"], tv), w=(tw["gg"],))
                if HGL < 4:
                    continue
                pt, tpt = self.PST[0], self.tPST[0]
                S.op("pe", lambda e: e.transpose(pt[:, 0:128], qt, self.c_identb[:]), r=(tw["qt"], self.tC), w=(tpt,))
                S.op("pe", lambda e: e.transpose(pt[:, 128:256], kt, self.c_identb[:]), r=(tw["kt"], self.tC), w=(tpt,))
                S.op("dve", lambda e: e.tensor_copy(out=QF, in_=pt[:, 0:128]), r=(tpt,), w=(tw["QF"],))
                S.op("dve", lambda e: e.tensor_copy(out=QZ[:, 0, 0:64], in_=pt[:, 0:64]), r=(tpt,), w=(tw["QZ"],))
                S.op("dve", lambda e: e.tensor_copy(out=QZ[:, 1, 64:128], in_=pt[:, 64:128]), r=(tpt,), w=(tw["QZ"],))
                S.op("dve", lambda e: e.tensor_copy(out=KTs, in_=pt[:, 128:256]), r=(tpt,), w=(tw["KTs"],))
                if HGL < 5:
                    continue
                psc, tpsc = self.PSBv[1], self.tPSB[1]
                S.op("pe", lambda e: e.matmul(psc[:, 0:128], lhsT=KTs, rhs=QF, start=True, stop=True), r=(tw["KTs"], tw["QF"]), w=(tpsc,))
                S.op("dve", lambda e: e.tensor_tensor(out=sTm, in0=psc[:, 0:128], in1=cMT, op=ALU.mult), r=(tpsc, tc), w=(tw["sTm"],))
                if HGL < 6:
                    continue
                pds, tpds = self.PSBv[2], self.tPSB[2]
                Sh = St[:, h, :]
                S.op("dve", lambda e: e.tensor_scalar(out=Sp0, in0=Sh, scalar1=ebT[:, 0:1], scalar2=None, op0=ALU.mult),
                     r=(tSt[h], tw["ebT"]), w=(tw["Sp0"],))
                S.op("pe", lambda e: e.matmul(pds[:, 0:128], lhsT=kh[0:64, :], rhs=vv[0:64, :], start=True, stop=True),
                     r=(tw["kh"], tw["vv"]), w=(tpds,))
                S.op("dve", lambda e: e.scalar_tensor_tensor(out=Sh, in0=Sh, scalar=ebT[:, 1:2], in1=pds[:, 0:128], op0=ALU.mult, op1=ALU.add),
                     r=(tSt[h], tw["ebT"], tpds), w=(tSt[h],))
                S.op("dve", lambda e: e.tensor_scalar(out=Sp1, in0=Sh, scalar1=ebT[:, 2:3], scalar2=None, op0=ALU.mult),
                     r=(tSt[h], tw["ebT"]), w=(tw["Sp1"],))
                S.op("pe", lambda e: e.matmul(pds[:, 128:256], lhsT=kh[64:128, :], rhs=vv[64:128, :], start=True, stop=True),
                     r=(tw["kh"], tw["vv"]), w=(tpds,))
                S.op("dve", lambda e: e.scalar_tensor_tensor(out=Sh, in0=Sh, scalar=ebT[:, 3:4], in1=pds[:, 128:256], op0=ALU.mult, op1=ALU.add),
                     r=(tSt[h], tw["ebT"], tpds), w=(tSt[h],))
                if HGL < 7:
                    continue
                po, tpo = self.PSBv[3], self.tPSB[3]
                S.op("pe", lambda e: e.matmul(po[:, 0:128], lhsT=sTm, rhs=vv, start=True, stop=False), r=(tw["sTm"], tw["vv"]), w=(tpo,))
                S.op("pe", lambda e: e.matmul(po[:, 0:128], lhsT=QZ[:, 0, :], rhs=Sp0, start=False, stop=False), r=(tw["QZ"], tw["Sp0"]), w=(tpo,))
                S.op("pe", lambda e: e.matmul(po[:, 0:128], lhsT=QZ[:, 1, :], rhs=Sp1, start=False, stop=True), r=(tw["QZ"], tw["Sp1"]), w=(tpo,))
                if HGL < 8:
                    continue
                S.op("act", lambda e: e.activation(out=junk, in_=po[:, 0:128], func=AF.Square, accum_out=ss[:, 0:1]), r=(tpo,), w=(tw["ss"], tw["junk"]))
                self.rsqrt_small(ss[:, 1:2], ss[:, 0:1], 1.0 / 128, RMS_EPS, (tw["ss"],), (tw["ss"],))
                S.op("dve", lambda e: e.scalar_tensor_tensor(out=on, in0=po[:, 0:128], scalar=ss[:, 1:2], in1=gg, op0=ALU.mult, op1=ALU.mult),
                     r=(tpo, tw["ss"], tw["gg"]), w=(tw["on"],))
                if HGL < 9:
                    continue
                pt1, tpt1 = self.PST[1], self.tPST[1]
                S.op("pe", lambda e: e.transpose(pt1[:, 0:128], on, self.c_identb[:]), r=(tw["on"], self.tC), w=(tpt1,))
                S.op("dve", lambda e, h=h, ts=ts: e.tensor_copy(out=self.OT[:, h, ts], in_=pt1[:, 0:128]), r=(tpt1,), w=(self.tOT[i],))
        S.dma("sp", self.d_state[l].rearrange("p (h v) -> p h v", h=16), St, r=tuple(tSt))

    def moe(self, L):
        S = self.S
        self.barrier()
        X1b = self.OT[:].rearrange("p k t -> p (k t)").rearrange("p (i d) -> p i d", i=NT)
        tX1b = [T(f"x1b{i}") for i in range(NT)]
        off = [0]

        def f(cols):
            o = off[0]
            off[0] += cols
            assert off[0] <= 8192, off[0]
            return self.XTraw[:, o:o + cols]

        def fb(cols):
            return f(cols // 2).bitcast(BF16)

        wr = f(NKC * 36).rearrange("p (k n) -> p k n", k=NKC)
        rb = f(36)
        ustrict, ones, iota = f(128), f(128), f(CAP)
        xT4 = [f(512), f(512)]
        A = f(NT * 32).rearrange("p (i e) -> p i e", i=NT)
        Gt = f(NT * 32).rearrange("p (i e) -> p i e", i=NT)
        VAL = f(NT * 32).rearrange("p (i e) -> p i e", i=NT)
        lg, gsel, pen, elm, elm2, oh1, oh2 = f(36), f(4), f(4), f(32), f(32), f(32), f(32)
        sm = f(16)
        junk4 = f(4)
        P = fb(NT * CAP).rearrange("p (i c) -> p i c", i=NT)
        Pg = fb(NT * CAP).rearrange("p (i c) -> p i c", i=NT)
        PgT = fb(NT * 128).rearrange("p (i t) -> p i t", i=NT)
        XS = fb(NKC * CAP).rearrange("p (k c) -> p k c", k=NKC)
        HT = fb(4 * CAP).rearrange("p (k c) -> p k c", k=4)
        hsg = f(4 * CAP)
        Y = fb(D)
        tc_ = T("mc")
        tr = T("mrt")
        tA = [T(f"mA{i}") for i in range(NT)]
        txT4 = [T("xT4a"), T("xT4b")]
        tP, tPg, tPgT, tXS, tHT, thsg, tY = T("P"), T("Pg"), T("PgT"), T("XS"), T("HT"), T("hsg"), T("Y")

        S.dma("sp", wr[:, :, 0:4], self.din("moe_w_rg")[L].rearrange("(k p) n -> p k n", p=128), w=(tc_,))
        S.dma("sp", wr[:, :, 4:36], self.din("moe_w_re")[L].rearrange("(k p) n -> p k n", p=128), w=(tc_,))
        S.dma("sp", rb[:, 0:4], self.din("moe_b_rg")[L:L + 1, :].partition_broadcast(128), w=(tc_,))
        S.dma("sp", rb[:, 4:36], self.din("moe_b_re")[L:L + 1, :].partition_broadcast(128), w=(tc_,))
        S.dma("sp", ustrict, self.din("c_ustrict")[:, :], w=(tc_,))
        S.dma("sp", ones, self.din("c_ones")[:, :], w=(tc_,))
        S.dma("sp", iota, self.din("c_iota")[:, :], w=(tc_,))

        for i in range(NT):
            if i % 2:
                S.op("act", lambda e, i=i: e.copy(out=X1b[:, i, :], in_=self.X[:, i, :]), r=(self.tX[i],), w=(tX1b[i],))
            else:
                S.op("dve", lambda e, i=i: e.tensor_copy(out=X1b[:, i, :], in_=self.X[:, i, :]), r=(self.tX[i],), w=(tX1b[i],))

        n = 0
        plg, tplg = self.PSBv[0], self.tPSB[0]
        for i in range(NT):
            for g in range(4):
                pb = n % 2
                n += 1
                ps, tps = self.PSF[pb], self.tPSF[pb]
                for j in range(4):
                    kc = g * 4 + j
                    S.op("pe", lambda e, kc=kc, j=j, ps=ps, i=i: e.transpose(
                        ps[:, j * 128:(j + 1) * 128], self.X[:, i, kc * 128:(kc + 1) * 128], self.c_ident[:]),
                        r=(self.tX[i], self.tC), w=(tps,))
                xt, txt = xT4[pb], txT4[pb]
                if pb:
                    S.op("act", lambda e, xt=xt, ps=ps: e.copy(out=xt, in_=ps[:]), r=(tps,), w=(txt,))
                else:
                    S.op("dve", lambda e, xt=xt, ps=ps: e.tensor_copy(out=xt, in_=ps[:]), r=(tps,), w=(txt,))
                for j in range(4):
                    kc = g * 4 + j
                    S.op("pe", lambda e, kc=kc, j=j, xt=xt: e.matmul(plg[:, 0:36], lhsT=xt[:, j * 128:(j + 1) * 128], rhs=wr[:, kc, :],
                                                                      start=(kc == 0), stop=(kc == NKC - 1)),
                         r=(txt, tc_), w=(tplg,))
            dv = lambda fn, r, w: S.op("dve", fn, r=r, w=w)
            dv(lambda e: e.tensor_tensor(out=lg, in0=plg[:, 0:36], in1=rb, op=ALU.add), (tplg, tc_), (tr,))
            gl, el = lg[:, 0:4], lg[:, 4:36]
            gmax, ngmax, sume, pgrp, m1, m2, dd, g1, g2 = (sm[:, k:k + 1] for k in range(9))
            dv(lambda e: e.tensor_reduce(out=gmax, in_=gl, axis=AX.X, op=ALU.max), (tr,), (tr,))
            dv(lambda e: e.tensor_scalar(out=gsel, in0=gl, scalar1=gmax, scalar2=None, op0=ALU.is_equal), (tr,), (tr,))
            dv(lambda e: e.tensor_scalar(out=ngmax, in0=gmax, scalar1=-1.0, scalar2=None, op0=ALU.mult), (tr,), (tr,))
            S.op("act", lambda e: e.activation(out=junk4, in_=gl, func=AF.Exp, bias=ngmax, scale=1.0, accum_out=sume), r=(tr,), w=(tr,))
            dv(lambda e: e.reciprocal(out=pgrp, in_=sume), (tr,), (tr,))
            dv(lambda e: e.tensor_scalar(out=pen, in0=gsel, scalar1=-1.0, scalar2=1e30, op0=ALU.add, op1=ALU.mult), (tr,), (tr,))
            for g in range(4):
                dv(lambda e, g=g: e.tensor_scalar(out=elm[:, g * 8:(g + 1) * 8], in0=el[:, g * 8:(g + 1) * 8], scalar1=pen[:, g:g + 1],
                                                  scalar2=None, op0=ALU.add), (tr,), (tr,))
            dv(lambda e: e.tensor_reduce(out=m1, in_=elm, axis=AX.X, op=ALU.max), (tr,), (tr,))
            dv(lambda e: e.tensor_scalar(out=oh1, in0=elm, scalar1=m1, scalar2=None, op0=ALU.is_equal), (tr,), (tr,))
            dv(lambda e: e.scalar_tensor_tensor(out=elm2, in0=oh1, scalar=-1e30, in1=elm, op0=ALU.mult, op1=ALU.add), (tr,), (tr,))
            dv(lambda e: e.tensor_reduce(out=m2, in_=elm2, axis=AX.X, op=ALU.max), (tr,), (tr,))
            dv(lambda e: e.tensor_scalar(out=oh2, in0=elm2, scalar1=m2, scalar2=None, op0=ALU.is_equal), (tr,), (tr,))
            dv(lambda e: e.tensor_tensor(out=dd, in0=m2, in1=m1, op=ALU.subtract), (tr,), (tr,))
            S.op("act", lambda e: e.activation(out=dd, in_=dd, func=AF.Exp), r=(tr,), w=(tr,))
            dv(lambda e: e.tensor_scalar(out=g1, in0=dd, scalar1=1.0, scalar2=None, op0=ALU.add), (tr,), (tr,))
            dv(lambda e: e.reciprocal(out=g1, in_=g1), (tr,), (tr,))
            dv(lambda e: e.tensor_tensor(out=g2, in0=dd, in1=g1, op=ALU.mult), (tr,), (tr,))
            dv(lambda e: e.tensor_tensor(out=g1, in0=g1, in1=pgrp, op=ALU.mult), (tr,), (tr,))
            dv(lambda e: e.tensor_tensor(out=g2, in0=g2, in1=pgrp, op=ALU.mult), (tr,), (tr,))
            dv(lambda e, i=i: e.tensor_tensor(out=A[:, i, :], in0=oh1, in1=oh2, op=ALU.add), (tr,), (tA[i],))
            dv(lambda e, i=i: e.tensor_scalar(out=Gt[:, i, :], in0=oh1, scalar1=g1, scalar2=None, op0=ALU.mult), (tr,), (tA[i],))
            dv(lambda e, i=i: e.scalar_tensor_tensor(out=Gt[:, i, :], in0=oh2, scalar=g2, in1=Gt[:, i, :], op0=ALU.mult, op1=ALU.add),
               (tr, tA[i]), (tA[i],))
        pr, tpr = self.PSBv[1], self.tPSB[1]
        for i in range(NT):
            for j in range(i):
                S.op("pe", lambda e, j=j: e.matmul(pr[:, 0:32], lhsT=ones, rhs=A[:, j, :], start=(j == 0), stop=False), r=(tc_, tA[j]), w=(tpr,))
            S.op("pe", lambda e, i=i: e.matmul(pr[:, 0:32], lhsT=ustrict, rhs=A[:, i, :], start=(i == 0), stop=True), r=(tc_, tA[i]), w=(tpr,))
            S.op("dve", lambda e, i=i: e.scalar_tensor_tensor(out=VAL[:, i, :], in0=pr[:, 0:32], scalar=1.0, in1=A[:, i, :], op0=ALU.add, op1=ALU.mult),
                 r=(tpr, tA[i]), w=(tA[i],))
            S.op("dve", lambda e, i=i: e.tensor_scalar(out=VAL[:, i, :], in0=VAL[:, i, :], scalar1=-1.0, scalar2=None, op0=ALU.add),
                 r=(tA[i],), w=(tA[i],))
        for i in range(NT):
            if i % 2:
                S.op("act", lambda e, i=i: e.mul(out=self.X[:, i, :], in_=self.X[:, i, :], mul=ALPHA), r=(self.tX[i],), w=(self.tX[i],))
            else:
                S.op("dve", lambda e, i=i: e.tensor_scalar(out=self.X[:, i, :], in0=self.X[:, i, :], scalar1=ALPHA, scalar2=None, op0=ALU.mult),
                     r=(self.tX[i],), w=(self.tX[i],))
        wg_d, wu_d, wd_d = self.din("moe_w_gate"), self.din("moe_w_up"), self.din("moe_w_down")
        ncp = 0
        for ex in range(NE):
            wg, twg = self.wload([(lambda b: b[:], wg_d[L, ex].rearrange("(kc p) n -> p kc n", p=128))])
            wu, twu = self.wload([(lambda b: b[:], wu_d[L, ex].rearrange("(kc p) n -> p kc n", p=128))])
            wdv = lambda b: b[:].rearrange("p k n -> p (k n)").rearrange("p (fc d) -> p fc d", fc=4)
            wd_, twd = self.wload([(wdv, wd_d[L, ex].rearrange("(fc p) d -> p fc d", p=128))])
            wd = wdv(wd_)
            for i in range(NT):
                S.op("dve", lambda e, i=i, ex=ex: e.tensor_scalar(out=P[:, i, :], in0=iota, scalar1=VAL[:, i, ex:ex + 1], scalar2=None, op0=ALU.is_equal),
                     r=(tc_, tA[i]), w=(tP,))
                S.op("dve", lambda e, i=i, ex=ex: e.tensor_scalar(out=Pg[:, i, :], in0=iota, scalar1=VAL[:, i, ex:ex + 1], scalar2=Gt[:, i, ex:ex + 1],
                                                                  op0=ALU.is_equal, op1=ALU.mult), r=(tc_, tA[i]), w=(tPg,))
            pt, tpt = self.PST[0], self.tPST[0]
            for i in range(NT):
                S.op("pe", lambda e, i=i: e.transpose(pt[:, i * 128:(i + 1) * 128], Pg[:, i, :], self.c_identb[:]), r=(tPg, self.tC), w=(tpt,))
            S.op("act", lambda e: e.copy(out=PgT, in_=pt[:].rearrange("p (i t) -> p i t", i=NT)), r=(tpt,), w=(tPgT,))
            for g in range(4):
                ps, tps = self.PSF[g % 2], self.tPSF[g % 2]
                for j in range(4):
                    kc = g * 4 + j
                    for i in range(NT):
                        S.op("pe", lambda e, kc=kc, j=j, i=i, ps=ps: e.matmul(ps[:, j * CAP:(j + 1) * CAP], lhsT=X1b[:, i, kc * 128:(kc + 1) * 128],
                                                                               rhs=P[:, i, :], start=(i == 0), stop=(i == NT - 1)),
                             r=(tX1b[i], tP), w=(tps,))
                dst = XS[:, g * 4:(g + 1) * 4, :]
                src = ps[:, 0:4 * CAP].rearrange("p (j c) -> p j c", j=4)
                if g % 2:
                    S.op("act", lambda e, dst=dst, src=src: e.copy(out=dst, in_=src), r=(tps,), w=(tXS,))
                else:
                    S.op("dve", lambda e, dst=dst, src=src: e.tensor_copy(out=dst, in_=src), r=(tps,), w=(tXS,))
            phg, tphg = self.PSBv[0], self.tPSB[0]
            phu, tphu = self.PSBv[1], self.tPSB[1]
            for (pw, tpw, wt, twt) in ((phg, tphg, wg, twg), (phu, tphu, wu, twu)):
                for fc in range(4):
                    for kc in range(NKC):
                        S.op("pe", lambda e, fc=fc, kc=kc, pw=pw, wt=wt: e.matmul(pw[:, fc * CAP:(fc + 1) * CAP], lhsT=wt[:, kc, fc * 128:(fc + 1) * 128],
                                                                                   rhs=XS[:, kc, :], start=(kc == 0), stop=(kc == NKC - 1)),
                             r=(twt, tXS), w=(tpw,))
            S.op("act", lambda e: e.activation(out=hsg, in_=phg[:, 0:4 * CAP], func=AF.Exp, scale=-1.0), r=(tphg,), w=(thsg,))
            S.op("dve", lambda e: e.tensor_scalar(out=hsg, in0=hsg, scalar1=1.0, scalar2=None, op0=ALU.add), r=(thsg,), w=(thsg,))
            S.op("dve", lambda e: e.reciprocal(out=hsg, in_=hsg), r=(thsg,), w=(thsg,))
            S.op("dve", lambda e: e.tensor_tensor(out=hsg, in0=hsg, in1=phg[:, 0:4 * CAP], op=ALU.mult), r=(thsg, tphg), w=(thsg,))
            S.op("dve", lambda e: e.tensor_tensor(out=HT[:].rearrange("p k c -> p (k c)"), in0=hsg, in1=phu[:, 0:4 * CAP], op=ALU.mult),
                 r=(thsg, tphu), w=(tHT,))
            for nn in range(4):
                py, tpy = self.PSBv[2 + nn % 2], self.tPSB[2 + nn % 2]
                for fc in range(4):
                    S.op("pe", lambda e, fc=fc, nn=nn, py=py: e.matmul(py[0:CAP, :], lhsT=HT[:, fc, :], rhs=wd[:, fc, nn * 512:(nn + 1) * 512],
                                                                        start=(fc == 0), stop=(fc == 3)), r=(tHT, twd), w=(tpy,))
                if nn % 2:
                    S.op("act", lambda e, nn=nn, py=py: e.copy(out=Y[0:CAP, nn * 512:(nn + 1) * 512], in_=py[0:CAP, :]), r=(tpy,), w=(tY,))
                else:
                    S.op("dve", lambda e, nn=nn, py=py: e.tensor_copy(out=Y[0:CAP, nn * 512:(nn + 1) * 512], in_=py[0:CAP, :]), r=(tpy,), w=(tY,))
            for i in range(NT):
                for nn in range(4):
                    pcb, tpcb = self.PSF[ncp % 2], self.tPSF[ncp % 2]
                    ncp += 1
                    S.op("pe", lambda e, i=i, nn=nn, pcb=pcb: e.matmul(pcb[:], lhsT=PgT[0:CAP, i, :], rhs=Y[0:CAP, nn * 512:(nn + 1) * 512], start=True, stop=True),
                         r=(tPgT, tY), w=(tpcb,))
                    xs = self.X[:, i, nn * 512:(nn + 1) * 512]
                    S.op("dve", lambda e, xs=xs, pcb=pcb: e.tensor_tensor(out=xs, in0=xs, in1=pcb[:], op=ALU.add), r=(tpcb, self.tX[i]), w=(self.tX[i],))
        self.barrier()

    def kv_alloc(self):
        self.KT2 = self.arb(4 * 1152).rearrange("p (h t) -> p h t", h=4)
        self.V = self.arb(9 * 256).rearrange("p (b c) -> p b c", b=9)
        self.tKV = T("KV")
        self.att_base = self.ar_off

    def save_halo(self):
        self.S.dma("sp", self.d_halo[:, :], self.X[:, NT - 1, :], r=(self.tX[NT - 1],))

    def kv_compute(self):
        S = self.S
        self.ar_off = self.att_base
        Xh = self.arf(D)
        XTh = self.arb(NKC * 128).rearrange("p (k t) -> p k t", k=NKC)
        tXh, tXTh = T("Xh"), T("XTh")
        S.dma("sp", Xh, self.d_halo[:, :], w=(tXh,))
        for g in range(4):
            ps, tps = self.PSF[g % 2], self.tPSF[g % 2]
            for j in range(4):
                kc = g * 4 + j
                S.op("pe", lambda e, kc=kc, j=j, ps=ps: e.transpose(ps[:, j * 128:(j + 1) * 128], Xh[:, kc * 128:(kc + 1) * 128], self.c_ident[:]),
                     r=(tXh, self.tC), w=(tps,))
            S.op("dve", lambda e, g=g, ps=ps: e.tensor_copy(out=XTh[:, g * 4:(g + 1) * 4, :], in_=ps[:].rearrange("p (j t) -> p j t", j=4)),
                 r=(tps,), w=(tXTh,))
        wkv = self.din("b_w_kv")
        bufv, tbv = self.wload([(lambda b: b[:], wkv.rearrange("(kc p) n -> p kc n", p=128))])
        wkd_v = lambda b: b[:].rearrange("p k (h f) -> p k h f", h=4)
        pieces = []
        for hk in range(4):
            for dup in range(2):
                pieces.append((lambda b, hk=hk, dup=dup: wkd_v(b)[:, :, hk, dup * 64:(dup + 1) * 64],
                               wkv[:, hk * 64:(hk + 1) * 64].rearrange("(kc p) n -> p kc n", p=128)))
        bufk_, tbk = self.wload(pieces)
        bufk = wkd_v(bufk_)
        n = 0
        for b in range(9):
            ps, tps = self.PSF[n % 2], self.tPSF[n % 2]
            n += 1
            for kc in range(NKC):
                lhsT = XTh[:, kc, :] if b == 0 else self.XT[:, kc, (b - 1) * 128:b * 128]
                rt = (tXTh,) if b == 0 else (self.tXT[b - 1],)
                S.op("pe", lambda e, kc=kc, ps=ps, lhsT=lhsT: e.matmul(ps[:, 0:256], lhsT=lhsT, rhs=bufv[:, kc, 256:512], start=(kc == 0), stop=(kc == NKC - 1)),
                     r=rt + (tbv,), w=(tps,))
            S.op("dve", lambda e, b=b, ps=ps: e.tensor_copy(out=self.V[:, b, :], in_=ps[:, 0:256]), r=(tps,), w=(self.tKV,))
        for hk in range(4):
            for part in range(3):
                ps, tps = self.PSF[n % 2], self.tPSF[n % 2]
                n += 1
                if part == 0:
                    rhs_fn, ncol, rt, dst = (lambda kc: XTh[:, kc, :]), 128, (tXTh,), self.KT2[:, hk, 0:128]
                else:
                    lo = (part - 1) * 512
                    rhs_fn, ncol, rt = (lambda kc, lo=lo: self.XT[:, kc, lo:lo + 512]), 512, tuple(self.tXT[(part - 1) * 4:(part - 1) * 4 + 4])
                    dst = self.KT2[:, hk, 128 + lo:128 + lo + 512]
                for kc in range(NKC):
                    S.op("pe", lambda e, kc=kc, ps=ps, hk=hk, rhs_fn=rhs_fn, ncol=ncol: e.matmul(ps[:, 0:ncol], lhsT=bufk[:, kc, hk, :], rhs=rhs_fn(kc),
                                                                                                 start=(kc == 0), stop=(kc == NKC - 1)),
                         r=rt + (tbk,), w=(tps,))
                S.op("act", lambda e, ps=ps, ncol=ncol, dst=dst: e.copy(out=dst, in_=ps[:, 0:ncol]), r=(tps,), w=(self.tKV,))
        self.barrier()

    def attention(self, j_layer):
        S = self.S
        self.barrier()
        self.ar_off = self.att_base
        f = self.arf
        biasm = f(1024).rearrange("p (h k) -> p h k", h=4)
        PB = self.arb(1024).rearrange("p (h k) -> p h k", h=4)
        PTs = self.arb(1024).rearrange("p (a q) -> p a q", a=8)
        QTg = self.arb(2048).rearrange("p (c t) -> p c t", c=2)
        OB = self.arb(256)
        SS2 = f(512).rearrange("p (h k) -> p h k", h=2)
        tSS = T("SS2")
        sm = f(32)
        sink, rm, negm, rs, es, rinv = (sm[:, 4 * k:4 * k + 4] for k in range(6))
        tb, tPB, tPTs, tQ, tOB, tsm = T("biasm"), T("PB"), T("PTs"), T("QTg"), T("OB"), T("asm")
        wq = self.din("b_w_q")
        btab = self.din("bias_tab")
        sinks = self.din("b_sinks")
        PSS3 = self.PSS[:].rearrange("p (h k) -> p h k", h=4)
        tS = (self.tPSB[0], self.tPSB[1])
        n = 0
        for hk in range(4):
            bufq, tbq = self.wload([(lambda b: b[:], wq[j_layer, :, hk * 512:(hk + 1) * 512].rearrange("(kc p) n -> p kc n", p=128))])
            for hh in range(2):
                h0 = 8 * hk + 4 * hh
                c0 = h0 // 2
                for cc in range(2):
                    for half in range(2):
                        ps, tps = self.PSF[n % 2], self.tPSF[n % 2]
                        n += 1
                        for kc in range(NKC):
                            S.op("pe", lambda e, kc=kc, ps=ps, cc=cc, half=half, hh=hh, bufq=bufq: e.matmul(
                                ps[:], lhsT=bufq[:, kc, hh * 256 + cc * 128:hh * 256 + (cc + 1) * 128], rhs=self.XT[:, kc, half * 512:(half + 1) * 512],
                                start=(kc == 0), stop=(kc == NKC - 1)), r=tuple(self.tXT[half * 4:half * 4 + 4]) + (tbq,), w=(tps,))
                        S.op("act", lambda e, ps=ps, cc=cc, half=half: e.copy(out=QTg[:, cc, half * 512:(half + 1) * 512], in_=ps[:]), r=(tps,), w=(tQ,))
                pos = lambda j: (j % 2) * 2 + j // 2
                for j in range(4):
                    S.dma("sp", biasm[:, pos(j), :], btab[h0 + j], w=(tb,))
                    S.dma("sp", sink[:, pos(j):pos(j) + 1], sinks[j_layer:j_layer + 1, h0 + j:h0 + j + 1].partition_broadcast(128), w=(tsm,))
                for j in range(4):
                    S.op("dve", lambda e, j=j: e.tensor_tensor(out=biasm[:, j, :], in0=biasm[:, j, :], in1=self.c_maskc[:], op=ALU.add), r=(tb, self.tC), w=(tb,))
                for nb in range(NT):
                    if os.environ.get("ATT_STOP") == "q":
                        continue
                    qs = slice(nb * 128, (nb + 1) * 128)
                    for j in range(4):
                        p, cc = j % 2, j // 2
                        S.op("pe", lambda e, j=j, p=p, cc=cc, qs=qs, nb=nb, hk=hk: e.matmul(
                            self.PSS[:, pos(j) * 256:(pos(j) + 1) * 256], lhsT=QTg[p * 64:(p + 1) * 64, cc, qs],
                            rhs=self.KT2[p * 64:(p + 1) * 64, hk, nb * 128:nb * 128 + 256], start=True, stop=True),
                            r=(tQ, self.tKV), w=(self.tPSB[j % 2],))
                    for bk in range(2):
                        j0 = 2 * bk
                        pbank = self.PSS[:, bk * 512:(bk + 1) * 512].rearrange("p (h k) -> p h k", h=2)
                        S.op("dve", lambda e, pbank=pbank, j0=j0: e.scalar_tensor_tensor(out=SS2, in0=pbank, scalar=0.125, in1=biasm[:, j0:j0 + 2, :],
                                                                                         op0=ALU.mult, op1=ALU.add), r=(tb, self.tPSB[bk]), w=(tSS,))
                        if nb == 0:
                            for jj in range(2):
                                S.op("dve", lambda e, jj=jj: e.tensor_tensor(out=SS2[:, jj, 0:128], in0=SS2[:, jj, 0:128], in1=self.c_halo[:], op=ALU.add),
                                     r=(self.tC, tSS), w=(tSS,))
                        S.op("dve", lambda e, j0=j0: e.tensor_reduce(out=rm[:, j0:j0 + 2], in_=SS2, axis=AX.X, op=ALU.max), r=(tSS,), w=(tsm,))
                        S.op("dve", lambda e, j0=j0: e.tensor_tensor(out=rm[:, j0:j0 + 2], in0=rm[:, j0:j0 + 2], in1=sink[:, j0:j0 + 2], op=ALU.max), r=(tsm,), w=(tsm,))
                        S.op("dve", lambda e, j0=j0: e.tensor_scalar(out=negm[:, j0:j0 + 2], in0=rm[:, j0:j0 + 2], scalar1=-1.0, scalar2=None, op0=ALU.mult), r=(tsm,), w=(tsm,))
                        for jj in range(2):
                            j = j0 + jj
                            S.op("act", lambda e, j=j, jj=jj: e.activation(out=PB[:, j, :], in_=SS2[:, jj, :], func=AF.Exp,
                                                                            bias=negm[:, j:j + 1], scale=1.0, accum_out=rs[:, j:j + 1]),
                                 r=(tSS, tsm), w=(tPB, tsm))
                    S.op("dve", lambda e: e.tensor_tensor(out=es, in0=sink, in1=negm, op=ALU.add), r=(tsm,), w=(tsm,))
                    S.op("act", lambda e: e.activation(out=es, in_=es, func=AF.Exp), r=(tsm,), w=(tsm,))
                    S.op("dve", lambda e: e.tensor_tensor(out=rinv, in0=rs, in1=es, op=ALU.add), r=(tsm,), w=(tsm,))
                    S.op("dve", lambda e: e.reciprocal(out=rinv, in_=rinv), r=(tsm,), w=(tsm,))
                    pt, tpt = self.PST[0], self.tPST[0]
                    for j in range(4):
                        for kb in range(2):
                            a = j * 2 + kb
                            S.op("pe", lambda e, j=j, kb=kb, a=a: e.transpose(pt[:, a * 128:(a + 1) * 128], PB[:, j, kb * 128:(kb + 1) * 128], self.c_identb[:]),
                                 r=(tPB, self.tC), w=(tpt,))
                    S.op("act", lambda e: e.copy(out=PTs, in_=pt[:].rearrange("p (a q) -> p a q", a=8)), r=(tpt,), w=(tPTs,))
                    po, tpo = self.PSBv[2], self.tPSB[2]
                    for j in range(4):
                        for kb in range(2):
                            S.op("pe", lambda e, j=j, kb=kb, nb=nb, hk=hk: e.matmul(po[:, j * 64:(j + 1) * 64], lhsT=PTs[:, pos(j) * 2 + kb, :],
                                                                                    rhs=self.V[:, nb + kb, hk * 64:(hk + 1) * 64],
                                                                                    start=(kb == 0), stop=(kb == 1)),
                                 r=(tPTs, self.tKV), w=(tpo,))
                    for j in range(4):
                        S.op("dve", lambda e, j=j: e.tensor_scalar(out=OB[:, j * 64:(j + 1) * 64], in0=po[:, j * 64:(j + 1) * 64], scalar1=rinv[:, pos(j):pos(j) + 1],
                                                                   scalar2=None, op0=ALU.mult), r=(tpo, tsm), w=(tOB,))
                    pt1, tpt1 = self.PST[1], self.tPST[1]
                    for cc in range(2):
                        S.op("pe", lambda e, cc=cc: e.transpose(pt1[:, cc * 128:(cc + 1) * 128], OB[:, cc * 128:(cc + 1) * 128], self.c_identb[:]),
                             r=(tOB, self.tC), w=(tpt1,))
                    S.op("dve", lambda e, c0=c0, qs=qs: e.tensor_copy(out=self.OT[:, c0:c0 + 2, qs], in_=pt1[:, 0:256].rearrange("p (c q) -> p c q", c=2)),
                         r=(tpt1,), w=(self.tOT[nb],))
        self.barrier()

    def _program(self):
        S = self.S
        self.c_eps = {}
        for eps in (LN_EPS, RMS_EPS):
            t = self.sb(f"eps{len(self.c_eps)}", [128, 1])
            self.c_eps[eps] = t
            S.op("dve", lambda e, t=t, eps=eps: e.memset(t[:], eps), w=(self.tC,))
        self.load_consts()
        stop = self.debug_stop
        if stop is not None and stop != "hg1":
            self.load_x(self.din("xm"))
        if stop in ("xt", "xt0"):
            self.make_xt()
        if stop == "xt":
            self.layernorm(0)
            self.store_out()
            return
        if stop == "hg1":
            self.load_x(self.din("xp"))
            self.make_xt()
            self.hgrn(1, "zero")
            self.barrier()
            self.load_x(self.din("xm"))
            self.make_xt()
            self.hgrn(1, "load")
            self.barrier()
            self.out_proj(self.din("a_w_out")[1])
            self.layernorm(2)
            self.store_out()
            return
        if stop == "att":
            self.phase()
            self.kv_alloc()
            self.load_x(self.din("xp"))
            self.save_halo()
            self.barrier()
            self.load_x(self.din("xm"))
            self.make_xt()
            self.kv_compute()
            if os.environ.get("ATT_STOP") == "kv":
                self.store_out()
                return
            self.attention(0)
            if os.environ.get("ATT_STOP"):
                self.store_out()
                return
            self.out_proj(self.din("b_w_out")[0])
            self.layernorm(4)
            self.store_out()
            return
        if stop == "moe":
            self.moe(0)
            self.layernorm(1)
            self.store_out()
            return
        if stop == "xt0":
            self.barrier()
            self.store_out()
            return
        def hgrn_layer(l, mode):
            self.make_xt()
            self.hgrn(l, mode)
            self.barrier()
            self.out_proj(self.din("a_w_out")[l])
            self.layernorm(2 * l)
            self.moe(l)
            self.layernorm(2 * l + 1)

        self.load_x(self.din("xp"))
        hgrn_layer(0, "zero")
        hgrn_layer(1, "zero")
        self.save_halo()
        self.barrier()
        self.load_x(self.din("xm"))
        hgrn_layer(0, "load")
        hgrn_layer(1, "load")
        self.make_xt()
        self.phase()
        self.kv_alloc()
        self.kv_compute()
        for j in range(2):
            if j:
                self.make_xt()
            self.attention(j)
            self.out_proj(self.din("b_w_out")[j])
            self.layernorm(4 + 2 * j)
            self.moe(2 + j)
            self.layernorm(5 + 2 * j)
        self.store_out()

    def store_out(self):
        S = self.S
        for i in range(NT):
            S.dma("sp", self.y[i * 128:(i + 1) * 128, :], self.X[:, i, :], r=(self.tX[i],))
        S.eng["sp"].wait_ge(S.sem["sp_d"], S.cnt["sp_d"])


_CACHE = {}


def _prep_inputs(inputs):
    c = host_consts()
    bucket = c.pop("_bucket")
    rel_bias = np.asarray(inputs["rel_bias"], np.float32)
    bias_tab = np.ascontiguousarray(rel_bias[bucket].transpose(2, 0, 1))
    shared = {k: np.ascontiguousarray(np.asarray(inputs[k], np.float32)) for k in
              ["a_w_in", "a_lower_bound", "a_norm_g", "a_w_out", "b_w_kv", "b_w_q", "b_sinks", "b_w_out",
               "moe_w_rg", "moe_b_rg", "moe_w_re", "moe_b_re", "moe_w_gate", "moe_w_up", "moe_w_down", "ln_g", "ln_b"]}
    shared["bias_tab"] = bias_tab
    shared.update(c)
    x = np.asarray(inputs["x"], np.float32)
    in_maps = []
    for core in range(8):
        b, half = core // 2, core % 2
        m = dict(shared)
        m["xp"] = np.ascontiguousarray(x[b, 0:SEG])
        m["xm"] = np.ascontiguousarray(x[b, half * SEG:(half + 1) * SEG])
        m["flag"] = np.full((128, 1), float(half), np.float32)
        m["halo_mask"] = np.full((128, 128), 0.0 if half else NEG, np.float32)
        in_maps.append(m)
    return in_maps


def kernel(**inputs):
    if "nc" not in _CACHE:
        p = Prog()
        _CACHE["nc"] = p.build()
        _CACHE["used"] = set(p.in_aps.keys())
    nc = _CACHE["nc"]
    in_maps = [{k: v for k, v in m.items() if k in _CACHE["used"]} for m in _prep_inputs(inputs)]
    res = run_bass_kernel_spmd(nc, in_maps, core_ids=list(range(8)))
    out = np.zeros((4, 2 * SEG, D), np.float32)
    for core in range(8):
        b, half = core // 2, core % 2
        out[b, half * SEG:(half + 1) * SEG] = res.results[core]["y"]
    return out
```

```python
import math
import os
HGL = int(os.environ.get('HGL', '99'))
HGH = int(os.environ.get('HGH', '16'))
HGV = int(os.environ.get('HGV', '3'))
from contextlib import ExitStack

import numpy as np
import concourse.bass as bass
import concourse.mybir as mybir
from concourse.bass_utils import run_bass_kernel_spmd

F32 = mybir.dt.float32
BF16 = mybir.dt.bfloat16
ALU = mybir.AluOpType
AF = mybir.ActivationFunctionType
AX = mybir.AxisListType

D = 2048
NT = 8
SEG = NT * 128
NKC = 16
CAP = 128
NE = 32
ALPHA = 8 ** 0.25
LN_EPS = 1e-5
RMS_EPS = 1e-6
NEG = -1e30
SAME_ENGINE_SYNC = True


class T:
    __slots__ = ("name", "w", "r", "excl")

    def __init__(self, name, excl=False):
        self.name = name
        self.w = None
        self.r = {}
        self.excl = excl


class Sched:
    def __init__(self, nc, es):
        self.nc = nc
        self.eng = {"pe": nc.tensor, "act": nc.scalar, "dve": nc.vector, "pool": nc.gpsimd, "sp": nc.sync}
        self.sem = {}
        self.cnt = {}
        for k in ["pe", "act", "dve", "pool", "sp", "pool_d", "sp_d"]:
            self.sem[k] = es.enter_context(nc.semaphore("sem_" + k))
            self.cnt[k] = 0
        self.waited = {k: {} for k in self.eng}
        self.n_ins = 0

    def _deps(self, e, r, w):
        deps = {}
        for t in r:
            if t.w is not None:
                k, v = t.w
                deps[k] = max(deps.get(k, 0), v)
        for t in w:
            if t.w is not None:
                k, v = t.w
                deps[k] = max(deps.get(k, 0), v)
            for k, v in t.r.items():
                deps[k] = max(deps.get(k, 0), v)
        for k, v in deps.items():
            if k == e and (e == "pe" or not SAME_ENGINE_SYNC):
                continue
            if self.waited[e].get(k, 0) < v:
                self.eng[e].wait_ge(self.sem[k], v)
                self.waited[e][k] = v

    def op(self, e, fn, r=(), w=()):
        xr = tuple(t for t in r if t.excl)
        if xr:
            r = tuple(t for t in r if not t.excl)
            w = tuple(w) + xr
        self._deps(e, r, w)
        ins = fn(self.eng[e])
        ins.then_inc(self.sem[e], 1)
        self.cnt[e] += 1
        self.n_ins += 1
        v = self.cnt[e]
        for t in r:
            t.r[e] = v
        for t in w:
            t.w = (e, v)
            t.r = {}

    def dma(self, q, out, in_, r=(), w=()):
        self._deps(q, r, w)
        dk = q + "_d"
        self.eng[q].dma_start(out=out, in_=in_).then_inc(self.sem[dk], 16)
        self.cnt[dk] += 16
        self.n_ins += 1
        v = self.cnt[dk]
        for t in r:
            t.r[dk] = v
        for t in w:
            t.w = (dk, v)
            t.r = {}

    def wait_all(self, e, tiles):
        self._deps(e, tiles, tiles)


def _t5_bucket_np(dist):
    n = np.clip(dist, 0, 127)
    max_exact = 16
    large = max_exact + (np.log(np.maximum(n, max_exact).astype(np.float32) / np.float32(max_exact))
                         / np.float32(math.log(128 / max_exact)) * np.float32(32 - max_exact)).astype(np.int32)
    large = np.minimum(large, 31)
    return np.where(n < max_exact, n, large)


def host_consts():
    c = {}
    t = np.arange(128)
    ch = t // 64
    same = ch[:, None] == ch[None, :]
    mid = ch * 64 + 31
    L1 = same * ((t[:, None] <= t[None, :]).astype(np.float32) - (t[:, None] <= mid[None, :]).astype(np.float32))
    L2 = same * (t[:, None] > t[None, :]).astype(np.float32)
    sel = np.zeros((128, 4), np.float32)
    sel[:, 0] = t <= 31
    sel[:, 1] = t < 64
    sel[:, 2] = (t >= 64) & (t <= 95)
    sel[:, 3] = t >= 64
    maskT = (same & (t[:, None] <= t[None, :])).astype(np.float32)
    ustrict = (t[:, None] < t[None, :]).astype(np.float32)
    c["c_ident"] = np.eye(128, dtype=np.float32)
    c["c_L1"] = L1.astype(np.float32)
    c["c_L2"] = L2.astype(np.float32)
    c["c_sel"] = sel
    c["c_maskT"] = maskT
    c["c_ustrict"] = ustrict
    c["c_ones"] = np.ones((128, 128), np.float32)
    c["c_iota"] = np.tile(np.arange(CAP, dtype=np.float32)[None, :], (128, 1))
    qi = np.arange(128)[:, None]
    kj = np.arange(256)[None, :]
    dist = qi + 128 - kj
    inwin = (dist >= 0) & (dist < 128)
    c["c_maskc"] = np.where(inwin, 0.0, NEG).astype(np.float32)
    c["_bucket"] = _t5_bucket_np(dist)
    return c


class Prog:
    def __init__(self, debug_stop=None):
        self.debug_stop = debug_stop
        self.nc = bass.Bass("TRN2", target_bir_lowering=False)
        self.es = ExitStack()
        self.in_shapes = {}
        self.in_aps = {}

    def dram_in(self, name, shape):
        self.in_shapes[name] = list(shape)
        return None

    def din(self, name):
        if name not in self.in_aps:
            self.in_aps[name] = self.nc.dram_tensor(name, self.in_shapes[name], F32, kind="ExternalInput").ap()
        return self.in_aps[name]

    def sb(self, name, shape, dt=F32):
        return self.es.enter_context(self.nc.sbuf_tensor("s_" + name, list(shape), dt))

    def build(self):
        nc = self.nc
        es = self.es
        with es:
            self.S = Sched(nc, es)
            self._declare()
            self._program()
        return nc

    def _declare(self):
        nc = self.nc
        di = self.dram_in
        di("xp", [SEG, D]); di("xm", [SEG, D])
        di("a_w_in", [2, D, 4 * D]); di("a_lower_bound", [2, D]); di("a_norm_g", [2, D]); di("a_w_out", [2, D, D])
        di("b_w_kv", [D, 512]); di("b_w_q", [2, D, D]); di("b_sinks", [2, 32]); di("b_w_out", [2, D, D])
        di("bias_tab", [32, 128, 256])
        di("moe_w_rg", [4, D, 4]); di("moe_b_rg", [4, 4]); di("moe_w_re", [4, D, 32]); di("moe_b_re", [4, 32])
        di("moe_w_gate", [4, NE, D, 512]); di("moe_w_up", [4, NE, D, 512]); di("moe_w_down", [4, NE, 512, D])
        di("ln_g", [8, D]); di("ln_b", [8, D]); di("flag", [128, 1]); di("halo_mask", [128, 128])
        for k, shp in [("c_ident", [128, 128]), ("c_L1", [128, 128]), ("c_L2", [128, 128]), ("c_sel", [128, 4]),
                       ("c_maskT", [128, 128]), ("c_ustrict", [128, 128]), ("c_ones", [128, 128]),
                       ("c_iota", [128, CAP]), ("c_maskc", [128, 256])]:
            di(k, shp)
        self.y = nc.dram_tensor("y", [SEG, D], F32, kind="ExternalOutput").ap()
        self.d_state = [nc.dram_tensor(f"d_state{l}", [128, 16 * 128], F32, kind="Internal").ap() for l in range(2)]
        self.d_halo = nc.dram_tensor("d_halo", [128, D], F32, kind="Internal").ap()

        sb = self.sb
        self.X = sb("X", [128, NT, D])
        self.XTraw = sb("XTraw", [128, 8192])
        self.XT = self.XTraw[:].bitcast(BF16).rearrange("p (k t) -> p k t", k=NKC)
        self.OT = sb("OT", [128, NKC, SEG], BF16)
        self.tX = [T(f"X{i}") for i in range(NT)]
        self.tXT = [T(f"XT{i}") for i in range(NT)]
        self.tOT = [T(f"OT{i}") for i in range(NT)]
        self.NB = 3
        self.WB = [sb(f"WB{i}", [128, NKC, 512], BF16) for i in range(self.NB)]
        self.tWB = [T(f"WB{i}") for i in range(self.NB)]
        self.wb_next = 0
        self.c_ident = sb("c_ident", [128, 128])
        self.c_identb = sb("c_identb", [128, 128], BF16)
        self.c_maskc = sb("c_maskc", [128, 256])
        self.c_flag = sb("c_flag", [128, 1])
        self.c_halo = sb("c_halo", [128, 128])
        self.tC = T("consts")
        self.lnst = sb("lnst", [128, 8])
        self.tlnst = T("lnst")
        self.ARN = 7300
        self.AR = sb("arena", [128, self.ARN])
        self.ar_off = 0
        self.PSS = self.es.enter_context(nc.psum_tensor("pss", [128, 1024], F32))
        self.PSG = [self.es.enter_context(nc.psum_tensor(f"psg{i}", [128, 512], F32)) for i in range(2)]
        self.PSBv = [self.PSS[:, 0:512], self.PSS[:, 512:1024], self.PSG[0][:], self.PSG[1][:]]
        self.PSF = [self.es.enter_context(nc.psum_tensor(f"psf{i}", [128, 512], F32)) for i in range(2)]
        self.PST = [self.es.enter_context(nc.psum_tensor(f"pst{i}", [128, 1024], BF16)) for i in range(2)]
        self.tPSB = [T(f"psb{i}", True) for i in range(4)]
        self.tPSF = [T(f"psf{i}", True) for i in range(2)]
        self.tPST = [T(f"pst{i}", True) for i in range(2)]

    def barrier(self):
        S = self.S
        for e in S.eng:
            for k in S.sem:
                if k == e:
                    continue
                if S.cnt[k] > S.waited[e].get(k, 0):
                    S.eng[e].wait_ge(S.sem[k], S.cnt[k])
                    S.waited[e][k] = S.cnt[k]

    def phase(self):
        self.barrier()
        self.ar_off = 0

    def arf(self, cols, shape=None):
        o = self.ar_off
        self.ar_off += cols
        assert self.ar_off <= self.ARN, f"arena overflow {self.ar_off}"
        v = self.AR[:, o:o + cols]
        return v

    def arb(self, cols):
        assert cols % 2 == 0
        return self.arf(cols // 2).bitcast(BF16)

    def wload(self, pieces):
        i = self.wb_next
        self.wb_next = (i + 1) % getattr(self, "NB_active", self.NB)
        buf, t = self.WB[i], self.tWB[i]
        for dst_fn, src in pieces:
            self.S.dma("pool", dst_fn(buf), src, r=(), w=(t,))
        return buf, t

    def load_consts(self):
        S = self.S
        for k in ["c_ident", "c_maskc"]:
            S.dma("sp", getattr(self, k)[:], self.din(k)[:, :], w=(self.tC,))
        S.dma("sp", self.c_flag[:], self.din("flag")[:, :], w=(self.tC,))
        S.dma("sp", self.c_halo[:], self.din("halo_mask")[:, :], w=(self.tC,))
        S.op("dve", lambda e: e.tensor_copy(out=self.c_identb[:], in_=self.c_ident[:]), r=(self.tC,), w=(self.tC,))

    def load_x(self, src):
        for i in range(NT):
            self.S.dma("sp", self.X[:, i, :], src[i * 128:(i + 1) * 128, :], w=(self.tX[i],))

    def make_xt(self):
        S = self.S
        n = 0
        for i in range(NT):
            for g in range(4):
                pb = n % 2
                n += 1
                ps, tps = self.PSF[pb], self.tPSF[pb]
                for j in range(4):
                    kc = g * 4 + j
                    S.op("pe", lambda e, kc=kc, j=j, ps=ps, i=i: e.transpose(
                        ps[:, j * 128:(j + 1) * 128], self.X[:, i, kc * 128:(kc + 1) * 128], self.c_ident[:]),
                        r=(self.tX[i], self.tC), w=(tps,))
                dst = self.XT[:, g * 4:(g + 1) * 4, i * 128:(i + 1) * 128]
                src = ps[:].rearrange("p (j t) -> p j t", j=4)
                if n % 2:
                    S.op("act", lambda e, dst=dst, src=src: e.copy(out=dst, in_=src), r=(tps,), w=(self.tXT[i],))
                else:
                    S.op("dve", lambda e, dst=dst, src=src: e.tensor_copy(out=dst, in_=src), r=(tps,), w=(self.tXT[i],))

    def rsqrt_small(self, out, in_, scale, eps, rt, wt):
        S = self.S
        S.op("act", lambda e: e.activation(out=out, in_=in_, func=AF.Ln, bias=self.epsb(eps), scale=scale), r=rt, w=wt)
        S.op("act", lambda e: e.activation(out=out, in_=out, func=AF.Exp, scale=-0.5), r=wt, w=wt)

    def epsb(self, eps):
        return self.c_eps[eps][:]

    def layernorm(self, idx):
        S = self.S
        self.barrier()
        vg = self.XTraw[:, 0:D]
        vb = self.XTraw[:, D:2 * D]
        tv = T("lnvec")
        S.dma("sp", vg, self.din("ln_g")[idx:idx + 1, :].partition_broadcast(128), w=(tv,))
        S.dma("sp", vb, self.din("ln_b")[idx:idx + 1, :].partition_broadcast(128), w=(tv,))
        st, tst = self.lnst, self.tlnst
        junk = self.OT[:, 0:2, :]
        tj = T("lnjunk")
        for i in range(NT):
            xi = self.X[:, i, :]
            xi3 = xi.rearrange("p (a b) -> p a b", a=2)
            S.op("act", lambda e, xi3=xi3: e.activation(out=junk, in_=xi3, func=AF.Copy, accum_out=st[:, 0:1]),
                 r=(self.tX[i],), w=(tst, tj))
            S.op("act", lambda e, xi3=xi3: e.activation(out=junk, in_=xi3, func=AF.Square, accum_out=st[:, 1:2]),
                 r=(self.tX[i],), w=(tst, tj))
            S.op("dve", lambda e: e.tensor_scalar(out=st[:, 2:4], in0=st[:, 0:2], scalar1=1.0 / D, scalar2=None, op0=ALU.mult),
                 r=(tst,), w=(tst,))
            S.op("dve", lambda e: e.tensor_tensor(out=st[:, 4:5], in0=st[:, 2:3], in1=st[:, 2:3], op=ALU.mult), r=(tst,), w=(tst,))
            S.op("dve", lambda e: e.tensor_tensor(out=st[:, 5:6], in0=st[:, 3:4], in1=st[:, 4:5], op=ALU.subtract), r=(tst,), w=(tst,))
            self.rsqrt_small(st[:, 6:7], st[:, 5:6], 1.0, LN_EPS, (tst,), (tst,))
            S.op("dve", lambda e: e.scalar_tensor_tensor(out=st[:, 7:8], in0=st[:, 2:3], scalar=-1.0, in1=st[:, 6:7], op0=ALU.mult, op1=ALU.mult),
                 r=(tst,), w=(tst,))
            S.op("act", lambda e, xi=xi: e.activation(out=xi, in_=xi, func=AF.Identity, bias=st[:, 7:8], scale=st[:, 6:7]),
                 r=(tst, self.tX[i]), w=(self.tX[i],))
            S.op("dve", lambda e, xi=xi: e.tensor_tensor(out=xi, in0=xi, in1=vg, op=ALU.mult), r=(tv, self.tX[i]), w=(self.tX[i],))
            S.op("dve", lambda e, xi=xi: e.tensor_tensor(out=xi, in0=xi, in1=vb, op=ALU.add), r=(tv, self.tX[i]), w=(self.tX[i],))
        self.barrier()

    def out_proj(self, w_dram):
        S = self.S
        n = 0
        for nch in range(4):
            buf, tb = self.wload([(lambda b: b[:], w_dram[:, nch * 512:(nch + 1) * 512].rearrange("(kc p) n -> p kc n", p=128))])
            for i in range(NT):
                pb = n % 2
                n += 1
                ps, tps = self.PSF[pb], self.tPSF[pb]
                for kc in range(NKC):
                    S.op("pe", lambda e, kc=kc, ps=ps, i=i, buf=buf: e.matmul(
                        ps[:], lhsT=self.OT[:, kc, i * 128:(i + 1) * 128], rhs=buf[:, kc, :], start=(kc == 0), stop=(kc == NKC - 1)),
                        r=(self.tOT[i], tb), w=(tps,))
                xs = self.X[:, i, nch * 512:(nch + 1) * 512]
                S.op("dve", lambda e, xs=xs, ps=ps: e.scalar_tensor_tensor(out=xs, in0=xs, scalar=ALPHA, in1=ps[:], op0=ALU.mult, op1=ALU.add),
                     r=(tps, self.tX[i]), w=(self.tX[i],))

    def hgrn(self, l, init_mode):
        S = self.S
        self.phase()
        f = self.arf
        St = f(2048).rearrange("p (h v) -> p h v", h=16)
        tSt = [T(f"St{h}") for h in range(16)]
        cL1, cL2, cMT, csel = f(128), f(128), f(128), f(4)
        tc = T("hc")
        for dst, k in [(cL1, "c_L1"), (cL2, "c_L2"), (cMT, "c_maskT"), (csel, "c_sel")]:
            S.dma("sp", dst, self.din(k)[:, :], w=(tc,))
        if init_mode == "zero":
            S.op("dve", lambda e: e.memset(St, 0.0), w=tuple(tSt))
        else:
            S.dma("sp", St, self.d_state[l].rearrange("p (h v) -> p h v", h=16), w=tuple(tSt))
            S.op("dve", lambda e: e.tensor_scalar(out=St, in0=St, scalar1=self.c_flag[:, 0:1], scalar2=None, op0=ALU.mult),
                 r=tuple(tSt) + (self.tC,), w=tuple(tSt))
        a0, a1, lb, oml, ng = f(128), f(128), f(128), f(128), f(128)
        tv = T("hvec")
        wb2 = self.WB[2][:].rearrange("p k n -> p (k n)").bitcast(F32)
        wb2_off = [0]

        def f2(cols):
            o = wb2_off[0]
            wb2_off[0] += cols
            assert wb2_off[0] <= 4096
            return wb2[:, o:o + cols]

        def mk_set(ff, sfx):
            fb_ = lambda cols: ff(cols // 2).bitcast(BF16)
            ws = {}
            for k in ["e1", "t1", "fg", "kk", "logf", "eq", "ek", "ekl", "sg", "gg"]:
                ws[k] = ff(128)
            ws["ebT"] = ff(4)
            ws["ss"] = ff(4)
            for k in ["qt", "kt", "kh", "vv", "QF", "KTs", "sTm", "Sp0", "Sp1", "on", "junk"]:
                ws[k] = fb_(128)
            ws["QZ"] = fb_(256).rearrange("p (a b) -> p a b", a=2)
            ws["tw"] = {k: T("hw_" + k + sfx) for k in ["e1", "t1", "fg", "kk", "logf", "eq", "ek", "ekl", "ebT", "sg", "gg", "ss", "qt", "kt", "kh", "vv",
                                                      "QF", "KTs", "sTm", "Sp0", "Sp1", "on", "QZ", "junk"]}
            S.op("dve", lambda e, ws=ws: e.memset(ws["QZ"], 0.0), w=(ws["tw"]["QZ"],))
            return ws

        wsets = [mk_set(f, "a"), mk_set(f2, "b")]
        self.NB_active = 2
        self.wb_next = 0
        w_in = self.din("a_w_in")
        alb = self.din("a_lower_bound")
        ang = self.din("a_norm_g")
        for h in range(HGH):
            hs = slice(h * 128, (h + 1) * 128)
            buf, tb = self.wload([(lambda b, p=p: b[:, :, p * 128:(p + 1) * 128],
                                   w_in[l, :, p * D + h * 128:p * D + (h + 1) * 128].rearrange("(kc p) n -> p kc n", p=128))
                                  for p in range(4)])
            S.dma("sp", ng, ang[l:l + 1, hs].partition_broadcast(128), w=(tv,))
            if l == 0:
                S.op("dve", lambda e: e.memset(lb, 0.0), w=(tv,))
            else:
                S.dma("sp", a0, alb[0:1, hs].partition_broadcast(128), w=(tv,))
                S.dma("sp", a1, alb[1:2, hs].partition_broadcast(128), w=(tv,))
                S.op("dve", lambda e: e.tensor_tensor(out=lb, in0=a0, in1=a1, op=ALU.subtract), r=(tv,), w=(tv,))
                S.op("act", lambda e: e.activation(out=lb, in_=lb, func=AF.Exp), r=(tv,), w=(tv,))
                S.op("dve", lambda e: e.tensor_scalar(out=lb, in0=lb, scalar1=1.0, scalar2=None, op0=ALU.add), r=(tv,), w=(tv,))
                S.op("dve", lambda e: e.reciprocal(out=lb, in_=lb), r=(tv,), w=(tv,))
            S.op("dve", lambda e: e.tensor_scalar(out=oml, in0=lb, scalar1=-1.0, scalar2=1.0, op0=ALU.mult, op1=ALU.add), r=(tv,), w=(tv,))
            def tile_body(i, ws, h=h, buf=buf, tb=tb):
                e1, t1, fg, kk, logf, eq, ek, ekl, ebT, sg, gg, ss = (ws[k] for k in ['e1', 't1', 'fg', 'kk', 'logf', 'eq', 'ek', 'ekl', 'ebT', 'sg', 'gg', 'ss'])
                qt, kt, kh, vv, QF, KTs, sTm, Sp0, Sp1, on, junk, QZ = (ws[k] for k in ['qt', 'kt', 'kh', 'vv', 'QF', 'KTs', 'sTm', 'Sp0', 'Sp1', 'on', 'junk', 'QZ'])
                tw = ws['tw']
                ts = slice(i * 128, (i + 1) * 128)
                pp, tpp = self.PSF[i % 2], self.tPSF[i % 2]
                for kc in range(NKC):
                    S.op("pe", lambda e, kc=kc, pp=pp, ts=ts, buf=buf: e.matmul(pp[:], lhsT=self.XT[:, kc, ts], rhs=buf[:, kc, :],
                                                                                 start=(kc == 0), stop=(kc == NKC - 1)),
                         r=(self.tXT[i], tb), w=(tpp,))
                if HGL < 1:
                    return
                pq, pf, pi_, pg = pp[:, 0:128], pp[:, 128:256], pp[:, 256:384], pp[:, 384:512]
                S.op("act", lambda e: e.activation(out=e1, in_=pf, func=AF.Exp, scale=-1.0), r=(tpp,), w=(tw["e1"],))
                S.op("dve", lambda e: e.tensor_scalar(out=e1, in0=e1, scalar1=1.0, scalar2=None, op0=ALU.add), r=(tw["e1"],), w=(tw["e1"],))
                S.op("dve", lambda e: e.reciprocal(out=e1, in_=e1), r=(tw["e1"],), w=(tw["e1"],))
                S.op("dve", lambda e: e.tensor_tensor(out=t1, in0=e1, in1=oml, op=ALU.mult), r=(tw["e1"], tv), w=(tw["t1"],))
                S.op("dve", lambda e: e.tensor_tensor(out=fg, in0=t1, in1=lb, op=ALU.add), r=(tw["t1"], tv), w=(tw["fg"],))
                S.op("dve", lambda e: e.tensor_scalar(out=fg, in0=fg, scalar1=1e-30, scalar2=None, op0=ALU.max), r=(tw["fg"],), w=(tw["fg"],))
                S.op("act", lambda e: e.activation(out=logf, in_=fg, func=AF.Ln), r=(tw["fg"],), w=(tw["logf"],))
                S.op("dve", lambda e: e.tensor_tensor(out=kk, in0=oml, in1=t1, op=ALU.subtract), r=(tw["t1"], tv), w=(tw["kk"],))
                if HGL < 2:
                    return
                pc, tpc = self.PSBv[0], self.tPSB[0]
                S.op("pe", lambda e: e.matmul(pc[:, 0:128], lhsT=cL1, rhs=logf, start=True, stop=True), r=(tc, tw["logf"]), w=(tpc,))
                if HGV >= 2:
                    S.op("pe", lambda e: e.matmul(pc[:, 128:256], lhsT=cL2, rhs=logf, start=True, stop=True), r=(tc, tw["logf"]), w=(tpc,))
                if HGV >= 3:
                    S.op("pe", lambda e: e.matmul(pc[:, 256:260], lhsT=logf, rhs=csel, start=True, stop=True), r=(tc, tw["logf"]), w=(tpc,))
                if HGL < 3 or os.environ.get('HGX') == '1':
                    return
                S.op("act", lambda e: e.activation(out=eq, in_=pc[:, 0:128], func=AF.Exp), r=(tpc,), w=(tw["eq"],))
                S.op("act", lambda e: e.activation(out=ek, in_=pc[:, 0:128], func=AF.Exp, scale=-1.0), r=(tpc,), w=(tw["ek"],))
                S.op("act", lambda e: e.activation(out=ekl, in_=pc[:, 128:256], func=AF.Exp), r=(tpc,), w=(tw["ekl"],))
                S.op("act", lambda e: e.activation(out=ebT, in_=pc[:, 256:260], func=AF.Exp), r=(tpc,), w=(tw["ebT"],))
                if os.environ.get('HGX') == '2':
                    return
                S.op("dve", lambda e: e.tensor_tensor(out=qt, in0=pq, in1=eq, op=ALU.mult), r=(tpp, tw["eq"]), w=(tw["qt"],))
                S.op("dve", lambda e: e.tensor_tensor(out=kt, in0=kk, in1=ek, op=ALU.mult), r=(tw["kk"], tw["ek"]), w=(tw["kt"],))
                S.op("dve", lambda e: e.tensor_tensor(out=kh, in0=kk, in1=ekl, op=ALU.mult), r=(tw["kk"], tw["ekl"]), w=(tw["kh"],))
                if os.environ.get('HGX') == '3':
                    return
                S.op("dve", lambda e: e.tensor_copy(out=vv, in_=pi_), r=(tpp,), w=(tw["vv"],))
                if os.environ.get('HGX') == '4':
                    return
                S.op("act", lambda e: e.activation(out=sg, in_=pg, func=AF.Exp, scale=-1.0), r=(tpp,), w=(tw["sg"],))
                S.op("dve", lambda e: e.tensor_scalar(out=sg, in0=sg, scalar1=1.0, scalar2=None, op0=ALU.add), r=(tw["sg"],), w=(tw["sg"],))
                S.op("dve", lambda e: e.reciprocal(out=sg, in_=sg), r=(tw["sg"],), w=(tw["sg"],))
                S.op("dve", lambda e: e.tensor_tensor(out=gg, in0=pg, in1=sg, op=ALU.mult), r=(tpp, tw["sg"]), w=(tw["gg"],))
                S.op("dve", lambda e: e.tensor_tensor(out=gg, in0=gg, in1=ng, op=ALU.mult), r=(tw["gg"], tv), w=(tw["gg"],))
                if HGL < 4:
                    return
                pt, tpt = self.PST[0], self.tPST[0]
                S.op("pe", lambda e: e.transpose(pt[:, 0:128], qt, self.c_identb[:]), r=(tw["qt"], self.tC), w=(tpt,))
                S.op("pe", lambda e: e.transpose(pt[:, 128:256], kt, self.c_identb[:]), r=(tw["kt"], self.tC), w=(tpt,))
                S.op("dve", lambda e: e.tensor_copy(out=QF, in_=pt[:, 0:128]), r=(tpt,), w=(tw["QF"],))
                S.op("dve", lambda e: e.tensor_copy(out=QZ[:, 0, 0:64], in_=pt[:, 0:64]), r=(tpt,), w=(tw["QZ"],))
                S.op("dve", lambda e: e.tensor_copy(out=QZ[:, 1, 64:128], in_=pt[:, 64:128]), r=(tpt,), w=(tw["QZ"],))
                S.op("dve", lambda e: e.tensor_copy(out=KTs, in_=pt[:, 128:256]), r=(tpt,), w=(tw["KTs"],))
                if HGL < 5:
                    return
                psc, tpsc = self.PSBv[1], self.tPSB[1]
                S.op("pe", lambda e: e.matmul(psc[:, 0:128], lhsT=KTs, rhs=QF, start=True, stop=True), r=(tw["KTs"], tw["QF"]), w=(tpsc,))
                S.op("dve", lambda e: e.tensor_tensor(out=sTm, in0=psc[:, 0:128], in1=cMT, op=ALU.mult), r=(tpsc, tc), w=(tw["sTm"],))
                if HGL < 6:
                    return
                pds, tpds = self.PSBv[2], self.tPSB[2]
                Sh = St[:, h, :]
                S.op("dve", lambda e: e.tensor_scalar(out=Sp0, in0=Sh, scalar1=ebT[:, 0:1], scalar2=None, op0=ALU.mult),
                     r=(tSt[h], tw["ebT"]), w=(tw["Sp0"],))
                S.op("pe", lambda e: e.matmul(pds[:, 0:128], lhsT=kh[0:64, :], rhs=vv[0:64, :], start=True, stop=True),
                     r=(tw["kh"], tw["vv"]), w=(tpds,))
                S.op("dve", lambda e: e.scalar_tensor_tensor(out=Sh, in0=Sh, scalar=ebT[:, 1:2], in1=pds[:, 0:128], op0=ALU.mult, op1=ALU.add),
                     r=(tSt[h], tw["ebT"], tpds), w=(tSt[h],))
                S.op("dve", lambda e: e.tensor_scalar(out=Sp1, in0=Sh, scalar1=ebT[:, 2:3], scalar2=None, op0=ALU.mult),
                     r=(tSt[h], tw["ebT"]), w=(tw["Sp1"],))
                S.op("pe", lambda e: e.matmul(pds[:, 128:256], lhsT=kh[64:128, :], rhs=vv[64:128, :], start=True, stop=True),
                     r=(tw["kh"], tw["vv"]), w=(tpds,))
                S.op("dve", lambda e: e.scalar_tensor_tensor(out=Sh, in0=Sh, scalar=ebT[:, 3:4], in1=pds[:, 128:256], op0=ALU.mult, op1=ALU.add),
                     r=(tSt[h], tw["ebT"], tpds), w=(tSt[h],))
                if HGL < 7:
                    return
                po, tpo = self.PSBv[3], self.tPSB[3]
                S.op("pe", lambda e: e.matmul(po[:, 0:128], lhsT=sTm, rhs=vv, start=True, stop=False), r=(tw["sTm"], tw["vv"]), w=(tpo,))
                S.op("pe", lambda e: e.matmul(po[:, 0:128], lhsT=QZ[:, 0, :], rhs=Sp0, start=False, stop=False), r=(tw["QZ"], tw["Sp0"]), w=(tpo,))
                S.op("pe", lambda e: e.matmul(po[:, 0:128], lhsT=QZ[:, 1, :], rhs=Sp1, start=False, stop=True), r=(tw["QZ"], tw["Sp1"]), w=(tpo,))
                if HGL < 8:
                    return
                S.op("act", lambda e: e.activation(out=junk, in_=po[:, 0:128], func=AF.Square, accum_out=ss[:, 0:1]), r=(tpo,), w=(tw["ss"], tw["junk"]))
                self.rsqrt_small(ss[:, 1:2], ss[:, 0:1], 1.0 / 128, RMS_EPS, (tw["ss"],), (tw["ss"],))
                S.op("dve", lambda e: e.scalar_tensor_tensor(out=on, in0=po[:, 0:128], scalar=ss[:, 1:2], in1=gg, op0=ALU.mult, op1=ALU.mult),
                     r=(tpo, tw["ss"], tw["gg"]), w=(tw["on"],))
                if HGL < 9:
                    return
                pt1, tpt1 = self.PST[1], self.tPST[1]
                S.op("pe", lambda e: e.transpose(pt1[:, 0:128], on, self.c_identb[:]), r=(tw["on"], self.tC), w=(tpt1,))
                S.op("dve", lambda e, h=h, ts=ts: e.tensor_copy(out=self.OT[:, h, ts], in_=pt1[:, 0:128]), r=(tpt1,), w=(self.tOT[i],))

            LAG = 30
            recs = []
            for i in range(NT):
                rec = []
                real_op = S.op
                S.op = lambda e, fn, r=(), w=(), rec=rec: rec.append((e, fn, tuple(r), tuple(w)))
                try:
                    tile_body(i, wsets[i % 2])
                finally:
                    S.op = real_op
                recs.append(rec)
            active = []
            nxt = 0
            while nxt < NT or active:
                if nxt < NT and len(active) < 2 and (not active or active[-1][1] >= min(LAG, len(active[-1][0]))):
                    active.append([recs[nxt], 0])
                    nxt += 1
                for a in list(active):
                    if a[1] < len(a[0]):
                        e_, fn_, r_, w_ = a[0][a[1]]
                        S.op(e_, fn_, r=r_, w=w_)
                        a[1] += 1
                active = [a for a in active if a[1] < len(a[0])]
        S.dma("sp", self.d_state[l].rearrange("p (h v) -> p h v", h=16), St, r=tuple(tSt))
        self.barrier()
        self.NB_active = 3
        self.wb_next = 0

    def moe(self, L):
        S = self.S
        self.barrier()
        X1b = self.OT[:].rearrange("p k t -> p (k t)").rearrange("p (i d) -> p i d", i=NT)
        tX1b = [T(f"x1b{i}") for i in range(NT)]
        off = [0]

        def f(cols):
            o = off[0]
            off[0] += cols
            assert off[0] <= 8192, off[0]
            return self.XTraw[:, o:o + cols]

        def fb(cols):
            return f(cols // 2).bitcast(BF16)

        wr = f(NKC * 36).rearrange("p (k n) -> p k n", k=NKC)
        rb = f(36)
        ustrict, ones, iota = f(128), f(128), f(CAP)
        xT4 = [f(512), f(512)]
        A = f(NT * 32).rearrange("p (i e) -> p i e", i=NT)
        Gt = f(NT * 32).rearrange("p (i e) -> p i e", i=NT)
        VAL = f(NT * 32).rearrange("p (i e) -> p i e", i=NT)
        lg, gsel, pen, elm, elm2, oh1, oh2 = f(36), f(4), f(4), f(32), f(32), f(32), f(32)
        sm = f(16)
        junk4 = f(4)
        P = fb(NT * CAP).rearrange("p (i c) -> p i c", i=NT)
        Pg = fb(NT * CAP).rearrange("p (i c) -> p i c", i=NT)
        PgT = fb(NT * 128).rearrange("p (i t) -> p i t", i=NT)
        XS = fb(NKC * CAP).rearrange("p (k c) -> p k c", k=NKC)
        HT = fb(4 * CAP).rearrange("p (k c) -> p k c", k=4)
        hsg = f(4 * CAP)
        Y = fb(D)
        tc_ = T("mc")
        tr = T("mrt")
        tA = [T(f"mA{i}") for i in range(NT)]
        txT4 = [T("xT4a"), T("xT4b")]
        tP, tPg, tPgT, tXS, tHT, thsg, tY = T("P"), T("Pg"), T("PgT"), T("XS"), T("HT"), T("hsg"), T("Y")

        S.dma("sp", wr[:, :, 0:4], self.din("moe_w_rg")[L].rearrange("(k p) n -> p k n", p=128), w=(tc_,))
        S.dma("sp", wr[:, :, 4:36], self.din("moe_w_re")[L].rearrange("(k p) n -> p k n", p=128), w=(tc_,))
        S.dma("sp", rb[:, 0:4], self.din("moe_b_rg")[L:L + 1, :].partition_broadcast(128), w=(tc_,))
        S.dma("sp", rb[:, 4:36], self.din("moe_b_re")[L:L + 1, :].partition_broadcast(128), w=(tc_,))
        S.dma("sp", ustrict, self.din("c_ustrict")[:, :], w=(tc_,))
        S.dma("sp", ones, self.din("c_ones")[:, :], w=(tc_,))
        S.dma("sp", iota, self.din("c_iota")[:, :], w=(tc_,))

        for i in range(NT):
            if i % 2:
                S.op("act", lambda e, i=i: e.copy(out=X1b[:, i, :], in_=self.X[:, i, :]), r=(self.tX[i],), w=(tX1b[i],))
            else:
                S.op("dve", lambda e, i=i: e.tensor_copy(out=X1b[:, i, :], in_=self.X[:, i, :]), r=(self.tX[i],), w=(tX1b[i],))

        n = 0
        plg, tplg = self.PSBv[0], self.tPSB[0]
        for i in range(NT):
            for g in range(4):
                pb = n % 2
                n += 1
                ps, tps = self.PSF[pb], self.tPSF[pb]
                for j in range(4):
                    kc = g * 4 + j
                    S.op("pe", lambda e, kc=kc, j=j, ps=ps, i=i: e.transpose(
                        ps[:, j * 128:(j + 1) * 128], self.X[:, i, kc * 128:(kc + 1) * 128], self.c_ident[:]),
                        r=(self.tX[i], self.tC), w=(tps,))
                xt, txt = xT4[pb], txT4[pb]
                if pb:
                    S.op("act", lambda e, xt=xt, ps=ps: e.copy(out=xt, in_=ps[:]), r=(tps,), w=(txt,))
                else:
                    S.op("dve", lambda e, xt=xt, ps=ps: e.tensor_copy(out=xt, in_=ps[:]), r=(tps,), w=(txt,))
                for j in range(4):
                    kc = g * 4 + j
                    S.op("pe", lambda e, kc=kc, j=j, xt=xt: e.matmul(plg[:, 0:36], lhsT=xt[:, j * 128:(j + 1) * 128], rhs=wr[:, kc, :],
                                                                      start=(kc == 0), stop=(kc == NKC - 1)),
                         r=(txt, tc_), w=(tplg,))
            dv = lambda fn, r, w: S.op("dve", fn, r=r, w=w)
            dv(lambda e: e.tensor_tensor(out=lg, in0=plg[:, 0:36], in1=rb, op=ALU.add), (tplg, tc_), (tr,))
            gl, el = lg[:, 0:4], lg[:, 4:36]
            gmax, ngmax, sume, pgrp, m1, m2, dd, g1, g2 = (sm[:, k:k + 1] for k in range(9))
            dv(lambda e: e.tensor_reduce(out=gmax, in_=gl, axis=AX.X, op=ALU.max), (tr,), (tr,))
            dv(lambda e: e.tensor_scalar(out=gsel, in0=gl, scalar1=gmax, scalar2=None, op0=ALU.is_equal), (tr,), (tr,))
            dv(lambda e: e.tensor_scalar(out=ngmax, in0=gmax, scalar1=-1.0, scalar2=None, op0=ALU.mult), (tr,), (tr,))
            S.op("act", lambda e: e.activation(out=junk4, in_=gl, func=AF.Exp, bias=ngmax, scale=1.0, accum_out=sume), r=(tr,), w=(tr,))
            dv(lambda e: e.reciprocal(out=pgrp, in_=sume), (tr,), (tr,))
            dv(lambda e: e.tensor_scalar(out=pen, in0=gsel, scalar1=-1.0, scalar2=1e30, op0=ALU.add, op1=ALU.mult), (tr,), (tr,))
            for g in range(4):
                dv(lambda e, g=g: e.tensor_scalar(out=elm[:, g * 8:(g + 1) * 8], in0=el[:, g * 8:(g + 1) * 8], scalar1=pen[:, g:g + 1],
                                                  scalar2=None, op0=ALU.add), (tr,), (tr,))
            dv(lambda e: e.tensor_reduce(out=m1, in_=elm, axis=AX.X, op=ALU.max), (tr,), (tr,))
            dv(lambda e: e.tensor_scalar(out=oh1, in0=elm, scalar1=m1, scalar2=None, op0=ALU.is_equal), (tr,), (tr,))
            dv(lambda e: e.scalar_tensor_tensor(out=elm2, in0=oh1, scalar=-1e30, in1=elm, op0=ALU.mult, op1=ALU.add), (tr,), (tr,))
            dv(lambda e: e.tensor_reduce(out=m2, in_=elm2, axis=AX.X, op=ALU.max), (tr,), (tr,))
            dv(lambda e: e.tensor_scalar(out=oh2, in0=elm2, scalar1=m2, scalar2=None, op0=ALU.is_equal), (tr,), (tr,))
            dv(lambda e: e.tensor_tensor(out=dd, in0=m2, in1=m1, op=ALU.subtract), (tr,), (tr,))
            S.op("act", lambda e: e.activation(out=dd, in_=dd, func=AF.Exp), r=(tr,), w=(tr,))
            dv(lambda e: e.tensor_scalar(out=g1, in0=dd, scalar1=1.0, scalar2=None, op0=ALU.add), (tr,), (tr,))
            dv(lambda e: e.reciprocal(out=g1, in_=g1), (tr,), (tr,))
            dv(lambda e: e.tensor_tensor(out=g2, in0=dd, in1=g1, op=ALU.mult), (tr,), (tr,))
            dv(lambda e: e.tensor_tensor(out=g1, in0=g1, in1=pgrp, op=ALU.mult), (tr,), (tr,))
            dv(lambda e: e.tensor_tensor(out=g2, in0=g2, in1=pgrp, op=ALU.mult), (tr,), (tr,))
            dv(lambda e, i=i: e.tensor_tensor(out=A[:, i, :], in0=oh1, in1=oh2, op=ALU.add), (tr,), (tA[i],))
            dv(lambda e, i=i: e.tensor_scalar(out=Gt[:, i, :], in0=oh1, scalar1=g1, scalar2=None, op0=ALU.mult), (tr,), (tA[i],))
            dv(lambda e, i=i: e.scalar_tensor_tensor(out=Gt[:, i, :], in0=oh2, scalar=g2, in1=Gt[:, i, :], op0=ALU.mult, op1=ALU.add),
               (tr, tA[i]), (tA[i],))
        pr, tpr = self.PSBv[1], self.tPSB[1]
        for i in range(NT):
            for j in range(i):
                S.op("pe", lambda e, j=j: e.matmul(pr[:, 0:32], lhsT=ones, rhs=A[:, j, :], start=(j == 0), stop=False), r=(tc_, tA[j]), w=(tpr,))
            S.op("pe", lambda e, i=i: e.matmul(pr[:, 0:32], lhsT=ustrict, rhs=A[:, i, :], start=(i == 0), stop=True), r=(tc_, tA[i]), w=(tpr,))
            S.op("dve", lambda e, i=i: e.scalar_tensor_tensor(out=VAL[:, i, :], in0=pr[:, 0:32], scalar=1.0, in1=A[:, i, :], op0=ALU.add, op1=ALU.mult),
                 r=(tpr, tA[i]), w=(tA[i],))
            S.op("dve", lambda e, i=i: e.tensor_scalar(out=VAL[:, i, :], in0=VAL[:, i, :], scalar1=-1.0, scalar2=None, op0=ALU.add),
                 r=(tA[i],), w=(tA[i],))
        for i in range(NT):
            if i % 2:
                S.op("act", lambda e, i=i: e.mul(out=self.X[:, i, :], in_=self.X[:, i, :], mul=ALPHA), r=(self.tX[i],), w=(self.tX[i],))
            else:
                S.op("dve", lambda e, i=i: e.tensor_scalar(out=self.X[:, i, :], in0=self.X[:, i, :], scalar1=ALPHA, scalar2=None, op0=ALU.mult),
                     r=(self.tX[i],), w=(self.tX[i],))
        wg_d, wu_d, wd_d = self.din("moe_w_gate"), self.din("moe_w_up"), self.din("moe_w_down")
        ncp = 0
        for ex in range(NE):
            wg, twg = self.wload([(lambda b: b[:], wg_d[L, ex].rearrange("(kc p) n -> p kc n", p=128))])
            wu, twu = self.wload([(lambda b: b[:], wu_d[L, ex].rearrange("(kc p) n -> p kc n", p=128))])
            wdv = lambda b: b[:].rearrange("p k n -> p (k n)").rearrange("p (fc d) -> p fc d", fc=4)
            wd_, twd = self.wload([(wdv, wd_d[L, ex].rearrange("(fc p) d -> p fc d", p=128))])
            wd = wdv(wd_)
            for i in range(NT):
                S.op("dve", lambda e, i=i, ex=ex: e.tensor_scalar(out=P[:, i, :], in0=iota, scalar1=VAL[:, i, ex:ex + 1], scalar2=None, op0=ALU.is_equal),
                     r=(tc_, tA[i]), w=(tP,))
                S.op("dve", lambda e, i=i, ex=ex: e.tensor_scalar(out=Pg[:, i, :], in0=iota, scalar1=VAL[:, i, ex:ex + 1], scalar2=Gt[:, i, ex:ex + 1],
                                                                  op0=ALU.is_equal, op1=ALU.mult), r=(tc_, tA[i]), w=(tPg,))
            pt, tpt = self.PST[0], self.tPST[0]
            for i in range(NT):
                S.op("pe", lambda e, i=i: e.transpose(pt[:, i * 128:(i + 1) * 128], Pg[:, i, :], self.c_identb[:]), r=(tPg, self.tC), w=(tpt,))
            S.op("act", lambda e: e.copy(out=PgT, in_=pt[:].rearrange("p (i t) -> p i t", i=NT)), r=(tpt,), w=(tPgT,))
            for g in range(4):
                ps, tps = self.PSF[g % 2], self.tPSF[g % 2]
                for j in range(4):
                    kc = g * 4 + j
                    for i in range(NT):
                        S.op("pe", lambda e, kc=kc, j=j, i=i, ps=ps: e.matmul(ps[:, j * CAP:(j + 1) * CAP], lhsT=X1b[:, i, kc * 128:(kc + 1) * 128],
                                                                               rhs=P[:, i, :], start=(i == 0), stop=(i == NT - 1)),
                             r=(tX1b[i], tP), w=(tps,))
                dst = XS[:, g * 4:(g + 1) * 4, :]
                src = ps[:, 0:4 * CAP].rearrange("p (j c) -> p j c", j=4)
                if g % 2:
                    S.op("act", lambda e, dst=dst, src=src: e.copy(out=dst, in_=src), r=(tps,), w=(tXS,))
                else:
                    S.op("dve", lambda e, dst=dst, src=src: e.tensor_copy(out=dst, in_=src), r=(tps,), w=(tXS,))
            phg, tphg = self.PSBv[0], self.tPSB[0]
            phu, tphu = self.PSBv[1], self.tPSB[1]
            for (pw, tpw, wt, twt) in ((phg, tphg, wg, twg), (phu, tphu, wu, twu)):
                for fc in range(4):
                    for kc in range(NKC):
                        S.op("pe", lambda e, fc=fc, kc=kc, pw=pw, wt=wt: e.matmul(pw[:, fc * CAP:(fc + 1) * CAP], lhsT=wt[:, kc, fc * 128:(fc + 1) * 128],
                                                                                   rhs=XS[:, kc, :], start=(kc == 0), stop=(kc == NKC - 1)),
                             r=(twt, tXS), w=(tpw,))
            S.op("act", lambda e: e.activation(out=hsg, in_=phg[:, 0:4 * CAP], func=AF.Exp, scale=-1.0), r=(tphg,), w=(thsg,))
            S.op("dve", lambda e: e.tensor_scalar(out=hsg, in0=hsg, scalar1=1.0, scalar2=None, op0=ALU.add), r=(thsg,), w=(thsg,))
            S.op("dve", lambda e: e.reciprocal(out=hsg, in_=hsg), r=(thsg,), w=(thsg,))
            S.op("dve", lambda e: e.tensor_tensor(out=hsg, in0=hsg, in1=phg[:, 0:4 * CAP], op=ALU.mult), r=(thsg, tphg), w=(thsg,))
            S.op("dve", lambda e: e.tensor_tensor(out=HT[:].rearrange("p k c -> p (k c)"), in0=hsg, in1=phu[:, 0:4 * CAP], op=ALU.mult),
                 r=(thsg, tphu), w=(tHT,))
            for nn in range(4):
                py, tpy = self.PSBv[2 + nn % 2], self.tPSB[2 + nn % 2]
                for fc in range(4):
                    S.op("pe", lambda e, fc=fc, nn=nn, py=py: e.matmul(py[0:CAP, :], lhsT=HT[:, fc, :], rhs=wd[:, fc, nn * 512:(nn + 1) * 512],
                                                                        start=(fc == 0), stop=(fc == 3)), r=(tHT, twd), w=(tpy,))
                if nn % 2:
                    S.op("act", lambda e, nn=nn, py=py: e.copy(out=Y[0:CAP, nn * 512:(nn + 1) * 512], in_=py[0:CAP, :]), r=(tpy,), w=(tY,))
                else:
                    S.op("dve", lambda e, nn=nn, py=py: e.tensor_copy(out=Y[0:CAP, nn * 512:(nn + 1) * 512], in_=py[0:CAP, :]), r=(tpy,), w=(tY,))
            for i in range(NT):
                for nn in range(4):
                    pcb, tpcb = self.PSF[ncp % 2], self.tPSF[ncp % 2]
                    ncp += 1
                    S.op("pe", lambda e, i=i, nn=nn, pcb=pcb: e.matmul(pcb[:], lhsT=PgT[0:CAP, i, :], rhs=Y[0:CAP, nn * 512:(nn + 1) * 512], start=True, stop=True),
                         r=(tPgT, tY), w=(tpcb,))
                    xs = self.X[:, i, nn * 512:(nn + 1) * 512]
                    S.op("dve", lambda e, xs=xs, pcb=pcb: e.tensor_tensor(out=xs, in0=xs, in1=pcb[:], op=ALU.add), r=(tpcb, self.tX[i]), w=(self.tX[i],))
        self.barrier()

    def kv_alloc(self):
        self.KT2 = self.arb(4 * 1152).rearrange("p (h t) -> p h t", h=4)
        self.V = self.arb(9 * 256).rearrange("p (b c) -> p b c", b=9)
        self.tKV = T("KV")
        self.att_base = self.ar_off

    def save_halo(self):
        self.S.dma("sp", self.d_halo[:, :], self.X[:, NT - 1, :], r=(self.tX[NT - 1],))

    def kv_compute(self):
        S = self.S
        self.ar_off = self.att_base
        Xh = self.arf(D)
        XTh = self.arb(NKC * 128).rearrange("p (k t) -> p k t", k=NKC)
        tXh, tXTh = T("Xh"), T("XTh")
        S.dma("sp", Xh, self.d_halo[:, :], w=(tXh,))
        for g in range(4):
            ps, tps = self.PSF[g % 2], self.tPSF[g % 2]
            for j in range(4):
                kc = g * 4 + j
                S.op("pe", lambda e, kc=kc, j=j, ps=ps: e.transpose(ps[:, j * 128:(j + 1) * 128], Xh[:, kc * 128:(kc + 1) * 128], self.c_ident[:]),
                     r=(tXh, self.tC), w=(tps,))
            S.op("dve", lambda e, g=g, ps=ps: e.tensor_copy(out=XTh[:, g * 4:(g + 1) * 4, :], in_=ps[:].rearrange("p (j t) -> p j t", j=4)),
                 r=(tps,), w=(tXTh,))
        wkv = self.din("b_w_kv")
        bufv, tbv = self.wload([(lambda b: b[:], wkv.rearrange("(kc p) n -> p kc n", p=128))])
        wkd_v = lambda b: b[:].rearrange("p k (h f) -> p k h f", h=4)
        pieces = []
        for hk in range(4):
            for dup in range(2):
                pieces.append((lambda b, hk=hk, dup=dup: wkd_v(b)[:, :, hk, dup * 64:(dup + 1) * 64],
                               wkv[:, hk * 64:(hk + 1) * 64].rearrange("(kc p) n -> p kc n", p=128)))
        bufk_, tbk = self.wload(pieces)
        bufk = wkd_v(bufk_)
        n = 0
        for b in range(9):
            ps, tps = self.PSF[n % 2], self.tPSF[n % 2]
            n += 1
            for kc in range(NKC):
                lhsT = XTh[:, kc, :] if b == 0 else self.XT[:, kc, (b - 1) * 128:b * 128]
                rt = (tXTh,) if b == 0 else (self.tXT[b - 1],)
                S.op("pe", lambda e, kc=kc, ps=ps, lhsT=lhsT: e.matmul(ps[:, 0:256], lhsT=lhsT, rhs=bufv[:, kc, 256:512], start=(kc == 0), stop=(kc == NKC - 1)),
                     r=rt + (tbv,), w=(tps,))
            S.op("dve", lambda e, b=b, ps=ps: e.tensor_copy(out=self.V[:, b, :], in_=ps[:, 0:256]), r=(tps,), w=(self.tKV,))
        for hk in range(4):
            for part in range(3):
                ps, tps = self.PSF[n % 2], self.tPSF[n % 2]
                n += 1
                if part == 0:
                    rhs_fn, ncol, rt, dst = (lambda kc: XTh[:, kc, :]), 128, (tXTh,), self.KT2[:, hk, 0:128]
                else:
                    lo = (part - 1) * 512
                    rhs_fn, ncol, rt = (lambda kc, lo=lo: self.XT[:, kc, lo:lo + 512]), 512, tuple(self.tXT[(part - 1) * 4:(part - 1) * 4 + 4])
                    dst = self.KT2[:, hk, 128 + lo:128 + lo + 512]
                for kc in range(NKC):
                    S.op("pe", lambda e, kc=kc, ps=ps, hk=hk, rhs_fn=rhs_fn, ncol=ncol: e.matmul(ps[:, 0:ncol], lhsT=bufk[:, kc, hk, :], rhs=rhs_fn(kc),
                                                                                                 start=(kc == 0), stop=(kc == NKC - 1)),
                         r=rt + (tbk,), w=(tps,))
                S.op("act", lambda e, ps=ps, ncol=ncol, dst=dst: e.copy(out=dst, in_=ps[:, 0:ncol]), r=(tps,), w=(self.tKV,))
        self.barrier()

    def attention(self, j_layer):
        S = self.S
        self.barrier()
        self.ar_off = self.att_base
        f = self.arf
        biasm = f(1024).rearrange("p (h k) -> p h k", h=4)
        PB = self.arb(1024).rearrange("p (h k) -> p h k", h=4)
        PTs = self.arb(1024).rearrange("p (a q) -> p a q", a=8)
        QTg = self.arb(2048).rearrange("p (c t) -> p c t", c=2)
        OB = self.arb(256)
        SS2 = f(512).rearrange("p (h k) -> p h k", h=2)
        tSS = T("SS2")
        sm = f(32)
        sink, rm, negm, rs, es, rinv = (sm[:, 4 * k:4 * k + 4] for k in range(6))
        tb, tPB, tPTs, tQ, tOB, tsm = T("biasm"), T("PB"), T("PTs"), T("QTg"), T("OB"), T("asm")
        wq = self.din("b_w_q")
        btab = self.din("bias_tab")
        sinks = self.din("b_sinks")
        PSS3 = self.PSS[:].rearrange("p (h k) -> p h k", h=4)
        tS = (self.tPSB[0], self.tPSB[1])
        n = 0
        for hk in range(4):
            bufq, tbq = self.wload([(lambda b: b[:], wq[j_layer, :, hk * 512:(hk + 1) * 512].rearrange("(kc p) n -> p kc n", p=128))])
            for hh in range(2):
                h0 = 8 * hk + 4 * hh
                c0 = h0 // 2
                for cc in range(2):
                    for half in range(2):
                        ps, tps = self.PSF[n % 2], self.tPSF[n % 2]
                        n += 1
                        for kc in range(NKC):
                            S.op("pe", lambda e, kc=kc, ps=ps, cc=cc, half=half, hh=hh, bufq=bufq: e.matmul(
                                ps[:], lhsT=bufq[:, kc, hh * 256 + cc * 128:hh * 256 + (cc + 1) * 128], rhs=self.XT[:, kc, half * 512:(half + 1) * 512],
                                start=(kc == 0), stop=(kc == NKC - 1)), r=tuple(self.tXT[half * 4:half * 4 + 4]) + (tbq,), w=(tps,))
                        S.op("act", lambda e, ps=ps, cc=cc, half=half: e.copy(out=QTg[:, cc, half * 512:(half + 1) * 512], in_=ps[:]), r=(tps,), w=(tQ,))
                pos = lambda j: (j % 2) * 2 + j // 2
                for j in range(4):
                    S.dma("sp", biasm[:, pos(j), :], btab[h0 + j], w=(tb,))
                    S.dma("sp", sink[:, pos(j):pos(j) + 1], sinks[j_layer:j_layer + 1, h0 + j:h0 + j + 1].partition_broadcast(128), w=(tsm,))
                for j in range(4):
                    S.op("dve", lambda e, j=j: e.tensor_tensor(out=biasm[:, j, :], in0=biasm[:, j, :], in1=self.c_maskc[:], op=ALU.add), r=(tb, self.tC), w=(tb,))
                for nb in range(NT):
                    if os.environ.get("ATT_STOP") == "q":
                        continue
                    qs = slice(nb * 128, (nb + 1) * 128)
                    for j in range(4):
                        p, cc = j % 2, j // 2
                        S.op("pe", lambda e, j=j, p=p, cc=cc, qs=qs, nb=nb, hk=hk: e.matmul(
                            self.PSS[:, pos(j) * 256:(pos(j) + 1) * 256], lhsT=QTg[p * 64:(p + 1) * 64, cc, qs],
                            rhs=self.KT2[p * 64:(p + 1) * 64, hk, nb * 128:nb * 128 + 256], start=True, stop=True),
                            r=(tQ, self.tKV), w=(self.tPSB[j % 2],))
                    for bk in range(2):
                        j0 = 2 * bk
                        pbank = self.PSS[:, bk * 512:(bk + 1) * 512].rearrange("p (h k) -> p h k", h=2)
                        S.op("dve", lambda e, pbank=pbank, j0=j0: e.scalar_tensor_tensor(out=SS2, in0=pbank, scalar=0.125, in1=biasm[:, j0:j0 + 2, :],
                                                                                         op0=ALU.mult, op1=ALU.add), r=(tb, self.tPSB[bk]), w=(tSS,))
                        if nb == 0:
                            for jj in range(2):
                                S.op("dve", lambda e, jj=jj: e.tensor_tensor(out=SS2[:, jj, 0:128], in0=SS2[:, jj, 0:128], in1=self.c_halo[:], op=ALU.add),
                                     r=(self.tC, tSS), w=(tSS,))
                        S.op("dve", lambda e, j0=j0: e.tensor_reduce(out=rm[:, j0:j0 + 2], in_=SS2, axis=AX.X, op=ALU.max), r=(tSS,), w=(tsm,))
                        S.op("dve", lambda e, j0=j0: e.tensor_tensor(out=rm[:, j0:j0 + 2], in0=rm[:, j0:j0 + 2], in1=sink[:, j0:j0 + 2], op=ALU.max), r=(tsm,), w=(tsm,))
                        S.op("dve", lambda e, j0=j0: e.tensor_scalar(out=negm[:, j0:j0 + 2], in0=rm[:, j0:j0 + 2], scalar1=-1.0, scalar2=None, op0=ALU.mult), r=(tsm,), w=(tsm,))
                        for jj in range(2):
                            j = j0 + jj
                            S.op("act", lambda e, j=j, jj=jj: e.activation(out=PB[:, j, :], in_=SS2[:, jj, :], func=AF.Exp,
                                                                            bias=negm[:, j:j + 1], scale=1.0, accum_out=rs[:, j:j + 1]),
                                 r=(tSS, tsm), w=(tPB, tsm))
                    S.op("dve", lambda e: e.tensor_tensor(out=es, in0=sink, in1=negm, op=ALU.add), r=(tsm,), w=(tsm,))
                    S.op("act", lambda e: e.activation(out=es, in_=es, func=AF.Exp), r=(tsm,), w=(tsm,))
                    S.op("dve", lambda e: e.tensor_tensor(out=rinv, in0=rs, in1=es, op=ALU.add), r=(tsm,), w=(tsm,))
                    S.op("dve", lambda e: e.reciprocal(out=rinv, in_=rinv), r=(tsm,), w=(tsm,))
                    pt, tpt = self.PST[0], self.tPST[0]
                    for j in range(4):
                        for kb in range(2):
                            a = j * 2 + kb
                            S.op("pe", lambda e, j=j, kb=kb, a=a: e.transpose(pt[:, a * 128:(a + 1) * 128], PB[:, j, kb * 128:(kb + 1) * 128], self.c_identb[:]),
                                 r=(tPB, self.tC), w=(tpt,))
                    S.op("act", lambda e: e.copy(out=PTs, in_=pt[:].rearrange("p (a q) -> p a q", a=8)), r=(tpt,), w=(tPTs,))
                    po, tpo = self.PSBv[2], self.tPSB[2]
                    for j in range(4):
                        for kb in range(2):
                            S.op("pe", lambda e, j=j, kb=kb, nb=nb, hk=hk: e.matmul(po[:, j * 64:(j + 1) * 64], lhsT=PTs[:, pos(j) * 2 + kb, :],
                                                                                    rhs=self.V[:, nb + kb, hk * 64:(hk + 1) * 64],
                                                                                    start=(kb == 0), stop=(kb == 1)),
                                 r=(tPTs, self.tKV), w=(tpo,))
                    for j in range(4):
                        S.op("dve", lambda e, j=j: e.tensor_scalar(out=OB[:, j * 64:(j + 1) * 64], in0=po[:, j * 64:(j + 1) * 64], scalar1=rinv[:, pos(j):pos(j) + 1],
                                                                   scalar2=None, op0=ALU.mult), r=(tpo, tsm), w=(tOB,))
                    pt1, tpt1 = self.PST[1], self.tPST[1]
                    for cc in range(2):
                        S.op("pe", lambda e, cc=cc: e.transpose(pt1[:, cc * 128:(cc + 1) * 128], OB[:, cc * 128:(cc + 1) * 128], self.c_identb[:]),
                             r=(tOB, self.tC), w=(tpt1,))
                    S.op("dve", lambda e, c0=c0, qs=qs: e.tensor_copy(out=self.OT[:, c0:c0 + 2, qs], in_=pt1[:, 0:256].rearrange("p (c q) -> p c q", c=2)),
                         r=(tpt1,), w=(self.tOT[nb],))
        self.barrier()

    def _program(self):
        S = self.S
        self.c_eps = {}
        for eps in (LN_EPS, RMS_EPS):
            t = self.sb(f"eps{len(self.c_eps)}", [128, 1])
            self.c_eps[eps] = t
            S.op("dve", lambda e, t=t, eps=eps: e.memset(t[:], eps), w=(self.tC,))
        self.load_consts()
        stop = self.debug_stop
        if stop is not None and stop != "hg1":
            self.load_x(self.din("xm"))
        if stop in ("xt", "xt0"):
            self.make_xt()
        if stop == "xt":
            self.layernorm(0)
            self.store_out()
            return
        if stop == "hg1":
            self.load_x(self.din("xp"))
            self.make_xt()
            self.hgrn(1, "zero")
            self.barrier()
            self.load_x(self.din("xm"))
            self.make_xt()
            self.hgrn(1, "load")
            self.barrier()
            self.out_proj(self.din("a_w_out")[1])
            self.layernorm(2)
            self.store_out()
            return
        if stop == "att":
            self.phase()
            self.kv_alloc()
            self.load_x(self.din("xp"))
            self.save_halo()
            self.barrier()
            self.load_x(self.din("xm"))
            self.make_xt()
            self.kv_compute()
            if os.environ.get("ATT_STOP") == "kv":
                self.store_out()
                return
            self.attention(0)
            if os.environ.get("ATT_STOP"):
                self.store_out()
                return
            self.out_proj(self.din("b_w_out")[0])
            self.layernorm(4)
            self.store_out()
            return
        if stop == "moe":
            self.moe(0)
            self.layernorm(1)
            self.store_out()
            return
        if stop == "xt0":
            self.barrier()
            self.store_out()
            return
        def hgrn_layer(l, mode):
            self.make_xt()
            self.hgrn(l, mode)
            self.barrier()
            self.out_proj(self.din("a_w_out")[l])
            self.layernorm(2 * l)
            self.moe(l)
            self.layernorm(2 * l + 1)

        self.load_x(self.din("xp"))
        hgrn_layer(0, "zero")
        hgrn_layer(1, "zero")
        self.save_halo()
        self.barrier()
        self.load_x(self.din("xm"))
        hgrn_layer(0, "load")
        hgrn_layer(1, "load")
        self.make_xt()
        self.phase()
        self.kv_alloc()
        self.kv_compute()
        for j in range(2):
            if j:
                self.make_xt()
            self.attention(j)
            self.out_proj(self.din("b_w_out")[j])
            self.layernorm(4 + 2 * j)
            self.moe(2 + j)
            self.layernorm(5 + 2 * j)
        self.store_out()

    def store_out(self):
        S = self.S
        for i in range(NT):
            S.dma("sp", self.y[i * 128:(i + 1) * 128, :], self.X[:, i, :], r=(self.tX[i],))
        S.eng["sp"].wait_ge(S.sem["sp_d"], S.cnt["sp_d"])


_CACHE = {}


def _prep_inputs(inputs):
    c = host_consts()
    bucket = c.pop("_bucket")
    rel_bias = np.asarray(inputs["rel_bias"], np.float32)
    bias_tab = np.ascontiguousarray(rel_bias[bucket].transpose(2, 0, 1))
    shared = {k: np.ascontiguousarray(np.asarray(inputs[k], np.float32)) for k in
              ["a_w_in", "a_lower_bound", "a_norm_g", "a_w_out", "b_w_kv", "b_w_q", "b_sinks", "b_w_out",
               "moe_w_rg", "moe_b_rg", "moe_w_re", "moe_b_re", "moe_w_gate", "moe_w_up", "moe_w_down", "ln_g", "ln_b"]}
    shared["bias_tab"] = bias_tab
    shared.update(c)
    x = np.asarray(inputs["x"], np.float32)
    in_maps = []
    for core in range(8):
        b, half = core // 2, core % 2
        m = dict(shared)
        m["xp"] = np.ascontiguousarray(x[b, 0:SEG])
        m["xm"] = np.ascontiguousarray(x[b, half * SEG:(half + 1) * SEG])
        m["flag"] = np.full((128, 1), float(half), np.float32)
        m["halo_mask"] = np.full((128, 128), 0.0 if half else NEG, np.float32)
        in_maps.append(m)
    return in_maps


def kernel(**inputs):
    if "nc" not in _CACHE:
        p = Prog()
        _CACHE["nc"] = p.build()
        _CACHE["used"] = set(p.in_aps.keys())
    nc = _CACHE["nc"]
    in_maps = [{k: v for k, v in m.items() if k in _CACHE["used"]} for m in _prep_inputs(inputs)]
    res = run_bass_kernel_spmd(nc, in_maps, core_ids=list(range(8)))
    out = np.zeros((4, 2 * SEG, D), np.float32)
    for core in range(8):
        b, half = core // 2, core % 2
        out[b, half * SEG:(half + 1) * SEG] = res.results[core]["y"]
    return out
```

```python
import math
import os
HGL = int(os.environ.get('HGL', '99'))
HGH = int(os.environ.get('HGH', '16'))
HGV = int(os.environ.get('HGV', '3'))
from contextlib import ExitStack

import numpy as np
import concourse.bass as bass
import concourse.mybir as mybir
from concourse.bass_utils import run_bass_kernel_spmd

F32 = mybir.dt.float32
BF16 = mybir.dt.bfloat16
ALU = mybir.AluOpType
AF = mybir.ActivationFunctionType
AX = mybir.AxisListType

D = 2048
NT = 8
SEG = NT * 128
NKC = 16
CAP = 128
NE = 32
ALPHA = 8 ** 0.25
LN_EPS = 1e-5
RMS_EPS = 1e-6
NEG = -1e30
SAME_ENGINE_SYNC = True


class T:
    __slots__ = ("name", "w", "r", "excl")

    def __init__(self, name, excl=False):
        self.name = name
        self.w = None
        self.r = {}
        self.excl = excl


class Sched:
    def __init__(self, nc, es):
        self.nc = nc
        self.eng = {"pe": nc.tensor, "act": nc.scalar, "dve": nc.vector, "pool": nc.gpsimd, "sp": nc.sync}
        self.sem = {}
        self.cnt = {}
        for k in ["pe", "act", "dve", "pool", "sp", "pool_d", "sp_d"]:
            self.sem[k] = es.enter_context(nc.semaphore("sem_" + k))
            self.cnt[k] = 0
        self.waited = {k: {} for k in self.eng}
        self.n_ins = 0
        self.rec = None

    def _deps(self, e, r, w):
        deps = {}
        for t in r:
            if t.w is not None:
                k, v = t.w
                deps[k] = max(deps.get(k, 0), v)
        for t in w:
            if t.w is not None:
                k, v = t.w
                deps[k] = max(deps.get(k, 0), v)
            for k, v in t.r.items():
                deps[k] = max(deps.get(k, 0), v)
        for k, v in deps.items():
            if k == e and (e == "pe" or not SAME_ENGINE_SYNC):
                continue
            if self.waited[e].get(k, 0) < v:
                self.eng[e].wait_ge(self.sem[k], v)
                self.waited[e][k] = v

    def op(self, e, fn, r=(), w=()):
        if self.rec is not None:
            self.rec.append(("op", e, fn, tuple(r), tuple(w)))
            return
        xr = tuple(t for t in r if t.excl)
        if xr:
            r = tuple(t for t in r if not t.excl)
            w = tuple(w) + xr
        self._deps(e, r, w)
        ins = fn(self.eng[e])
        ins.then_inc(self.sem[e], 1)
        self.cnt[e] += 1
        self.n_ins += 1
        v = self.cnt[e]
        for t in r:
            t.r[e] = v
        for t in w:
            t.w = (e, v)
            t.r = {}

    def emit(self, ent):
        if ent[0] == "op":
            self.op(ent[1], ent[2], r=ent[3], w=ent[4])
        else:
            self.dma(ent[1], ent[2], ent[3], r=ent[4], w=ent[5])

    def dma(self, q, out, in_, r=(), w=()):
        if self.rec is not None:
            self.rec.append(("dma", q, out, in_, tuple(r), tuple(w)))
            return
        self._deps(q, r, w)
        dk = q + "_d"
        self.eng[q].dma_start(out=out, in_=in_).then_inc(self.sem[dk], 16)
        self.cnt[dk] += 16
        self.n_ins += 1
        v = self.cnt[dk]
        for t in r:
            t.r[dk] = v
        for t in w:
            t.w = (dk, v)
            t.r = {}

    def wait_all(self, e, tiles):
        self._deps(e, tiles, tiles)


def _t5_bucket_np(dist):
    n = np.clip(dist, 0, 127)
    max_exact = 16
    large = max_exact + (np.log(np.maximum(n, max_exact).astype(np.float32) / np.float32(max_exact))
                         / np.float32(math.log(128 / max_exact)) * np.float32(32 - max_exact)).astype(np.int32)
    large = np.minimum(large, 31)
    return np.where(n < max_exact, n, large)


def host_consts():
    c = {}
    t = np.arange(128)
    ch = t // 64
    same = ch[:, None] == ch[None, :]
    mid = ch * 64 + 31
    L1 = same * ((t[:, None] <= t[None, :]).astype(np.float32) - (t[:, None] <= mid[None, :]).astype(np.float32))
    L2 = same * (t[:, None] > t[None, :]).astype(np.float32)
    sel = np.zeros((128, 4), np.float32)
    sel[:, 0] = t <= 31
    sel[:, 1] = t < 64
    sel[:, 2] = (t >= 64) & (t <= 95)
    sel[:, 3] = t >= 64
    maskT = (same & (t[:, None] <= t[None, :])).astype(np.float32)
    ustrict = (t[:, None] < t[None, :]).astype(np.float32)
    c["c_ident"] = np.eye(128, dtype=np.float32)
    c["c_L1"] = L1.astype(np.float32)
    c["c_L2"] = L2.astype(np.float32)
    c["c_sel"] = sel
    c["c_maskT"] = maskT
    c["c_ustrict"] = ustrict
    c["c_ones"] = np.ones((128, 128), np.float32)
    c["c_iota"] = np.tile(np.arange(CAP, dtype=np.float32)[None, :], (128, 1))
    qi = np.arange(128)[:, None]
    kj = np.arange(256)[None, :]
    dist = qi + 128 - kj
    inwin = (dist >= 0) & (dist < 128)
    c["c_maskc"] = np.where(inwin, 0.0, NEG).astype(np.float32)
    c["_bucket"] = _t5_bucket_np(dist)
    return c


class Prog:
    def __init__(self, debug_stop=None):
        self.debug_stop = debug_stop
        self.nc = bass.Bass("TRN2", target_bir_lowering=False)
        self.es = ExitStack()
        self.in_shapes = {}
        self.in_aps = {}

    def dram_in(self, name, shape):
        self.in_shapes[name] = list(shape)
        return None

    def din(self, name):
        if name not in self.in_aps:
            self.in_aps[name] = self.nc.dram_tensor(name, self.in_shapes[name], F32, kind="ExternalInput").ap()
        return self.in_aps[name]

    def sb(self, name, shape, dt=F32):
        return self.es.enter_context(self.nc.sbuf_tensor("s_" + name, list(shape), dt))

    def build(self):
        nc = self.nc
        es = self.es
        with es:
            self.S = Sched(nc, es)
            self._declare()
            self._program()
        return nc

    def _declare(self):
        nc = self.nc
        di = self.dram_in
        di("xp", [SEG, D]); di("xm", [SEG, D])
        di("a_w_in", [2, D, 4 * D]); di("a_lower_bound", [2, D]); di("a_norm_g", [2, D]); di("a_w_out", [2, D, D])
        di("b_w_kv", [D, 512]); di("b_w_q", [2, D, D]); di("b_sinks", [2, 32]); di("b_w_out", [2, D, D])
        di("bias_tab", [32, 128, 256])
        di("moe_w_rg", [4, D, 4]); di("moe_b_rg", [4, 4]); di("moe_w_re", [4, D, 32]); di("moe_b_re", [4, 32])
        di("moe_w_gate", [4, NE, D, 512]); di("moe_w_up", [4, NE, D, 512]); di("moe_w_down", [4, NE, 512, D])
        di("ln_g", [8, D]); di("ln_b", [8, D]); di("flag", [128, 1]); di("halo_mask", [128, 128])
        for k, shp in [("c_ident", [128, 128]), ("c_L1", [128, 128]), ("c_L2", [128, 128]), ("c_sel", [128, 4]),
                       ("c_maskT", [128, 128]), ("c_ustrict", [128, 128]), ("c_ones", [128, 128]),
                       ("c_iota", [128, CAP]), ("c_maskc", [128, 256])]:
            di(k, shp)
        self.y = nc.dram_tensor("y", [SEG, D], F32, kind="ExternalOutput").ap()
        self.d_state = [nc.dram_tensor(f"d_state{l}", [128, 16 * 128], F32, kind="Internal").ap() for l in range(2)]
        self.d_halo = nc.dram_tensor("d_halo", [128, D], F32, kind="Internal").ap()

        sb = self.sb
        self.X = sb("X", [128, NT, D])
        self.XTraw = sb("XTraw", [128, 8192])
        self.XT = self.XTraw[:].bitcast(BF16).rearrange("p (k t) -> p k t", k=NKC)
        self.OT = sb("OT", [128, NKC, SEG], BF16)
        self.tX = [T(f"X{i}") for i in range(NT)]
        self.tXT = [T(f"XT{i}") for i in range(NT)]
        self.tOT = [T(f"OT{i}") for i in range(NT)]
        self.NB = 3
        self.WB = [sb(f"WB{i}", [128, NKC, 512], BF16) for i in range(self.NB)]
        self.tWB = [T(f"WB{i}") for i in range(self.NB)]
        self.wb_next = 0
        self.c_ident = sb("c_ident", [128, 128])
        self.c_identb = sb("c_identb", [128, 128], BF16)
        self.c_maskc = sb("c_maskc", [128, 256])
        self.c_flag = sb("c_flag", [128, 1])
        self.c_halo = sb("c_halo", [128, 128])
        self.tC = T("consts")
        self.lnst = sb("lnst", [128, 8])
        self.tlnst = T("lnst")
        self.ARN = 7300
        self.AR = sb("arena", [128, self.ARN])
        self.ar_off = 0
        self.PSS = self.es.enter_context(nc.psum_tensor("pss", [128, 1024], F32))
        self.PSG = [self.es.enter_context(nc.psum_tensor(f"psg{i}", [128, 512], F32)) for i in range(2)]
        self.PSBv = [self.PSS[:, 0:512], self.PSS[:, 512:1024], self.PSG[0][:], self.PSG[1][:]]
        self.PSF = [self.es.enter_context(nc.psum_tensor(f"psf{i}", [128, 512], F32)) for i in range(2)]
        self.PST = [self.es.enter_context(nc.psum_tensor(f"pst{i}", [128, 1024], BF16)) for i in range(2)]
        self.tPSB = [T(f"psb{i}", True) for i in range(4)]
        self.tPSF = [T(f"psf{i}", True) for i in range(2)]
        self.tPST = [T(f"pst{i}", True) for i in range(2)]

    def barrier(self):
        S = self.S
        for e in S.eng:
            for k in S.sem:
                if k == e:
                    continue
                if S.cnt[k] > S.waited[e].get(k, 0):
                    S.eng[e].wait_ge(S.sem[k], S.cnt[k])
                    S.waited[e][k] = S.cnt[k]

    def phase(self):
        self.barrier()
        self.ar_off = 0

    def arf(self, cols, shape=None):
        o = self.ar_off
        self.ar_off += cols
        assert self.ar_off <= self.ARN, f"arena overflow {self.ar_off}"
        v = self.AR[:, o:o + cols]
        return v

    def arb(self, cols):
        assert cols % 2 == 0
        return self.arf(cols // 2).bitcast(BF16)

    def wload(self, pieces):
        i = self.wb_next
        self.wb_next = (i + 1) % getattr(self, "NB_active", self.NB)
        buf, t = self.WB[i], self.tWB[i]
        for dst_fn, src in pieces:
            self.S.dma("pool", dst_fn(buf), src, r=(), w=(t,))
        return buf, t

    def load_consts(self):
        S = self.S
        for k in ["c_ident", "c_maskc"]:
            S.dma("sp", getattr(self, k)[:], self.din(k)[:, :], w=(self.tC,))
        S.dma("sp", self.c_flag[:], self.din("flag")[:, :], w=(self.tC,))
        S.dma("sp", self.c_halo[:], self.din("halo_mask")[:, :], w=(self.tC,))
        S.op("dve", lambda e: e.tensor_copy(out=self.c_identb[:], in_=self.c_ident[:]), r=(self.tC,), w=(self.tC,))

    def load_x(self, src):
        for i in range(NT):
            self.S.dma("sp", self.X[:, i, :], src[i * 128:(i + 1) * 128, :], w=(self.tX[i],))

    def make_xt(self):
        S = self.S
        n = 0
        for i in range(NT):
            for g in range(4):
                pb = n % 2
                n += 1
                ps, tps = self.PSF[pb], self.tPSF[pb]
                for j in range(4):
                    kc = g * 4 + j
                    S.op("pe", lambda e, kc=kc, j=j, ps=ps, i=i: e.transpose(
                        ps[:, j * 128:(j + 1) * 128], self.X[:, i, kc * 128:(kc + 1) * 128], self.c_ident[:]),
                        r=(self.tX[i], self.tC), w=(tps,))
                dst = self.XT[:, g * 4:(g + 1) * 4, i * 128:(i + 1) * 128]
                src = ps[:].rearrange("p (j t) -> p j t", j=4)
                if n % 2:
                    S.op("act", lambda e, dst=dst, src=src: e.copy(out=dst, in_=src), r=(tps,), w=(self.tXT[i],))
                else:
                    S.op("dve", lambda e, dst=dst, src=src: e.tensor_copy(out=dst, in_=src), r=(tps,), w=(self.tXT[i],))

    def rsqrt_small(self, out, in_, scale, eps, rt, wt):
        S = self.S
        S.op("act", lambda e: e.activation(out=out, in_=in_, func=AF.Ln, bias=self.epsb(eps), scale=scale), r=rt, w=wt)
        S.op("act", lambda e: e.activation(out=out, in_=out, func=AF.Exp, scale=-0.5), r=wt, w=wt)

    def epsb(self, eps):
        return self.c_eps[eps][:]

    def layernorm(self, idx):
        S = self.S
        self.barrier()
        vg = self.XTraw[:, 0:D]
        vb = self.XTraw[:, D:2 * D]
        tv = T("lnvec")
        S.dma("sp", vg, self.din("ln_g")[idx:idx + 1, :].partition_broadcast(128), w=(tv,))
        S.dma("sp", vb, self.din("ln_b")[idx:idx + 1, :].partition_broadcast(128), w=(tv,))
        st, tst = self.lnst, self.tlnst
        junk = self.OT[:, 0:2, :]
        tj = T("lnjunk")
        for i in range(NT):
            xi = self.X[:, i, :]
            xi3 = xi.rearrange("p (a b) -> p a b", a=2)
            S.op("act", lambda e, xi3=xi3: e.activation(out=junk, in_=xi3, func=AF.Copy, accum_out=st[:, 0:1]),
                 r=(self.tX[i],), w=(tst, tj))
            S.op("act", lambda e, xi3=xi3: e.activation(out=junk, in_=xi3, func=AF.Square, accum_out=st[:, 1:2]),
                 r=(self.tX[i],), w=(tst, tj))
            S.op("dve", lambda e: e.tensor_scalar(out=st[:, 2:4], in0=st[:, 0:2], scalar1=1.0 / D, scalar2=None, op0=ALU.mult),
                 r=(tst,), w=(tst,))
            S.op("dve", lambda e: e.tensor_tensor(out=st[:, 4:5], in0=st[:, 2:3], in1=st[:, 2:3], op=ALU.mult), r=(tst,), w=(tst,))
            S.op("dve", lambda e: e.tensor_tensor(out=st[:, 5:6], in0=st[:, 3:4], in1=st[:, 4:5], op=ALU.subtract), r=(tst,), w=(tst,))
            self.rsqrt_small(st[:, 6:7], st[:, 5:6], 1.0, LN_EPS, (tst,), (tst,))
            S.op("dve", lambda e: e.scalar_tensor_tensor(out=st[:, 7:8], in0=st[:, 2:3], scalar=-1.0, in1=st[:, 6:7], op0=ALU.mult, op1=ALU.mult),
                 r=(tst,), w=(tst,))
            S.op("act", lambda e, xi=xi: e.activation(out=xi, in_=xi, func=AF.Identity, bias=st[:, 7:8], scale=st[:, 6:7]),
                 r=(tst, self.tX[i]), w=(self.tX[i],))
            S.op("dve", lambda e, xi=xi: e.tensor_tensor(out=xi, in0=xi, in1=vg, op=ALU.mult), r=(tv, self.tX[i]), w=(self.tX[i],))
            S.op("dve", lambda e, xi=xi: e.tensor_tensor(out=xi, in0=xi, in1=vb, op=ALU.add), r=(tv, self.tX[i]), w=(self.tX[i],))
        self.barrier()

    def out_proj(self, w_dram):
        S = self.S
        n = 0
        for nch in range(4):
            buf, tb = self.wload([(lambda b: b[:], w_dram[:, nch * 512:(nch + 1) * 512].rearrange("(kc p) n -> p kc n", p=128))])
            for i in range(NT):
                pb = n % 2
                n += 1
                ps, tps = self.PSF[pb], self.tPSF[pb]
                for kc in range(NKC):
                    S.op("pe", lambda e, kc=kc, ps=ps, i=i, buf=buf: e.matmul(
                        ps[:], lhsT=self.OT[:, kc, i * 128:(i + 1) * 128], rhs=buf[:, kc, :], start=(kc == 0), stop=(kc == NKC - 1)),
                        r=(self.tOT[i], tb), w=(tps,))
                xs = self.X[:, i, nch * 512:(nch + 1) * 512]
                S.op("dve", lambda e, xs=xs, ps=ps: e.scalar_tensor_tensor(out=xs, in0=xs, scalar=ALPHA, in1=ps[:], op0=ALU.mult, op1=ALU.add),
                     r=(tps, self.tX[i]), w=(self.tX[i],))

    def hgrn(self, l, init_mode):
        S = self.S
        self.phase()
        f = self.arf
        St = f(2048).rearrange("p (h v) -> p h v", h=16)
        tSt = [T(f"St{h}") for h in range(16)]
        cL1, cL2, cMT, csel = f(128), f(128), f(128), f(4)
        tc = T("hc")
        for dst, k in [(cL1, "c_L1"), (cL2, "c_L2"), (cMT, "c_maskT"), (csel, "c_sel")]:
            S.dma("sp", dst, self.din(k)[:, :], w=(tc,))
        if init_mode == "zero":
            S.op("dve", lambda e: e.memset(St, 0.0), w=tuple(tSt))
        else:
            S.dma("sp", St, self.d_state[l].rearrange("p (h v) -> p h v", h=16), w=tuple(tSt))
            S.op("dve", lambda e: e.tensor_scalar(out=St, in0=St, scalar1=self.c_flag[:, 0:1], scalar2=None, op0=ALU.mult),
                 r=tuple(tSt) + (self.tC,), w=tuple(tSt))
        a0, a1, lb, oml, ng = f(128), f(128), f(128), f(128), f(128)
        tv = T("hvec")
        wb2 = self.WB[2][:].rearrange("p k n -> p (k n)").bitcast(F32)
        wb2_off = [0]

        def f2(cols):
            o = wb2_off[0]
            wb2_off[0] += cols
            assert wb2_off[0] <= 4096
            return wb2[:, o:o + cols]

        def mk_set(ff, sfx):
            fb_ = lambda cols: ff(cols // 2).bitcast(BF16)
            ws = {}
            for k in ["e1", "t1", "fg", "kk", "logf", "eq", "ek", "ekl", "sg", "gg"]:
                ws[k] = ff(128)
            ws["ebT"] = ff(4)
            ws["ss"] = ff(4)
            for k in ["qt", "kt", "kh", "vv", "QF", "KTs", "sTm", "Sp0", "Sp1", "on", "junk"]:
                ws[k] = fb_(128)
            ws["QZ"] = fb_(256).rearrange("p (a b) -> p a b", a=2)
            ws["tw"] = {k: T("hw_" + k + sfx) for k in ["e1", "t1", "fg", "kk", "logf", "eq", "ek", "ekl", "ebT", "sg", "gg", "ss", "qt", "kt", "kh", "vv",
                                                      "QF", "KTs", "sTm", "Sp0", "Sp1", "on", "QZ", "junk"]}
            S.op("dve", lambda e, ws=ws: e.memset(ws["QZ"], 0.0), w=(ws["tw"]["QZ"],))
            return ws

        wsets = [mk_set(f, "a"), mk_set(f2, "b")]
        self.NB_active = 2
        self.wb_next = 0
        w_in = self.din("a_w_in")
        alb = self.din("a_lower_bound")
        ang = self.din("a_norm_g")
        for h in range(HGH):
            hs = slice(h * 128, (h + 1) * 128)
            buf, tb = self.wload([(lambda b, p=p: b[:, :, p * 128:(p + 1) * 128],
                                   w_in[l, :, p * D + h * 128:p * D + (h + 1) * 128].rearrange("(kc p) n -> p kc n", p=128))
                                  for p in range(4)])
            S.dma("sp", ng, ang[l:l + 1, hs].partition_broadcast(128), w=(tv,))
            if l == 0:
                S.op("dve", lambda e: e.memset(lb, 0.0), w=(tv,))
            else:
                S.dma("sp", a0, alb[0:1, hs].partition_broadcast(128), w=(tv,))
                S.dma("sp", a1, alb[1:2, hs].partition_broadcast(128), w=(tv,))
                S.op("dve", lambda e: e.tensor_tensor(out=lb, in0=a0, in1=a1, op=ALU.subtract), r=(tv,), w=(tv,))
                S.op("act", lambda e: e.activation(out=lb, in_=lb, func=AF.Exp), r=(tv,), w=(tv,))
                S.op("dve", lambda e: e.tensor_scalar(out=lb, in0=lb, scalar1=1.0, scalar2=None, op0=ALU.add), r=(tv,), w=(tv,))
                S.op("dve", lambda e: e.reciprocal(out=lb, in_=lb), r=(tv,), w=(tv,))
            S.op("dve", lambda e: e.tensor_scalar(out=oml, in0=lb, scalar1=-1.0, scalar2=1.0, op0=ALU.mult, op1=ALU.add), r=(tv,), w=(tv,))
            def tile_body(i, ws, h=h, buf=buf, tb=tb):
                e1, t1, fg, kk, logf, eq, ek, ekl, ebT, sg, gg, ss = (ws[k] for k in ['e1', 't1', 'fg', 'kk', 'logf', 'eq', 'ek', 'ekl', 'ebT', 'sg', 'gg', 'ss'])
                qt, kt, kh, vv, QF, KTs, sTm, Sp0, Sp1, on, junk, QZ = (ws[k] for k in ['qt', 'kt', 'kh', 'vv', 'QF', 'KTs', 'sTm', 'Sp0', 'Sp1', 'on', 'junk', 'QZ'])
                tw = ws['tw']
                ts = slice(i * 128, (i + 1) * 128)
                pp, tpp = self.PSF[i % 2], self.tPSF[i % 2]
                for kc in range(NKC):
                    S.op("pe", lambda e, kc=kc, pp=pp, ts=ts, buf=buf: e.matmul(pp[:], lhsT=self.XT[:, kc, ts], rhs=buf[:, kc, :],
                                                                                 start=(kc == 0), stop=(kc == NKC - 1)),
                         r=(self.tXT[i], tb), w=(tpp,))
                if HGL < 1:
                    return
                pq, pf, pi_, pg = pp[:, 0:128], pp[:, 128:256], pp[:, 256:384], pp[:, 384:512]
                S.op("act", lambda e: e.activation(out=e1, in_=pf, func=AF.Exp, scale=-1.0), r=(tpp,), w=(tw["e1"],))
                S.op("dve", lambda e: e.tensor_scalar(out=e1, in0=e1, scalar1=1.0, scalar2=None, op0=ALU.add), r=(tw["e1"],), w=(tw["e1"],))
                S.op("dve", lambda e: e.reciprocal(out=e1, in_=e1), r=(tw["e1"],), w=(tw["e1"],))
                S.op("dve", lambda e: e.tensor_tensor(out=t1, in0=e1, in1=oml, op=ALU.mult), r=(tw["e1"], tv), w=(tw["t1"],))
                S.op("dve", lambda e: e.tensor_tensor(out=fg, in0=t1, in1=lb, op=ALU.add), r=(tw["t1"], tv), w=(tw["fg"],))
                S.op("dve", lambda e: e.tensor_scalar(out=fg, in0=fg, scalar1=1e-30, scalar2=None, op0=ALU.max), r=(tw["fg"],), w=(tw["fg"],))
                S.op("act", lambda e: e.activation(out=logf, in_=fg, func=AF.Ln), r=(tw["fg"],), w=(tw["logf"],))
                S.op("dve", lambda e: e.tensor_tensor(out=kk, in0=oml, in1=t1, op=ALU.subtract), r=(tw["t1"], tv), w=(tw["kk"],))
                if HGL < 2:
                    return
                pc, tpc = self.PSBv[0], self.tPSB[0]
                S.op("pe", lambda e: e.matmul(pc[:, 0:128], lhsT=cL1, rhs=logf, start=True, stop=True), r=(tc, tw["logf"]), w=(tpc,))
                if HGV >= 2:
                    S.op("pe", lambda e: e.matmul(pc[:, 128:256], lhsT=cL2, rhs=logf, start=True, stop=True), r=(tc, tw["logf"]), w=(tpc,))
                if HGV >= 3:
                    S.op("pe", lambda e: e.matmul(pc[:, 256:260], lhsT=logf, rhs=csel, start=True, stop=True), r=(tc, tw["logf"]), w=(tpc,))
                if HGL < 3 or os.environ.get('HGX') == '1':
                    return
                S.op("act", lambda e: e.activation(out=eq, in_=pc[:, 0:128], func=AF.Exp), r=(tpc,), w=(tw["eq"],))
                S.op("act", lambda e: e.activation(out=ek, in_=pc[:, 0:128], func=AF.Exp, scale=-1.0), r=(tpc,), w=(tw["ek"],))
                S.op("act", lambda e: e.activation(out=ekl, in_=pc[:, 128:256], func=AF.Exp), r=(tpc,), w=(tw["ekl"],))
                S.op("act", lambda e: e.activation(out=ebT, in_=pc[:, 256:260], func=AF.Exp), r=(tpc,), w=(tw["ebT"],))
                if os.environ.get('HGX') == '2':
                    return
                S.op("dve", lambda e: e.tensor_tensor(out=qt, in0=pq, in1=eq, op=ALU.mult), r=(tpp, tw["eq"]), w=(tw["qt"],))
                S.op("dve", lambda e: e.tensor_tensor(out=kt, in0=kk, in1=ek, op=ALU.mult), r=(tw["kk"], tw["ek"]), w=(tw["kt"],))
                S.op("dve", lambda e: e.tensor_tensor(out=kh, in0=kk, in1=ekl, op=ALU.mult), r=(tw["kk"], tw["ekl"]), w=(tw["kh"],))
                if os.environ.get('HGX') == '3':
                    return
                S.op("dve", lambda e: e.tensor_copy(out=vv, in_=pi_), r=(tpp,), w=(tw["vv"],))
                if os.environ.get('HGX') == '4':
                    return
                S.op("act", lambda e: e.activation(out=sg, in_=pg, func=AF.Exp, scale=-1.0), r=(tpp,), w=(tw["sg"],))
                S.op("dve", lambda e: e.tensor_scalar(out=sg, in0=sg, scalar1=1.0, scalar2=None, op0=ALU.add), r=(tw["sg"],), w=(tw["sg"],))
                S.op("dve", lambda e: e.reciprocal(out=sg, in_=sg), r=(tw["sg"],), w=(tw["sg"],))
                S.op("dve", lambda e: e.tensor_tensor(out=gg, in0=pg, in1=sg, op=ALU.mult), r=(tpp, tw["sg"]), w=(tw["gg"],))
                S.op("dve", lambda e: e.tensor_tensor(out=gg, in0=gg, in1=ng, op=ALU.mult), r=(tw["gg"], tv), w=(tw["gg"],))
                if HGL < 4:
                    return
                pt, tpt = self.PST[0], self.tPST[0]
                S.op("pe", lambda e: e.transpose(pt[:, 0:128], qt, self.c_identb[:]), r=(tw["qt"], self.tC), w=(tpt,))
                S.op("pe", lambda e: e.transpose(pt[:, 128:256], kt, self.c_identb[:]), r=(tw["kt"], self.tC), w=(tpt,))
                S.op("dve", lambda e: e.tensor_copy(out=QF, in_=pt[:, 0:128]), r=(tpt,), w=(tw["QF"],))
                S.op("dve", lambda e: e.tensor_copy(out=QZ[:, 0, 0:64], in_=pt[:, 0:64]), r=(tpt,), w=(tw["QZ"],))
                S.op("dve", lambda e: e.tensor_copy(out=QZ[:, 1, 64:128], in_=pt[:, 64:128]), r=(tpt,), w=(tw["QZ"],))
                S.op("dve", lambda e: e.tensor_copy(out=KTs, in_=pt[:, 128:256]), r=(tpt,), w=(tw["KTs"],))
                if HGL < 5:
                    return
                psc, tpsc = self.PSBv[1], self.tPSB[1]
                S.op("pe", lambda e: e.matmul(psc[:, 0:128], lhsT=KTs, rhs=QF, start=True, stop=True), r=(tw["KTs"], tw["QF"]), w=(tpsc,))
                S.op("dve", lambda e: e.tensor_tensor(out=sTm, in0=psc[:, 0:128], in1=cMT, op=ALU.mult), r=(tpsc, tc), w=(tw["sTm"],))
                if HGL < 6:
                    return
                pds, tpds = self.PSBv[2], self.tPSB[2]
                Sh = St[:, h, :]
                S.op("dve", lambda e: e.tensor_scalar(out=Sp0, in0=Sh, scalar1=ebT[:, 0:1], scalar2=None, op0=ALU.mult),
                     r=(tSt[h], tw["ebT"]), w=(tw["Sp0"],))
                S.op("pe", lambda e: e.matmul(pds[:, 0:128], lhsT=kh[0:64, :], rhs=vv[0:64, :], start=True, stop=True),
                     r=(tw["kh"], tw["vv"]), w=(tpds,))
                S.op("dve", lambda e: e.scalar_tensor_tensor(out=Sh, in0=Sh, scalar=ebT[:, 1:2], in1=pds[:, 0:128], op0=ALU.mult, op1=ALU.add),
                     r=(tSt[h], tw["ebT"], tpds), w=(tSt[h],))
                S.op("dve", lambda e: e.tensor_scalar(out=Sp1, in0=Sh, scalar1=ebT[:, 2:3], scalar2=None, op0=ALU.mult),
                     r=(tSt[h], tw["ebT"]), w=(tw["Sp1"],))
                S.op("pe", lambda e: e.matmul(pds[:, 128:256], lhsT=kh[64:128, :], rhs=vv[64:128, :], start=True, stop=True),
                     r=(tw["kh"], tw["vv"]), w=(tpds,))
                S.op("dve", lambda e: e.scalar_tensor_tensor(out=Sh, in0=Sh, scalar=ebT[:, 3:4], in1=pds[:, 128:256], op0=ALU.mult, op1=ALU.add),
                     r=(tSt[h], tw["ebT"], tpds), w=(tSt[h],))
                if HGL < 7:
                    return
                po, tpo = self.PSBv[3], self.tPSB[3]
                S.op("pe", lambda e: e.matmul(po[:, 0:128], lhsT=sTm, rhs=vv, start=True, stop=False), r=(tw["sTm"], tw["vv"]), w=(tpo,))
                S.op("pe", lambda e: e.matmul(po[:, 0:128], lhsT=QZ[:, 0, :], rhs=Sp0, start=False, stop=False), r=(tw["QZ"], tw["Sp0"]), w=(tpo,))
                S.op("pe", lambda e: e.matmul(po[:, 0:128], lhsT=QZ[:, 1, :], rhs=Sp1, start=False, stop=True), r=(tw["QZ"], tw["Sp1"]), w=(tpo,))
                if HGL < 8:
                    return
                S.op("act", lambda e: e.activation(out=junk, in_=po[:, 0:128], func=AF.Square, accum_out=ss[:, 0:1]), r=(tpo,), w=(tw["ss"], tw["junk"]))
                self.rsqrt_small(ss[:, 1:2], ss[:, 0:1], 1.0 / 128, RMS_EPS, (tw["ss"],), (tw["ss"],))
                S.op("dve", lambda e: e.scalar_tensor_tensor(out=on, in0=po[:, 0:128], scalar=ss[:, 1:2], in1=gg, op0=ALU.mult, op1=ALU.mult),
                     r=(tpo, tw["ss"], tw["gg"]), w=(tw["on"],))
                if HGL < 9:
                    return
                pt1, tpt1 = self.PST[1], self.tPST[1]
                S.op("pe", lambda e: e.transpose(pt1[:, 0:128], on, self.c_identb[:]), r=(tw["on"], self.tC), w=(tpt1,))
                S.op("dve", lambda e, h=h, ts=ts: e.tensor_copy(out=self.OT[:, h, ts], in_=pt1[:, 0:128]), r=(tpt1,), w=(self.tOT[i],))

            LAG = 30
            recs = []
            for i in range(NT):
                rec = []
                real_op = S.op
                S.op = lambda e, fn, r=(), w=(), rec=rec: rec.append((e, fn, tuple(r), tuple(w)))
                try:
                    tile_body(i, wsets[i % 2])
                finally:
                    S.op = real_op
                recs.append(rec)
            active = []
            nxt = 0
            while nxt < NT or active:
                if nxt < NT and len(active) < 2 and (not active or active[-1][1] >= min(LAG, len(active[-1][0]))):
                    active.append([recs[nxt], 0])
                    nxt += 1
                for a in list(active):
                    if a[1] < len(a[0]):
                        e_, fn_, r_, w_ = a[0][a[1]]
                        S.op(e_, fn_, r=r_, w=w_)
                        a[1] += 1
                active = [a for a in active if a[1] < len(a[0])]
        S.dma("sp", self.d_state[l].rearrange("p (h v) -> p h v", h=16), St, r=tuple(tSt))
        self.barrier()
        self.NB_active = 3
        self.wb_next = 0

    def moe(self, L):
        S = self.S
        self.barrier()
        X1b = self.OT[:].rearrange("p k t -> p (k t)").rearrange("p (i d) -> p i d", i=NT)
        tX1b = [T(f"x1b{i}") for i in range(NT)]
        off = [0]

        def f(cols):
            o = off[0]
            off[0] += cols
            assert off[0] <= 8192, off[0]
            return self.XTraw[:, o:o + cols]

        def fb(cols):
            return f(cols // 2).bitcast(BF16)

        wr = f(NKC * 36).rearrange("p (k n) -> p k n", k=NKC)
        rb = f(36)
        ustrict, ones, iota = f(128), f(128), f(CAP)
        xT4 = [f(512), f(512)]
        A = f(NT * 32).rearrange("p (i e) -> p i e", i=NT)
        Gt = f(NT * 32).rearrange("p (i e) -> p i e", i=NT)
        VAL = f(NT * 32).rearrange("p (i e) -> p i e", i=NT)
        lg, gsel, pen, elm, elm2, oh1, oh2 = f(36), f(4), f(4), f(32), f(32), f(32), f(32)
        sm = f(16)
        junk4 = f(4)
        aoff = [3456]

        def fb2(cols):
            o = aoff[0]
            aoff[0] += cols // 2
            assert aoff[0] <= self.ARN
            return self.AR[:, o:o + cols // 2].bitcast(BF16)

        sets = []
        for fbx, sfx in ((fb, "a"), (fb2, "b")):
            sets.append(dict(
                P=fbx(NT * CAP).rearrange("p (i c) -> p i c", i=NT),
                Pg=fbx(NT * CAP).rearrange("p (i c) -> p i c", i=NT),
                PgT=fbx(NT * 128).rearrange("p (i t) -> p i t", i=NT),
                XS=fbx(NKC * CAP).rearrange("p (k c) -> p k c", k=NKC),
                tP=[T("P" + sfx + str(i)) for i in range(NT)], tPg=[T("Pg" + sfx + str(i)) for i in range(NT)],
                tPgT=T("PgT" + sfx), tXS=[T("XS" + sfx + str(g)) for g in range(4)]))
        HT = fb(4 * CAP).rearrange("p (k c) -> p k c", k=4)
        hsg = f(4 * CAP)
        Y = fb(D)
        tc_ = T("mc")
        tr = T("mrt")
        tA = [T(f"mA{i}") for i in range(NT)]
        txT4 = [T("xT4a"), T("xT4b")]
        tHT, thsg = T("HT"), T("hsg")
        tY = [T(f"Y{k}") for k in range(4)]

        S.dma("sp", wr[:, :, 0:4], self.din("moe_w_rg")[L].rearrange("(k p) n -> p k n", p=128), w=(tc_,))
        S.dma("sp", wr[:, :, 4:36], self.din("moe_w_re")[L].rearrange("(k p) n -> p k n", p=128), w=(tc_,))
        S.dma("sp", rb[:, 0:4], self.din("moe_b_rg")[L:L + 1, :].partition_broadcast(128), w=(tc_,))
        S.dma("sp", rb[:, 4:36], self.din("moe_b_re")[L:L + 1, :].partition_broadcast(128), w=(tc_,))
        S.dma("sp", ustrict, self.din("c_ustrict")[:, :], w=(tc_,))
        S.dma("sp", ones, self.din("c_ones")[:, :], w=(tc_,))
        S.dma("sp", iota, self.din("c_iota")[:, :], w=(tc_,))

        for i in range(NT):
            if i % 2:
                S.op("act", lambda e, i=i: e.copy(out=X1b[:, i, :], in_=self.X[:, i, :]), r=(self.tX[i],), w=(tX1b[i],))
            else:
                S.op("dve", lambda e, i=i: e.tensor_copy(out=X1b[:, i, :], in_=self.X[:, i, :]), r=(self.tX[i],), w=(tX1b[i],))

        n = 0
        plg, tplg = self.PSBv[0], self.tPSB[0]
        for i in range(NT):
            for g in range(4):
                pb = n % 2
                n += 1
                ps, tps = self.PSF[pb], self.tPSF[pb]
                for j in range(4):
                    kc = g * 4 + j
                    S.op("pe", lambda e, kc=kc, j=j, ps=ps, i=i: e.transpose(
                        ps[:, j * 128:(j + 1) * 128], self.X[:, i, kc * 128:(kc + 1) * 128], self.c_ident[:]),
                        r=(self.tX[i], self.tC), w=(tps,))
                xt, txt = xT4[pb], txT4[pb]
                if pb:
                    S.op("act", lambda e, xt=xt, ps=ps: e.copy(out=xt, in_=ps[:]), r=(tps,), w=(txt,))
                else:
                    S.op("dve", lambda e, xt=xt, ps=ps: e.tensor_copy(out=xt, in_=ps[:]), r=(tps,), w=(txt,))
                for j in range(4):
                    kc = g * 4 + j
                    S.op("pe", lambda e, kc=kc, j=j, xt=xt: e.matmul(plg[:, 0:36], lhsT=xt[:, j * 128:(j + 1) * 128], rhs=wr[:, kc, :],
                                                                      start=(kc == 0), stop=(kc == NKC - 1)),
                         r=(txt, tc_), w=(tplg,))
            dv = lambda fn, r, w: S.op("dve", fn, r=r, w=w)
            dv(lambda e: e.tensor_tensor(out=lg, in0=plg[:, 0:36], in1=rb, op=ALU.add), (tplg, tc_), (tr,))
            gl, el = lg[:, 0:4], lg[:, 4:36]
            gmax, ngmax, sume, pgrp, m1, m2, dd, g1, g2 = (sm[:, k:k + 1] for k in range(9))
            dv(lambda e: e.tensor_reduce(out=gmax, in_=gl, axis=AX.X, op=ALU.max), (tr,), (tr,))
            dv(lambda e: e.tensor_scalar(out=gsel, in0=gl, scalar1=gmax, scalar2=None, op0=ALU.is_equal), (tr,), (tr,))
            dv(lambda e: e.tensor_scalar(out=ngmax, in0=gmax, scalar1=-1.0, scalar2=None, op0=ALU.mult), (tr,), (tr,))
            S.op("act", lambda e: e.activation(out=junk4, in_=gl, func=AF.Exp, bias=ngmax, scale=1.0, accum_out=sume), r=(tr,), w=(tr,))
            dv(lambda e: e.reciprocal(out=pgrp, in_=sume), (tr,), (tr,))
            dv(lambda e: e.tensor_scalar(out=pen, in0=gsel, scalar1=-1.0, scalar2=1e30, op0=ALU.add, op1=ALU.mult), (tr,), (tr,))
            for g in range(4):
                dv(lambda e, g=g: e.tensor_scalar(out=elm[:, g * 8:(g + 1) * 8], in0=el[:, g * 8:(g + 1) * 8], scalar1=pen[:, g:g + 1],
                                                  scalar2=None, op0=ALU.add), (tr,), (tr,))
            dv(lambda e: e.tensor_reduce(out=m1, in_=elm, axis=AX.X, op=ALU.max), (tr,), (tr,))
            dv(lambda e: e.tensor_scalar(out=oh1, in0=elm, scalar1=m1, scalar2=None, op0=ALU.is_equal), (tr,), (tr,))
            dv(lambda e: e.scalar_tensor_tensor(out=elm2, in0=oh1, scalar=-1e30, in1=elm, op0=ALU.mult, op1=ALU.add), (tr,), (tr,))
            dv(lambda e: e.tensor_reduce(out=m2, in_=elm2, axis=AX.X, op=ALU.max), (tr,), (tr,))
            dv(lambda e: e.tensor_scalar(out=oh2, in0=elm2, scalar1=m2, scalar2=None, op0=ALU.is_equal), (tr,), (tr,))
            dv(lambda e: e.tensor_tensor(out=dd, in0=m2, in1=m1, op=ALU.subtract), (tr,), (tr,))
            S.op("act", lambda e: e.activation(out=dd, in_=dd, func=AF.Exp), r=(tr,), w=(tr,))
            dv(lambda e: e.tensor_scalar(out=g1, in0=dd, scalar1=1.0, scalar2=None, op0=ALU.add), (tr,), (tr,))
            dv(lambda e: e.reciprocal(out=g1, in_=g1), (tr,), (tr,))
            dv(lambda e: e.tensor_tensor(out=g2, in0=dd, in1=g1, op=ALU.mult), (tr,), (tr,))
            dv(lambda e: e.tensor_tensor(out=g1, in0=g1, in1=pgrp, op=ALU.mult), (tr,), (tr,))
            dv(lambda e: e.tensor_tensor(out=g2, in0=g2, in1=pgrp, op=ALU.mult), (tr,), (tr,))
            dv(lambda e, i=i: e.tensor_tensor(out=A[:, i, :], in0=oh1, in1=oh2, op=ALU.add), (tr,), (tA[i],))
            dv(lambda e, i=i: e.tensor_scalar(out=Gt[:, i, :], in0=oh1, scalar1=g1, scalar2=None, op0=ALU.mult), (tr,), (tA[i],))
            dv(lambda e, i=i: e.scalar_tensor_tensor(out=Gt[:, i, :], in0=oh2, scalar=g2, in1=Gt[:, i, :], op0=ALU.mult, op1=ALU.add),
               (tr, tA[i]), (tA[i],))
        pr, tpr = self.PSBv[1], self.tPSB[1]
        for i in range(NT):
            for j in range(i):
                S.op("pe", lambda e, j=j: e.matmul(pr[:, 0:32], lhsT=ones, rhs=A[:, j, :], start=(j == 0), stop=False), r=(tc_, tA[j]), w=(tpr,))
            S.op("pe", lambda e, i=i: e.matmul(pr[:, 0:32], lhsT=ustrict, rhs=A[:, i, :], start=(i == 0), stop=True), r=(tc_, tA[i]), w=(tpr,))
            S.op("dve", lambda e, i=i: e.scalar_tensor_tensor(out=VAL[:, i, :], in0=pr[:, 0:32], scalar=1.0, in1=A[:, i, :], op0=ALU.add, op1=ALU.mult),
                 r=(tpr, tA[i]), w=(tA[i],))
            S.op("dve", lambda e, i=i: e.tensor_scalar(out=VAL[:, i, :], in0=VAL[:, i, :], scalar1=-1.0, scalar2=None, op0=ALU.add),
                 r=(tA[i],), w=(tA[i],))
        for i in range(NT):
            if i % 2:
                S.op("act", lambda e, i=i: e.mul(out=self.X[:, i, :], in_=self.X[:, i, :], mul=ALPHA), r=(self.tX[i],), w=(self.tX[i],))
            else:
                S.op("dve", lambda e, i=i: e.tensor_scalar(out=self.X[:, i, :], in0=self.X[:, i, :], scalar1=ALPHA, scalar2=None, op0=ALU.mult),
                     r=(self.tX[i],), w=(self.tX[i],))
        wg_d, wu_d, wd_d = self.din("moe_w_gate"), self.din("moe_w_up"), self.din("moe_w_down")
        wts = {}

        def loads(ex):
            wg, twg = self.wload([(lambda b: b[:], wg_d[L, ex].rearrange("(kc p) n -> p kc n", p=128))])
            wu, twu = self.wload([(lambda b: b[:], wu_d[L, ex].rearrange("(kc p) n -> p kc n", p=128))])
            wdv = lambda b: b[:].rearrange("p k n -> p (k n)").rearrange("p (fc d) -> p fc d", fc=4)
            wd_, twd = self.wload([(wdv, wd_d[L, ex].rearrange("(fc p) d -> p fc d", p=128))])
            wts[ex] = (wg, twg, wu, twu, wdv(wd_), twd)

        def front(ex):
            st = sets[ex % 2]
            P, Pg, PgT, XS = st["P"], st["Pg"], st["PgT"], st["XS"]
            for i in range(NT):
                S.op("dve", lambda e, i=i: e.tensor_scalar(out=P[:, i, :], in0=iota, scalar1=VAL[:, i, ex:ex + 1], scalar2=None, op0=ALU.is_equal),
                     r=(tc_, tA[i]), w=(st["tP"][i],))
                S.op("dve", lambda e, i=i: e.tensor_scalar(out=Pg[:, i, :], in0=iota, scalar1=VAL[:, i, ex:ex + 1], scalar2=Gt[:, i, ex:ex + 1],
                                                           op0=ALU.is_equal, op1=ALU.mult), r=(tc_, tA[i]), w=(st["tPg"][i],))
            pt, tpt = self.PST[0], self.tPST[0]
            for i in range(NT):
                S.op("pe", lambda e, i=i: e.transpose(pt[:, i * 128:(i + 1) * 128], Pg[:, i, :], self.c_identb[:]), r=(st["tPg"][i], self.tC), w=(tpt,))
            S.op("act", lambda e: e.copy(out=PgT, in_=pt[:].rearrange("p (i t) -> p i t", i=NT)), r=(tpt,), w=(st["tPgT"],))
            for g in range(4):
                ps, tps = self.PSF[g % 2], self.tPSF[g % 2]
                for j in range(4):
                    kc = g * 4 + j
                    for i in range(NT):
                        S.op("pe", lambda e, kc=kc, j=j, i=i, ps=ps: e.matmul(ps[:, j * CAP:(j + 1) * CAP], lhsT=X1b[:, i, kc * 128:(kc + 1) * 128],
                                                                               rhs=P[:, i, :], start=(i == 0), stop=(i == NT - 1)),
                             r=(tX1b[i], st["tP"][i]), w=(tps,))
                dst = XS[:, g * 4:(g + 1) * 4, :]
                src = ps[:, 0:4 * CAP].rearrange("p (j c) -> p j c", j=4)
                if g % 2:
                    S.op("act", lambda e, dst=dst, src=src: e.copy(out=dst, in_=src), r=(tps,), w=(st["tXS"][g],))
                else:
                    S.op("dve", lambda e, dst=dst, src=src: e.tensor_copy(out=dst, in_=src), r=(tps,), w=(st["tXS"][g],))

        def back(ex):
            st = sets[ex % 2]
            PgT, XS = st["PgT"], st["XS"]
            wg, twg, wu, twu, wd, twd = wts[ex]
            phg, tphg = self.PSBv[0], self.tPSB[0]
            phu, tphu = self.PSBv[1], self.tPSB[1]
            for (pw, tpw, wt, twt) in ((phg, tphg, wg, twg), (phu, tphu, wu, twu)):
                for fc in range(4):
                    for kc in range(NKC):
                        S.op("pe", lambda e, fc=fc, kc=kc, pw=pw, wt=wt: e.matmul(pw[:, fc * CAP:(fc + 1) * CAP], lhsT=wt[:, kc, fc * 128:(fc + 1) * 128],
                                                                                   rhs=XS[:, kc, :], start=(kc == 0), stop=(kc == NKC - 1)),
                             r=(twt, st["tXS"][kc // 4]), w=(tpw,))
            S.op("act", lambda e: e.activation(out=hsg, in_=phg[:, 0:4 * CAP], func=AF.Exp, scale=-1.0), r=(tphg,), w=(thsg,))
            S.op("dve", lambda e: e.tensor_scalar(out=hsg, in0=hsg, scalar1=1.0, scalar2=None, op0=ALU.add), r=(thsg,), w=(thsg,))
            S.op("dve", lambda e: e.reciprocal(out=hsg, in_=hsg), r=(thsg,), w=(thsg,))
            S.op("dve", lambda e: e.tensor_tensor(out=hsg, in0=hsg, in1=phg[:, 0:4 * CAP], op=ALU.mult), r=(thsg, tphg), w=(thsg,))
            S.op("dve", lambda e: e.tensor_tensor(out=HT[:].rearrange("p k c -> p (k c)"), in0=hsg, in1=phu[:, 0:4 * CAP], op=ALU.mult),
                 r=(thsg, tphu), w=(tHT,))
            for nn in range(4):
                py, tpy = self.PSBv[2 + nn % 2], self.tPSB[2 + nn % 2]
                for fc in range(4):
                    S.op("pe", lambda e, fc=fc, nn=nn, py=py: e.matmul(py[0:CAP, :], lhsT=HT[:, fc, :], rhs=wd[:, fc, nn * 512:(nn + 1) * 512],
                                                                        start=(fc == 0), stop=(fc == 3)), r=(tHT, twd), w=(tpy,))
                if nn % 2:
                    S.op("act", lambda e, nn=nn, py=py: e.copy(out=Y[0:CAP, nn * 512:(nn + 1) * 512], in_=py[0:CAP, :]), r=(tpy,), w=(tY[nn],))
                else:
                    S.op("dve", lambda e, nn=nn, py=py: e.tensor_copy(out=Y[0:CAP, nn * 512:(nn + 1) * 512], in_=py[0:CAP, :]), r=(tpy,), w=(tY[nn],))
            ncp = 0
            for i in range(NT):
                for nn in range(4):
                    pcb, tpcb = self.PSBv[2 + ncp % 2], self.tPSB[2 + ncp % 2]
                    ncp += 1
                    S.op("pe", lambda e, i=i, nn=nn, pcb=pcb: e.matmul(pcb[:], lhsT=PgT[0:CAP, i, :], rhs=Y[0:CAP, nn * 512:(nn + 1) * 512], start=True, stop=True),
                         r=(st["tPgT"], tY[nn]), w=(tpcb,))
                    xs = self.X[:, i, nn * 512:(nn + 1) * 512]
                    S.op("dve", lambda e, xs=xs, pcb=pcb: e.tensor_tensor(out=xs, in0=xs, in1=pcb[:], op=ALU.add), r=(tpcb, tXq[i][nn]), w=(tXq[i][nn],))

        self.barrier()
        tXq = [[T(f"Xq{i}_{nn}") for nn in range(4)] for i in range(NT)]
        recF, recB, recL = [], [], []
        for ex in range(NE):
            S.rec = []
            loads(ex)
            recL.append(S.rec)
            assert len(S.rec) == 3
            S.rec = []
            front(ex)
            recF.append(S.rec)
            S.rec = []
            back(ex)
            recB.append(S.rec)
            S.rec = None
        for ent in recL[0] + recF[0]:
            S.emit(ent)
        for ex in range(NE):
            b = recB[ex]
            fnx = recF[ex + 1] if ex + 1 < NE else []
            lnx = recL[ex + 1] if ex + 1 < NE else []
            nb_, nf_ = len(b), len(fnx)
            fi = 0
            for bi, ent in enumerate(b):
                S.emit(ent)
                if lnx and bi == 127:
                    S.emit(lnx[0])
                    S.emit(lnx[1])
                if lnx and bi == 127 + 5 + 20:
                    S.emit(lnx[2])
                tgt = ((bi + 1) * nf_) // nb_
                while fi < tgt:
                    S.emit(fnx[fi])
                    fi += 1
            while fi < nf_:
                S.emit(fnx[fi])
                fi += 1
        self.barrier()

    def kv_alloc(self):
        self.KT2 = self.arb(4 * 1152).rearrange("p (h t) -> p h t", h=4)
        self.V = self.arb(9 * 256).rearrange("p (b c) -> p b c", b=9)
        self.tKV = T("KV")
        self.att_base = self.ar_off

    def save_halo(self):
        self.S.dma("sp", self.d_halo[:, :], self.X[:, NT - 1, :], r=(self.tX[NT - 1],))

    def kv_compute(self):
        S = self.S
        self.ar_off = self.att_base
        Xh = self.arf(D)
        XTh = self.arb(NKC * 128).rearrange("p (k t) -> p k t", k=NKC)
        tXh, tXTh = T("Xh"), T("XTh")
        S.dma("sp", Xh, self.d_halo[:, :], w=(tXh,))
        for g in range(4):
            ps, tps = self.PSF[g % 2], self.tPSF[g % 2]
            for j in range(4):
                kc = g * 4 + j
                S.op("pe", lambda e, kc=kc, j=j, ps=ps: e.transpose(ps[:, j * 128:(j + 1) * 128], Xh[:, kc * 128:(kc + 1) * 128], self.c_ident[:]),
                     r=(tXh, self.tC), w=(tps,))
            S.op("dve", lambda e, g=g, ps=ps: e.tensor_copy(out=XTh[:, g * 4:(g + 1) * 4, :], in_=ps[:].rearrange("p (j t) -> p j t", j=4)),
                 r=(tps,), w=(tXTh,))
        wkv = self.din("b_w_kv")
        bufv, tbv = self.wload([(lambda b: b[:], wkv.rearrange("(kc p) n -> p kc n", p=128))])
        wkd_v = lambda b: b[:].rearrange("p k (h f) -> p k h f", h=4)
        pieces = []
        for hk in range(4):
            for dup in range(2):
                pieces.append((lambda b, hk=hk, dup=dup: wkd_v(b)[:, :, hk, dup * 64:(dup + 1) * 64],
                               wkv[:, hk * 64:(hk + 1) * 64].rearrange("(kc p) n -> p kc n", p=128)))
        bufk_, tbk = self.wload(pieces)
        bufk = wkd_v(bufk_)
        n = 0
        for b in range(9):
            ps, tps = self.PSF[n % 2], self.tPSF[n % 2]
            n += 1
            for kc in range(NKC):
                lhsT = XTh[:, kc, :] if b == 0 else self.XT[:, kc, (b - 1) * 128:b * 128]
                rt = (tXTh,) if b == 0 else (self.tXT[b - 1],)
                S.op("pe", lambda e, kc=kc, ps=ps, lhsT=lhsT: e.matmul(ps[:, 0:256], lhsT=lhsT, rhs=bufv[:, kc, 256:512], start=(kc == 0), stop=(kc == NKC - 1)),
                     r=rt + (tbv,), w=(tps,))
            S.op("dve", lambda e, b=b, ps=ps: e.tensor_copy(out=self.V[:, b, :], in_=ps[:, 0:256]), r=(tps,), w=(self.tKV,))
        for hk in range(4):
            for part in range(3):
                ps, tps = self.PSF[n % 2], self.tPSF[n % 2]
                n += 1
                if part == 0:
                    rhs_fn, ncol, rt, dst = (lambda kc: XTh[:, kc, :]), 128, (tXTh,), self.KT2[:, hk, 0:128]
                else:
                    lo = (part - 1) * 512
                    rhs_fn, ncol, rt = (lambda kc, lo=lo: self.XT[:, kc, lo:lo + 512]), 512, tuple(self.tXT[(part - 1) * 4:(part - 1) * 4 + 4])
                    dst = self.KT2[:, hk, 128 + lo:128 + lo + 512]
                for kc in range(NKC):
                    S.op("pe", lambda e, kc=kc, ps=ps, hk=hk, rhs_fn=rhs_fn, ncol=ncol: e.matmul(ps[:, 0:ncol], lhsT=bufk[:, kc, hk, :], rhs=rhs_fn(kc),
                                                                                                 start=(kc == 0), stop=(kc == NKC - 1)),
                         r=rt + (tbk,), w=(tps,))
                S.op("act", lambda e, ps=ps, ncol=ncol, dst=dst: e.copy(out=dst, in_=ps[:, 0:ncol]), r=(tps,), w=(self.tKV,))
        self.barrier()

    def attention(self, j_layer):
        S = self.S
        self.barrier()
        self.ar_off = self.att_base
        f = self.arf
        biasm = f(1024).rearrange("p (h k) -> p h k", h=4)
        PB = self.arb(1024).rearrange("p (h k) -> p h k", h=4)
        PTs = self.arb(1024).rearrange("p (a q) -> p a q", a=8)
        QTg = self.arb(2048).rearrange("p (c t) -> p c t", c=2)
        OB = self.arb(256)
        SS2 = f(512).rearrange("p (h k) -> p h k", h=2)
        tSS = T("SS2")
        sm = f(32)
        sink, rm, negm, rs, es, rinv = (sm[:, 4 * k:4 * k + 4] for k in range(6))
        tb, tPB, tPTs, tQ, tOB, tsm = T("biasm"), T("PB"), T("PTs"), T("QTg"), T("OB"), T("asm")
        wq = self.din("b_w_q")
        btab = self.din("bias_tab")
        sinks = self.din("b_sinks")
        PSS3 = self.PSS[:].rearrange("p (h k) -> p h k", h=4)
        tS = (self.tPSB[0], self.tPSB[1])
        n = 0
        for hk in range(4):
            bufq, tbq = self.wload([(lambda b: b[:], wq[j_layer, :, hk * 512:(hk + 1) * 512].rearrange("(kc p) n -> p kc n", p=128))])
            for hh in range(2):
                h0 = 8 * hk + 4 * hh
                c0 = h0 // 2
                for cc in range(2):
                    for half in range(2):
                        ps, tps = self.PSF[n % 2], self.tPSF[n % 2]
                        n += 1
                        for kc in range(NKC):
                            S.op("pe", lambda e, kc=kc, ps=ps, cc=cc, half=half, hh=hh, bufq=bufq: e.matmul(
                                ps[:], lhsT=bufq[:, kc, hh * 256 + cc * 128:hh * 256 + (cc + 1) * 128], rhs=self.XT[:, kc, half * 512:(half + 1) * 512],
                                start=(kc == 0), stop=(kc == NKC - 1)), r=tuple(self.tXT[half * 4:half * 4 + 4]) + (tbq,), w=(tps,))
                        S.op("act", lambda e, ps=ps, cc=cc, half=half: e.copy(out=QTg[:, cc, half * 512:(half + 1) * 512], in_=ps[:]), r=(tps,), w=(tQ,))
                pos = lambda j: (j % 2) * 2 + j // 2
                for j in range(4):
                    S.dma("sp", biasm[:, pos(j), :], btab[h0 + j], w=(tb,))
                    S.dma("sp", sink[:, pos(j):pos(j) + 1], sinks[j_layer:j_layer + 1, h0 + j:h0 + j + 1].partition_broadcast(128), w=(tsm,))
                for j in range(4):
                    S.op("dve", lambda e, j=j: e.tensor_tensor(out=biasm[:, j, :], in0=biasm[:, j, :], in1=self.c_maskc[:], op=ALU.add), r=(tb, self.tC), w=(tb,))
                for nb in range(NT):
                    if os.environ.get("ATT_STOP") == "q":
                        continue
                    qs = slice(nb * 128, (nb + 1) * 128)
                    for j in range(4):
                        p, cc = j % 2, j // 2
                        S.op("pe", lambda e, j=j, p=p, cc=cc, qs=qs, nb=nb, hk=hk: e.matmul(
                            self.PSS[:, pos(j) * 256:(pos(j) + 1) * 256], lhsT=QTg[p * 64:(p + 1) * 64, cc, qs],
                            rhs=self.KT2[p * 64:(p + 1) * 64, hk, nb * 128:nb * 128 + 256], start=True, stop=True),
                            r=(tQ, self.tKV), w=(self.tPSB[j % 2],))
                    for bk in range(2):
                        j0 = 2 * bk
                        pbank = self.PSS[:, bk * 512:(bk + 1) * 512].rearrange("p (h k) -> p h k", h=2)
                        S.op("dve", lambda e, pbank=pbank, j0=j0: e.scalar_tensor_tensor(out=SS2, in0=pbank, scalar=0.125, in1=biasm[:, j0:j0 + 2, :],
                                                                                         op0=ALU.mult, op1=ALU.add), r=(tb, self.tPSB[bk]), w=(tSS,))
                        if nb == 0:
                            for jj in range(2):
                                S.op("dve", lambda e, jj=jj: e.tensor_tensor(out=SS2[:, jj, 0:128], in0=SS2[:, jj, 0:128], in1=self.c_halo[:], op=ALU.add),
                                     r=(self.tC, tSS), w=(tSS,))
                        S.op("dve", lambda e, j0=j0: e.tensor_reduce(out=rm[:, j0:j0 + 2], in_=SS2, axis=AX.X, op=ALU.max), r=(tSS,), w=(tsm,))
                        S.op("dve", lambda e, j0=j0: e.tensor_tensor(out=rm[:, j0:j0 + 2], in0=rm[:, j0:j0 + 2], in1=sink[:, j0:j0 + 2], op=ALU.max), r=(tsm,), w=(tsm,))
                        S.op("dve", lambda e, j0=j0: e.tensor_scalar(out=negm[:, j0:j0 + 2], in0=rm[:, j0:j0 + 2], scalar1=-1.0, scalar2=None, op0=ALU.mult), r=(tsm,), w=(tsm,))
                        for jj in range(2):
                            j = j0 + jj
                            S.op("act", lambda e, j=j, jj=jj: e.activation(out=PB[:, j, :], in_=SS2[:, jj, :], func=AF.Exp,
                                                                            bias=negm[:, j:j + 1], scale=1.0, accum_out=rs[:, j:j + 1]),
                                 r=(tSS, tsm), w=(tPB, tsm))
                    S.op("dve", lambda e: e.tensor_tensor(out=es, in0=sink, in1=negm, op=ALU.add), r=(tsm,), w=(tsm,))
                    S.op("act", lambda e: e.activation(out=es, in_=es, func=AF.Exp), r=(tsm,), w=(tsm,))
                    S.op("dve", lambda e: e.tensor_tensor(out=rinv, in0=rs, in1=es, op=ALU.add), r=(tsm,), w=(tsm,))
                    S.op("dve", lambda e: e.reciprocal(out=rinv, in_=rinv), r=(tsm,), w=(tsm,))
                    pt, tpt = self.PST[0], self.tPST[0]
                    for j in range(4):
                        for kb in range(2):
                            a = j * 2 + kb
                            S.op("pe", lambda e, j=j, kb=kb, a=a: e.transpose(pt[:, a * 128:(a + 1) * 128], PB[:, j, kb * 128:(kb + 1) * 128], self.c_identb[:]),
                                 r=(tPB, self.tC), w=(tpt,))
                    S.op("act", lambda e: e.copy(out=PTs, in_=pt[:].rearrange("p (a q) -> p a q", a=8)), r=(tpt,), w=(tPTs,))
                    po, tpo = self.PSBv[2], self.tPSB[2]
                    for j in range(4):
                        for kb in range(2):
                            S.op("pe", lambda e, j=j, kb=kb, nb=nb, hk=hk: e.matmul(po[:, j * 64:(j + 1) * 64], lhsT=PTs[:, pos(j) * 2 + kb, :],
                                                                                    rhs=self.V[:, nb + kb, hk * 64:(hk + 1) * 64],
                                                                                    start=(kb == 0), stop=(kb == 1)),
                                 r=(tPTs, self.tKV), w=(tpo,))
                    for j in range(4):
                        S.op("dve", lambda e, j=j: e.tensor_scalar(out=OB[:, j * 64:(j + 1) * 64], in0=po[:, j * 64:(j + 1) * 64], scalar1=rinv[:, pos(j):pos(j) + 1],
                                                                   scalar2=None, op0=ALU.mult), r=(tpo, tsm), w=(tOB,))
                    pt1, tpt1 = self.PST[1], self.tPST[1]
                    for cc in range(2):
                        S.op("pe", lambda e, cc=cc: e.transpose(pt1[:, cc * 128:(cc + 1) * 128], OB[:, cc * 128:(cc + 1) * 128], self.c_identb[:]),
                             r=(tOB, self.tC), w=(tpt1,))
                    S.op("dve", lambda e, c0=c0, qs=qs: e.tensor_copy(out=self.OT[:, c0:c0 + 2, qs], in_=pt1[:, 0:256].rearrange("p (c q) -> p c q", c=2)),
                         r=(tpt1,), w=(self.tOT[nb],))
        self.barrier()

    def _program(self):
        S = self.S
        self.c_eps = {}
        for eps in (LN_EPS, RMS_EPS):
            t = self.sb(f"eps{len(self.c_eps)}", [128, 1])
            self.c_eps[eps] = t
            S.op("dve", lambda e, t=t, eps=eps: e.memset(t[:], eps), w=(self.tC,))
        self.load_consts()
        stop = self.debug_stop
        if stop is not None and stop != "hg1":
            self.load_x(self.din("xm"))
        if stop in ("xt", "xt0"):
            self.make_xt()
        if stop == "xt":
            self.layernorm(0)
            self.store_out()
            return
        if stop == "hg1":
            self.load_x(self.din("xp"))
            self.make_xt()
            self.hgrn(1, "zero")
            self.barrier()
            self.load_x(self.din("xm"))
            self.make_xt()
            self.hgrn(1, "load")
            self.barrier()
            self.out_proj(self.din("a_w_out")[1])
            self.layernorm(2)
            self.store_out()
            return
        if stop == "att":
            self.phase()
            self.kv_alloc()
            self.load_x(self.din("xp"))
            self.save_halo()
            self.barrier()
            self.load_x(self.din("xm"))
            self.make_xt()
            self.kv_compute()
            if os.environ.get("ATT_STOP") == "kv":
                self.store_out()
                return
            self.attention(0)
            if os.environ.get("ATT_STOP"):
                self.store_out()
                return
            self.out_proj(self.din("b_w_out")[0])
            self.layernorm(4)
            self.store_out()
            return
        if stop == "moe":
            self.moe(0)
            self.layernorm(1)
            self.store_out()
            return
        if stop == "xt0":
            self.barrier()
            self.store_out()
            return
        def hgrn_layer(l, mode):
            self.make_xt()
            self.hgrn(l, mode)
            self.barrier()
            self.out_proj(self.din("a_w_out")[l])
            self.layernorm(2 * l)
            self.moe(l)
            self.layernorm(2 * l + 1)

        self.load_x(self.din("xp"))
        hgrn_layer(0, "zero")
        hgrn_layer(1, "zero")
        self.save_halo()
        self.barrier()
        self.load_x(self.din("xm"))
        hgrn_layer(0, "load")
        hgrn_layer(1, "load")
        self.make_xt()
        self.phase()
        self.kv_alloc()
        self.kv_compute()
        for j in range(2):
            if j:
                self.make_xt()
            self.attention(j)
            self.out_proj(self.din("b_w_out")[j])
            self.layernorm(4 + 2 * j)
            self.moe(2 + j)
            self.layernorm(5 + 2 * j)
        self.store_out()

    def store_out(self):
        S = self.S
        for i in range(NT):
            S.dma("sp", self.y[i * 128:(i + 1) * 128, :], self.X[:, i, :], r=(self.tX[i],))
        S.eng["sp"].wait_ge(S.sem["sp_d"], S.cnt["sp_d"])


_CACHE = {}


def _prep_inputs(inputs):
    c = host_consts()
    bucket = c.pop("_bucket")
    rel_bias = np.asarray(inputs["rel_bias"], np.float32)
    bias_tab = np.ascontiguousarray(rel_bias[bucket].transpose(2, 0, 1))
    shared = {k: np.ascontiguousarray(np.asarray(inputs[k], np.float32)) for k in
              ["a_w_in", "a_lower_bound", "a_norm_g", "a_w_out", "b_w_kv", "b_w_q", "b_sinks", "b_w_out",
               "moe_w_rg", "moe_b_rg", "moe_w_re", "moe_b_re", "moe_w_gate", "moe_w_up", "moe_w_down", "ln_g", "ln_b"]}
    shared["bias_tab"] = bias_tab
    shared.update(c)
    x = np.asarray(inputs["x"], np.float32)
    in_maps = []
    for core in range(8):
        b, half = core // 2, core % 2
        m = dict(shared)
        m["xp"] = np.ascontiguousarray(x[b, 0:SEG])
        m["xm"] = np.ascontiguousarray(x[b, half * SEG:(half + 1) * SEG])
        m["flag"] = np.full((128, 1), float(half), np.float32)
        m["halo_mask"] = np.full((128, 128), 0.0 if half else NEG, np.float32)
        in_maps.append(m)
    return in_maps


def kernel(**inputs):
    if "nc" not in _CACHE:
        p = Prog()
        _CACHE["nc"] = p.build()
        _CACHE["used"] = set(p.in_aps.keys())
    nc = _CACHE["nc"]
    in_maps = [{k: v for k, v in m.items() if k in _CACHE["used"]} for m in _prep_inputs(inputs)]
    res = run_bass_kernel_spmd(nc, in_maps, core_ids=list(range(8)))
    out = np.zeros((4, 2 * SEG, D), np.float32)
    for core in range(8):
        b, half = core // 2, core % 2
        out[b, half * SEG:(half + 1) * SEG] = res.results[core]["y"]
    return out
```

```python
import math
import os
HGL = int(os.environ.get('HGL', '99'))
HGH = int(os.environ.get('HGH', '16'))
HGV = int(os.environ.get('HGV', '3'))
from contextlib import ExitStack

import numpy as np
import concourse.bass as bass
import concourse.mybir as mybir
from concourse.bass_utils import run_bass_kernel_spmd

F32 = mybir.dt.float32
BF16 = mybir.dt.bfloat16
ALU = mybir.AluOpType
AF = mybir.ActivationFunctionType
AX = mybir.AxisListType

D = 2048
NT = 8
SEG = NT * 128
NKC = 16
CAP = 128
NE = 32
ALPHA = 8 ** 0.25
LN_EPS = 1e-5
RMS_EPS = 1e-6
NEG = -1e30
SAME_ENGINE_SYNC = True


class T:
    __slots__ = ("name", "w", "r", "excl")

    def __init__(self, name, excl=False):
        self.name = name
        self.w = None
        self.r = {}
        self.excl = excl


class Sched:
    def __init__(self, nc, es):
        self.nc = nc
        self.eng = {"pe": nc.tensor, "act": nc.scalar, "dve": nc.vector, "pool": nc.gpsimd, "sp": nc.sync}
        self.sem = {}
        self.cnt = {}
        for k in ["pe", "act", "dve", "pool", "sp", "pool_d", "sp_d"]:
            self.sem[k] = es.enter_context(nc.semaphore("sem_" + k))
            self.cnt[k] = 0
        self.waited = {k: {} for k in self.eng}
        self.n_ins = 0
        self.rec = None

    def _deps(self, e, r, w):
        deps = {}
        for t in r:
            if t.w is not None:
                k, v = t.w
                deps[k] = max(deps.get(k, 0), v)
        for t in w:
            if t.w is not None:
                k, v = t.w
                deps[k] = max(deps.get(k, 0), v)
            for k, v in t.r.items():
                deps[k] = max(deps.get(k, 0), v)
        for k, v in deps.items():
            if k == e and (e == "pe" or not SAME_ENGINE_SYNC):
                continue
            if self.waited[e].get(k, 0) < v:
                self.eng[e].wait_ge(self.sem[k], v)
                self.waited[e][k] = v

    def op(self, e, fn, r=(), w=()):
        if self.rec is not None:
            self.rec.append(("op", e, fn, tuple(r), tuple(w)))
            return
        xr = tuple(t for t in r if t.excl)
        if xr:
            r = tuple(t for t in r if not t.excl)
            w = tuple(w) + xr
        self._deps(e, r, w)
        ins = fn(self.eng[e])
        ins.then_inc(self.sem[e], 1)
        self.cnt[e] += 1
        self.n_ins += 1
        v = self.cnt[e]
        for t in r:
            t.r[e] = v
        for t in w:
            t.w = (e, v)
            t.r = {}

    def emit(self, ent):
        if ent[0] == "op":
            self.op(ent[1], ent[2], r=ent[3], w=ent[4])
        else:
            self.dma(ent[1], ent[2], ent[3], r=ent[4], w=ent[5])

    def dma(self, q, out, in_, r=(), w=()):
        if self.rec is not None:
            self.rec.append(("dma", q, out, in_, tuple(r), tuple(w)))
            return
        self._deps(q, r, w)
        dk = q + "_d"
        self.eng[q].dma_start(out=out, in_=in_).then_inc(self.sem[dk], 16)
        self.cnt[dk] += 16
        self.n_ins += 1
        v = self.cnt[dk]
        for t in r:
            t.r[dk] = v
        for t in w:
            t.w = (dk, v)
            t.r = {}

    def wait_all(self, e, tiles):
        self._deps(e, tiles, tiles)


def _t5_bucket_np(dist):
    n = np.clip(dist, 0, 127)
    max_exact = 16
    large = max_exact + (np.log(np.maximum(n, max_exact).astype(np.float32) / np.float32(max_exact))
                         / np.float32(math.log(128 / max_exact)) * np.float32(32 - max_exact)).astype(np.int32)
    large = np.minimum(large, 31)
    return np.where(n < max_exact, n, large)


def host_consts():
    c = {}
    t = np.arange(128)
    ch = t // 64
    same = ch[:, None] == ch[None, :]
    mid = ch * 64 + 31
    L1 = same * ((t[:, None] <= t[None, :]).astype(np.float32) - (t[:, None] <= mid[None, :]).astype(np.float32))
    L2 = same * (t[:, None] > t[None, :]).astype(np.float32)
    sel = np.zeros((128, 4), np.float32)
    sel[:, 0] = t <= 31
    sel[:, 1] = t < 64
    sel[:, 2] = (t >= 64) & (t <= 95)
    sel[:, 3] = t >= 64
    maskT = (same & (t[:, None] <= t[None, :])).astype(np.float32)
    ustrict = (t[:, None] < t[None, :]).astype(np.float32)
    c["c_ident"] = np.eye(128, dtype=np.float32)
    c["c_L1"] = L1.astype(np.float32)
    c["c_L2"] = L2.astype(np.float32)
    c["c_sel"] = sel
    c["c_maskT"] = maskT
    c["c_ustrict"] = ustrict
    c["c_ones"] = np.ones((128, 128), np.float32)
    c["c_iota"] = np.tile(np.arange(CAP, dtype=np.float32)[None, :], (128, 1))
    qi = np.arange(128)[:, None]
    kj = np.arange(256)[None, :]
    dist = qi + 128 - kj
    inwin = (dist >= 0) & (dist < 128)
    c["c_maskc"] = np.where(inwin, 0.0, NEG).astype(np.float32)
    c["_bucket"] = _t5_bucket_np(dist)
    return c


class Prog:
    def __init__(self, debug_stop=None):
        self.debug_stop = debug_stop
        self.nc = bass.Bass("TRN2", target_bir_lowering=False)
        self.es = ExitStack()
        self.in_shapes = {}
        self.in_aps = {}

    def dram_in(self, name, shape):
        self.in_shapes[name] = list(shape)
        return None

    def din(self, name):
        if name not in self.in_aps:
            self.in_aps[name] = self.nc.dram_tensor(name, self.in_shapes[name], F32, kind="ExternalInput").ap()
        return self.in_aps[name]

    def sb(self, name, shape, dt=F32):
        return self.es.enter_context(self.nc.sbuf_tensor("s_" + name, list(shape), dt))

    def build(self):
        nc = self.nc
        es = self.es
        with es:
            self.S = Sched(nc, es)
            self._declare()
            self._program()
        return nc

    def _declare(self):
        nc = self.nc
        di = self.dram_in
        di("xp", [SEG, D]); di("xm", [SEG, D])
        di("a_w_in", [2, D, 4 * D]); di("a_lower_bound", [2, D]); di("a_norm_g", [2, D]); di("a_w_out", [2, D, D])
        di("b_w_kv", [D, 512]); di("b_w_q", [2, D, D]); di("b_sinks", [2, 32]); di("b_w_out", [2, D, D])
        di("bias_tab", [32, 128, 256])
        di("moe_w_rg", [4, D, 4]); di("moe_b_rg", [4, 4]); di("moe_w_re", [4, D, 32]); di("moe_b_re", [4, 32])
        di("moe_w_gate", [4, NE, D, 512]); di("moe_w_up", [4, NE, D, 512]); di("moe_w_down", [4, NE, 512, D])
        di("ln_g", [8, D]); di("ln_b", [8, D]); di("flag", [128, 1]); di("halo_mask", [128, 128])
        for k, shp in [("c_ident", [128, 128]), ("c_L1", [128, 128]), ("c_L2", [128, 128]), ("c_sel", [128, 4]),
                       ("c_maskT", [128, 128]), ("c_ustrict", [128, 128]), ("c_ones", [128, 128]),
                       ("c_iota", [128, CAP]), ("c_maskc", [128, 256])]:
            di(k, shp)
        self.y = nc.dram_tensor("y", [SEG, D], F32, kind="ExternalOutput").ap()
        self.d_state = [nc.dram_tensor(f"d_state{l}", [128, 16 * 128], F32, kind="Internal").ap() for l in range(2)]
        self.d_halo = nc.dram_tensor("d_halo", [128, D], F32, kind="Internal").ap()

        sb = self.sb
        self.X = sb("X", [128, NT, D])
        self.XTraw = sb("XTraw", [128, 8192])
        self.XT = self.XTraw[:].bitcast(BF16).rearrange("p (k t) -> p k t", k=NKC)
        self.OT = sb("OT", [128, NKC, SEG], BF16)
        self.tX = [T(f"X{i}") for i in range(NT)]
        self.tXT = [T(f"XT{i}") for i in range(NT)]
        self.tOT = [T(f"OT{i}") for i in range(NT)]
        self.NB = 3
        self.WB = [sb(f"WB{i}", [128, NKC, 512], BF16) for i in range(self.NB)]
        self.tWB = [T(f"WB{i}") for i in range(self.NB)]
        self.wb_next = 0
        self.c_ident = sb("c_ident", [128, 128])
        self.c_identb = sb("c_identb", [128, 128], BF16)
        self.c_maskc = sb("c_maskc", [128, 256])
        self.c_flag = sb("c_flag", [128, 1])
        self.c_halo = sb("c_halo", [128, 128])
        self.tC = T("consts")
        self.lnst = sb("lnst", [128, 8])
        self.tlnst = T("lnst")
        self.ARN = 7300
        self.AR = sb("arena", [128, self.ARN])
        self.ar_off = 0
        self.PSS = self.es.enter_context(nc.psum_tensor("pss", [128, 1024], F32))
        self.PSG = [self.es.enter_context(nc.psum_tensor(f"psg{i}", [128, 512], F32)) for i in range(2)]
        self.PSBv = [self.PSS[:, 0:512], self.PSS[:, 512:1024], self.PSG[0][:], self.PSG[1][:]]
        self.PSF = [self.es.enter_context(nc.psum_tensor(f"psf{i}", [128, 512], F32)) for i in range(2)]
        self.PST = [self.es.enter_context(nc.psum_tensor(f"pst{i}", [128, 1024], BF16)) for i in range(2)]
        self.tPSB = [T(f"psb{i}", True) for i in range(4)]
        self.tPSF = [T(f"psf{i}", True) for i in range(2)]
        self.tPST = [T(f"pst{i}", True) for i in range(2)]

    def barrier(self):
        S = self.S
        for e in S.eng:
            for k in S.sem:
                if k == e:
                    continue
                if S.cnt[k] > S.waited[e].get(k, 0):
                    S.eng[e].wait_ge(S.sem[k], S.cnt[k])
                    S.waited[e][k] = S.cnt[k]

    def phase(self):
        self.barrier()
        self.ar_off = 0

    def arf(self, cols, shape=None):
        o = self.ar_off
        self.ar_off += cols
        assert self.ar_off <= self.ARN, f"arena overflow {self.ar_off}"
        v = self.AR[:, o:o + cols]
        return v

    def arb(self, cols):
        assert cols % 2 == 0
        return self.arf(cols // 2).bitcast(BF16)

    def wload(self, pieces):
        i = self.wb_next
        self.wb_next = (i + 1) % getattr(self, "NB_active", self.NB)
        buf, t = self.WB[i], self.tWB[i]
        for dst_fn, src in pieces:
            self.S.dma("pool", dst_fn(buf), src, r=(), w=(t,))
        return buf, t

    def load_consts(self):
        S = self.S
        for k in ["c_ident", "c_maskc"]:
            S.dma("sp", getattr(self, k)[:], self.din(k)[:, :], w=(self.tC,))
        S.dma("sp", self.c_flag[:], self.din("flag")[:, :], w=(self.tC,))
        S.dma("sp", self.c_halo[:], self.din("halo_mask")[:, :], w=(self.tC,))
        S.op("dve", lambda e: e.tensor_copy(out=self.c_identb[:], in_=self.c_ident[:]), r=(self.tC,), w=(self.tC,))

    def load_x(self, src):
        for i in range(NT):
            self.S.dma("sp", self.X[:, i, :], src[i * 128:(i + 1) * 128, :], w=(self.tX[i],))

    def make_xt(self):
        S = self.S
        n = 0
        for i in range(NT):
            for g in range(4):
                pb = n % 2
                n += 1
                ps, tps = self.PSF[pb], self.tPSF[pb]
                for j in range(4):
                    kc = g * 4 + j
                    S.op("pe", lambda e, kc=kc, j=j, ps=ps, i=i: e.transpose(
                        ps[:, j * 128:(j + 1) * 128], self.X[:, i, kc * 128:(kc + 1) * 128], self.c_ident[:]),
                        r=(self.tX[i], self.tC), w=(tps,))
                dst = self.XT[:, g * 4:(g + 1) * 4, i * 128:(i + 1) * 128]
                src = ps[:].rearrange("p (j t) -> p j t", j=4)
                if n % 2:
                    S.op("act", lambda e, dst=dst, src=src: e.copy(out=dst, in_=src), r=(tps,), w=(self.tXT[i],))
                else:
                    S.op("dve", lambda e, dst=dst, src=src: e.tensor_copy(out=dst, in_=src), r=(tps,), w=(self.tXT[i],))

    def rsqrt_small(self, out, in_, scale, eps, rt, wt):
        S = self.S
        S.op("act", lambda e: e.activation(out=out, in_=in_, func=AF.Ln, bias=self.epsb(eps), scale=scale), r=rt, w=wt)
        S.op("act", lambda e: e.activation(out=out, in_=out, func=AF.Exp, scale=-0.5), r=wt, w=wt)

    def epsb(self, eps):
        return self.c_eps[eps][:]

    def layernorm(self, idx):
        S = self.S
        self.barrier()
        vg = self.XTraw[:, 0:D]
        vb = self.XTraw[:, D:2 * D]
        tv = T("lnvec")
        S.dma("sp", vg, self.din("ln_g")[idx:idx + 1, :].partition_broadcast(128), w=(tv,))
        S.dma("sp", vb, self.din("ln_b")[idx:idx + 1, :].partition_broadcast(128), w=(tv,))
        st, tst = self.lnst, self.tlnst
        junk = self.OT[:, 0:2, :]
        tj = T("lnjunk")
        for i in range(NT):
            xi = self.X[:, i, :]
            xi3 = xi.rearrange("p (a b) -> p a b", a=2)
            S.op("act", lambda e, xi3=xi3: e.activation(out=junk, in_=xi3, func=AF.Copy, accum_out=st[:, 0:1]),
                 r=(self.tX[i],), w=(tst, tj))
            S.op("act", lambda e, xi3=xi3: e.activation(out=junk, in_=xi3, func=AF.Square, accum_out=st[:, 1:2]),
                 r=(self.tX[i],), w=(tst, tj))
            S.op("dve", lambda e: e.tensor_scalar(out=st[:, 2:4], in0=st[:, 0:2], scalar1=1.0 / D, scalar2=None, op0=ALU.mult),
                 r=(tst,), w=(tst,))
            S.op("dve", lambda e: e.tensor_tensor(out=st[:, 4:5], in0=st[:, 2:3], in1=st[:, 2:3], op=ALU.mult), r=(tst,), w=(tst,))
            S.op("dve", lambda e: e.tensor_tensor(out=st[:, 5:6], in0=st[:, 3:4], in1=st[:, 4:5], op=ALU.subtract), r=(tst,), w=(tst,))
            self.rsqrt_small(st[:, 6:7], st[:, 5:6], 1.0, LN_EPS, (tst,), (tst,))
            S.op("dve", lambda e: e.scalar_tensor_tensor(out=st[:, 7:8], in0=st[:, 2:3], scalar=-1.0, in1=st[:, 6:7], op0=ALU.mult, op1=ALU.mult),
                 r=(tst,), w=(tst,))
            S.op("act", lambda e, xi=xi: e.activation(out=xi, in_=xi, func=AF.Identity, bias=st[:, 7:8], scale=st[:, 6:7]),
                 r=(tst, self.tX[i]), w=(self.tX[i],))
            S.op("dve", lambda e, xi=xi: e.tensor_tensor(out=xi, in0=xi, in1=vg, op=ALU.mult), r=(tv, self.tX[i]), w=(self.tX[i],))
            S.op("dve", lambda e, xi=xi: e.tensor_tensor(out=xi, in0=xi, in1=vb, op=ALU.add), r=(tv, self.tX[i]), w=(self.tX[i],))
        self.barrier()

    def out_proj(self, w_dram):
        S = self.S
        n = 0
        for nch in range(4):
            buf, tb = self.wload([(lambda b: b[:], w_dram[:, nch * 512:(nch + 1) * 512].rearrange("(kc p) n -> p kc n", p=128))])
            for i in range(NT):
                pb = n % 2
                n += 1
                ps, tps = self.PSF[pb], self.tPSF[pb]
                for kc in range(NKC):
                    S.op("pe", lambda e, kc=kc, ps=ps, i=i, buf=buf: e.matmul(
                        ps[:], lhsT=self.OT[:, kc, i * 128:(i + 1) * 128], rhs=buf[:, kc, :], start=(kc == 0), stop=(kc == NKC - 1)),
                        r=(self.tOT[i], tb), w=(tps,))
                xs = self.X[:, i, nch * 512:(nch + 1) * 512]
                S.op("dve", lambda e, xs=xs, ps=ps: e.scalar_tensor_tensor(out=xs, in0=xs, scalar=ALPHA, in1=ps[:], op0=ALU.mult, op1=ALU.add),
                     r=(tps, self.tX[i]), w=(self.tX[i],))

    def hgrn(self, l, init_mode):
        S = self.S
        self.phase()
        f = self.arf
        St = f(2048).rearrange("p (h v) -> p h v", h=16)
        tSt = [T(f"St{h}") for h in range(16)]
        cL1, cL2, cMT, csel = f(128), f(128), f(128), f(4)
        tc = T("hc")
        for dst, k in [(cL1, "c_L1"), (cL2, "c_L2"), (cMT, "c_maskT"), (csel, "c_sel")]:
            S.dma("sp", dst, self.din(k)[:, :], w=(tc,))
        if init_mode == "zero":
            S.op("dve", lambda e: e.memset(St, 0.0), w=tuple(tSt))
        else:
            S.dma("sp", St, self.d_state[l].rearrange("p (h v) -> p h v", h=16), w=tuple(tSt))
            S.op("dve", lambda e: e.tensor_scalar(out=St, in0=St, scalar1=self.c_flag[:, 0:1], scalar2=None, op0=ALU.mult),
                 r=tuple(tSt) + (self.tC,), w=tuple(tSt))
        a0, a1, lb, oml, ng = f(128), f(128), f(128), f(128), f(128)
        tv = T("hvec")
        wb2 = self.WB[2][:].rearrange("p k n -> p (k n)").bitcast(F32)
        wb2_off = [0]

        def f2(cols):
            o = wb2_off[0]
            wb2_off[0] += cols
            assert wb2_off[0] <= 4096
            return wb2[:, o:o + cols]

        def mk_set(ff, sfx):
            fb_ = lambda cols: ff(cols // 2).bitcast(BF16)
            ws = {}
            for k in ["e1", "t1", "fg", "kk", "logf", "eq", "ek", "ekl", "sg", "gg"]:
                ws[k] = ff(128)
            ws["ebT"] = ff(4)
            ws["ss"] = ff(4)
            for k in ["qt", "kt", "kh", "vv", "QF", "KTs", "sTm", "Sp0", "Sp1", "on", "junk"]:
                ws[k] = fb_(128)
            ws["QZ"] = fb_(256).rearrange("p (a b) -> p a b", a=2)
            ws["tw"] = {k: T("hw_" + k + sfx) for k in ["e1", "t1", "fg", "kk", "logf", "eq", "ek", "ekl", "ebT", "sg", "gg", "ss", "qt", "kt", "kh", "vv",
                                                      "QF", "KTs", "sTm", "Sp0", "Sp1", "on", "QZ", "junk"]}
            S.op("dve", lambda e, ws=ws: e.memset(ws["QZ"], 0.0), w=(ws["tw"]["QZ"],))
            return ws

        wsets = [mk_set(f, "a"), mk_set(f2, "b")]
        self.NB_active = 2
        self.wb_next = 0
        w_in = self.din("a_w_in")
        alb = self.din("a_lower_bound")
        ang = self.din("a_norm_g")
        for h in range(HGH):
            hs = slice(h * 128, (h + 1) * 128)
            buf, tb = self.wload([(lambda b, p=p: b[:, :, p * 128:(p + 1) * 128],
                                   w_in[l, :, p * D + h * 128:p * D + (h + 1) * 128].rearrange("(kc p) n -> p kc n", p=128))
                                  for p in range(4)])
            S.dma("sp", ng, ang[l:l + 1, hs].partition_broadcast(128), w=(tv,))
            if l == 0:
                S.op("dve", lambda e: e.memset(lb, 0.0), w=(tv,))
            else:
                S.dma("sp", a0, alb[0:1, hs].partition_broadcast(128), w=(tv,))
                S.dma("sp", a1, alb[1:2, hs].partition_broadcast(128), w=(tv,))
                S.op("dve", lambda e: e.tensor_tensor(out=lb, in0=a0, in1=a1, op=ALU.subtract), r=(tv,), w=(tv,))
                S.op("act", lambda e: e.activation(out=lb, in_=lb, func=AF.Exp), r=(tv,), w=(tv,))
                S.op("dve", lambda e: e.tensor_scalar(out=lb, in0=lb, scalar1=1.0, scalar2=None, op0=ALU.add), r=(tv,), w=(tv,))
                S.op("dve", lambda e: e.reciprocal(out=lb, in_=lb), r=(tv,), w=(tv,))
            S.op("dve", lambda e: e.tensor_scalar(out=oml, in0=lb, scalar1=-1.0, scalar2=1.0, op0=ALU.mult, op1=ALU.add), r=(tv,), w=(tv,))
            def tile_body(i, ws, h=h, buf=buf, tb=tb):
                e1, t1, fg, kk, logf, eq, ek, ekl, ebT, sg, gg, ss = (ws[k] for k in ['e1', 't1', 'fg', 'kk', 'logf', 'eq', 'ek', 'ekl', 'ebT', 'sg', 'gg', 'ss'])
                qt, kt, kh, vv, QF, KTs, sTm, Sp0, Sp1, on, junk, QZ = (ws[k] for k in ['qt', 'kt', 'kh', 'vv', 'QF', 'KTs', 'sTm', 'Sp0', 'Sp1', 'on', 'junk', 'QZ'])
                tw = ws['tw']
                ts = slice(i * 128, (i + 1) * 128)
                pp, tpp = self.PSF[i % 2], self.tPSF[i % 2]
                for kc in range(NKC):
                    S.op("pe", lambda e, kc=kc, pp=pp, ts=ts, buf=buf: e.matmul(pp[:], lhsT=self.XT[:, kc, ts], rhs=buf[:, kc, :],
                                                                                 start=(kc == 0), stop=(kc == NKC - 1)),
                         r=(self.tXT[i], tb), w=(tpp,))
                if HGL < 1:
                    return
                pq, pf, pi_, pg = pp[:, 0:128], pp[:, 128:256], pp[:, 256:384], pp[:, 384:512]
                S.op("act", lambda e: e.activation(out=e1, in_=pf, func=AF.Exp, scale=-1.0), r=(tpp,), w=(tw["e1"],))
                S.op("dve", lambda e: e.tensor_scalar(out=e1, in0=e1, scalar1=1.0, scalar2=None, op0=ALU.add), r=(tw["e1"],), w=(tw["e1"],))
                S.op("dve", lambda e: e.reciprocal(out=e1, in_=e1), r=(tw["e1"],), w=(tw["e1"],))
                S.op("dve", lambda e: e.tensor_tensor(out=t1, in0=e1, in1=oml, op=ALU.mult), r=(tw["e1"], tv), w=(tw["t1"],))
                S.op("dve", lambda e: e.tensor_tensor(out=fg, in0=t1, in1=lb, op=ALU.add), r=(tw["t1"], tv), w=(tw["fg"],))
                S.op("dve", lambda e: e.tensor_scalar(out=fg, in0=fg, scalar1=1e-30, scalar2=None, op0=ALU.max), r=(tw["fg"],), w=(tw["fg"],))
                S.op("act", lambda e: e.activation(out=logf, in_=fg, func=AF.Ln), r=(tw["fg"],), w=(tw["logf"],))
                S.op("dve", lambda e: e.tensor_tensor(out=kk, in0=oml, in1=t1, op=ALU.subtract), r=(tw["t1"], tv), w=(tw["kk"],))
                if HGL < 2:
                    return
                pc, tpc = self.PSBv[0], self.tPSB[0]
                S.op("pe", lambda e: e.matmul(pc[:, 0:128], lhsT=cL1, rhs=logf, start=True, stop=True), r=(tc, tw["logf"]), w=(tpc,))
                if HGV >= 2:
                    S.op("pe", lambda e: e.matmul(pc[:, 128:256], lhsT=cL2, rhs=logf, start=True, stop=True), r=(tc, tw["logf"]), w=(tpc,))
                if HGV >= 3:
                    S.op("pe", lambda e: e.matmul(pc[:, 256:260], lhsT=logf, rhs=csel, start=True, stop=True), r=(tc, tw["logf"]), w=(tpc,))
                if HGL < 3 or os.environ.get('HGX') == '1':
                    return
                S.op("act", lambda e: e.activation(out=eq, in_=pc[:, 0:128], func=AF.Exp), r=(tpc,), w=(tw["eq"],))
                S.op("act", lambda e: e.activation(out=ek, in_=pc[:, 0:128], func=AF.Exp, scale=-1.0), r=(tpc,), w=(tw["ek"],))
                S.op("act", lambda e: e.activation(out=ekl, in_=pc[:, 128:256], func=AF.Exp), r=(tpc,), w=(tw["ekl"],))
                S.op("act", lambda e: e.activation(out=ebT, in_=pc[:, 256:260], func=AF.Exp), r=(tpc,), w=(tw["ebT"],))
                if os.environ.get('HGX') == '2':
                    return
                S.op("dve", lambda e: e.tensor_tensor(out=qt, in0=pq, in1=eq, op=ALU.mult), r=(tpp, tw["eq"]), w=(tw["qt"],))
                S.op("dve", lambda e: e.tensor_tensor(out=kt, in0=kk, in1=ek, op=ALU.mult), r=(tw["kk"], tw["ek"]), w=(tw["kt"],))
                S.op("dve", lambda e: e.tensor_tensor(out=kh, in0=kk, in1=ekl, op=ALU.mult), r=(tw["kk"], tw["ekl"]), w=(tw["kh"],))
                if os.environ.get('HGX') == '3':
                    return
                S.op("dve", lambda e: e.tensor_copy(out=vv, in_=pi_), r=(tpp,), w=(tw["vv"],))
                if os.environ.get('HGX') == '4':
                    return
                S.op("act", lambda e: e.activation(out=sg, in_=pg, func=AF.Exp, scale=-1.0), r=(tpp,), w=(tw["sg"],))
                S.op("dve", lambda e: e.tensor_scalar(out=sg, in0=sg, scalar1=1.0, scalar2=None, op0=ALU.add), r=(tw["sg"],), w=(tw["sg"],))
                S.op("dve", lambda e: e.reciprocal(out=sg, in_=sg), r=(tw["sg"],), w=(tw["sg"],))
                S.op("dve", lambda e: e.tensor_tensor(out=gg, in0=pg, in1=sg, op=ALU.mult), r=(tpp, tw["sg"]), w=(tw["gg"],))
                S.op("dve", lambda e: e.tensor_tensor(out=gg, in0=gg, in1=ng, op=ALU.mult), r=(tw["gg"], tv), w=(tw["gg"],))
                if HGL < 4:
                    return
                pt, tpt = self.PST[0], self.tPST[0]
                S.op("pe", lambda e: e.transpose(pt[:, 0:128], qt, self.c_identb[:]), r=(tw["qt"], self.tC), w=(tpt,))
                S.op("pe", lambda e: e.transpose(pt[:, 128:256], kt, self.c_identb[:]), r=(tw["kt"], self.tC), w=(tpt,))
                S.op("dve", lambda e: e.tensor_copy(out=QF, in_=pt[:, 0:128]), r=(tpt,), w=(tw["QF"],))
                S.op("dve", lambda e: e.tensor_copy(out=QZ[:, 0, 0:64], in_=pt[:, 0:64]), r=(tpt,), w=(tw["QZ"],))
                S.op("dve", lambda e: e.tensor_copy(out=QZ[:, 1, 64:128], in_=pt[:, 64:128]), r=(tpt,), w=(tw["QZ"],))
                S.op("dve", lambda e: e.tensor_copy(out=KTs, in_=pt[:, 128:256]), r=(tpt,), w=(tw["KTs"],))
                if HGL < 5:
                    return
                psc, tpsc = self.PSBv[1], self.tPSB[1]
                S.op("pe", lambda e: e.matmul(psc[:, 0:128], lhsT=KTs, rhs=QF, start=True, stop=True), r=(tw["KTs"], tw["QF"]), w=(tpsc,))
                S.op("dve", lambda e: e.tensor_tensor(out=sTm, in0=psc[:, 0:128], in1=cMT, op=ALU.mult), r=(tpsc, tc), w=(tw["sTm"],))
                if HGL < 6:
                    return
                pds, tpds = self.PSBv[2], self.tPSB[2]
                Sh = St[:, h, :]
                S.op("dve", lambda e: e.tensor_scalar(out=Sp0, in0=Sh, scalar1=ebT[:, 0:1], scalar2=None, op0=ALU.mult),
                     r=(tSt[h], tw["ebT"]), w=(tw["Sp0"],))
                S.op("pe", lambda e: e.matmul(pds[:, 0:128], lhsT=kh[0:64, :], rhs=vv[0:64, :], start=True, stop=True),
                     r=(tw["kh"], tw["vv"]), w=(tpds,))
                S.op("dve", lambda e: e.scalar_tensor_tensor(out=Sh, in0=Sh, scalar=ebT[:, 1:2], in1=pds[:, 0:128], op0=ALU.mult, op1=ALU.add),
                     r=(tSt[h], tw["ebT"], tpds), w=(tSt[h],))
                S.op("dve", lambda e: e.tensor_scalar(out=Sp1, in0=Sh, scalar1=ebT[:, 2:3], scalar2=None, op0=ALU.mult),
                     r=(tSt[h], tw["ebT"]), w=(tw["Sp1"],))
                S.op("pe", lambda e: e.matmul(pds[:, 128:256], lhsT=kh[64:128, :], rhs=vv[64:128, :], start=True, stop=True),
                     r=(tw["kh"], tw["vv"]), w=(tpds,))
                S.op("dve", lambda e: e.scalar_tensor_tensor(out=Sh, in0=Sh, scalar=ebT[:, 3:4], in1=pds[:, 128:256], op0=ALU.mult, op1=ALU.add),
                     r=(tSt[h], tw["ebT"], tpds), w=(tSt[h],))
                if HGL < 7:
                    return
                po, tpo = self.PSBv[3], self.tPSB[3]
                S.op("pe", lambda e: e.matmul(po[:, 0:128], lhsT=sTm, rhs=vv, start=True, stop=False), r=(tw["sTm"], tw["vv"]), w=(tpo,))
                S.op("pe", lambda e: e.matmul(po[:, 0:128], lhsT=QZ[:, 0, :], rhs=Sp0, start=False, stop=False), r=(tw["QZ"], tw["Sp0"]), w=(tpo,))
                S.op("pe", lambda e: e.matmul(po[:, 0:128], lhsT=QZ[:, 1, :], rhs=Sp1, start=False, stop=True), r=(tw["QZ"], tw["Sp1"]), w=(tpo,))
                if HGL < 8:
                    return
                S.op("act", lambda e: e.activation(out=junk, in_=po[:, 0:128], func=AF.Square, accum_out=ss[:, 0:1]), r=(tpo,), w=(tw["ss"], tw["junk"]))
                self.rsqrt_small(ss[:, 1:2], ss[:, 0:1], 1.0 / 128, RMS_EPS, (tw["ss"],), (tw["ss"],))
                S.op("dve", lambda e: e.scalar_tensor_tensor(out=on, in0=po[:, 0:128], scalar=ss[:, 1:2], in1=gg, op0=ALU.mult, op1=ALU.mult),
                     r=(tpo, tw["ss"], tw["gg"]), w=(tw["on"],))
                if HGL < 9:
                    return
                pt1, tpt1 = self.PST[1], self.tPST[1]
                S.op("pe", lambda e: e.transpose(pt1[:, 0:128], on, self.c_identb[:]), r=(tw["on"], self.tC), w=(tpt1,))
                S.op("dve", lambda e, h=h, ts=ts: e.tensor_copy(out=self.OT[:, h, ts], in_=pt1[:, 0:128]), r=(tpt1,), w=(self.tOT[i],))

            LAG = 30
            recs = []
            for i in range(NT):
                rec = []
                real_op = S.op
                S.op = lambda e, fn, r=(), w=(), rec=rec: rec.append((e, fn, tuple(r), tuple(w)))
                try:
                    tile_body(i, wsets[i % 2])
                finally:
                    S.op = real_op
                recs.append(rec)
            active = []
            nxt = 0
            while nxt < NT or active:
                if nxt < NT and len(active) < 2 and (not active or active[-1][1] >= min(LAG, len(active[-1][0]))):
                    active.append([recs[nxt], 0])
                    nxt += 1
                for a in list(active):
                    if a[1] < len(a[0]):
                        e_, fn_, r_, w_ = a[0][a[1]]
                        S.op(e_, fn_, r=r_, w=w_)
                        a[1] += 1
                active = [a for a in active if a[1] < len(a[0])]
        S.dma("sp", self.d_state[l].rearrange("p (h v) -> p h v", h=16), St, r=tuple(tSt))
        self.barrier()
        self.NB_active = 3
        self.wb_next = 0

    def moe(self, L):
        S = self.S
        self.barrier()
        X1b = self.OT[:].rearrange("p k t -> p (k t)").rearrange("p (i d) -> p i d", i=NT)
        tX1b = [T(f"x1b{i}") for i in range(NT)]
        off = [0]

        def f(cols):
            o = off[0]
            off[0] += cols
            assert off[0] <= 8192, off[0]
            return self.XTraw[:, o:o + cols]

        def fb(cols):
            return f(cols // 2).bitcast(BF16)

        wr = f(NKC * 36).rearrange("p (k n) -> p k n", k=NKC)
        rb = f(36)
        ustrict, ones, iota = f(128), f(128), f(CAP)
        xT4 = [f(512), f(512)]
        A = f(NT * 32).rearrange("p (i e) -> p i e", i=NT)
        Gt = f(NT * 32).rearrange("p (i e) -> p i e", i=NT)
        VAL = f(NT * 32).rearrange("p (i e) -> p i e", i=NT)
        lg, gsel, pen, elm, elm2, oh1, oh2 = f(36), f(4), f(4), f(32), f(32), f(32), f(32)
        sm = f(16)
        junk4 = f(4)
        aoff = [3456]

        def fb2(cols):
            o = aoff[0]
            aoff[0] += cols // 2
            assert aoff[0] <= self.ARN
            return self.AR[:, o:o + cols // 2].bitcast(BF16)

        sets = []
        for fbx, sfx in ((fb, "a"), (fb2, "b")):
            sets.append(dict(
                P=fbx(NT * CAP).rearrange("p (i c) -> p i c", i=NT),
                Pg=fbx(NT * CAP).rearrange("p (i c) -> p i c", i=NT),
                PgT=fbx(NT * 128).rearrange("p (i t) -> p i t", i=NT),
                XS=fbx(NKC * CAP).rearrange("p (k c) -> p k c", k=NKC),
                tP=[T("P" + sfx + str(i)) for i in range(NT)], tPg=[T("Pg" + sfx + str(i)) for i in range(NT)],
                tPgT=T("PgT" + sfx), tXS=[T("XS" + sfx + str(g)) for g in range(4)]))
        HT = fb(4 * CAP).rearrange("p (k c) -> p k c", k=4)
        hsg = f(4 * CAP)
        Y = fb(D)
        tc_ = T("mc")
        tr = T("mrt")
        tA = [T(f"mA{i}") for i in range(NT)]
        txT4 = [T("xT4a"), T("xT4b")]
        tHT, thsg = T("HT"), T("hsg")
        tY = [T(f"Y{k}") for k in range(4)]

        S.dma("sp", wr[:, :, 0:4], self.din("moe_w_rg")[L].rearrange("(k p) n -> p k n", p=128), w=(tc_,))
        S.dma("sp", wr[:, :, 4:36], self.din("moe_w_re")[L].rearrange("(k p) n -> p k n", p=128), w=(tc_,))
        S.dma("sp", rb[:, 0:4], self.din("moe_b_rg")[L:L + 1, :].partition_broadcast(128), w=(tc_,))
        S.dma("sp", rb[:, 4:36], self.din("moe_b_re")[L:L + 1, :].partition_broadcast(128), w=(tc_,))
        S.dma("sp", ustrict, self.din("c_ustrict")[:, :], w=(tc_,))
        S.dma("sp", ones, self.din("c_ones")[:, :], w=(tc_,))
        S.dma("sp", iota, self.din("c_iota")[:, :], w=(tc_,))

        for i in range(NT):
            if i % 2:
                S.op("act", lambda e, i=i: e.copy(out=X1b[:, i, :], in_=self.X[:, i, :]), r=(self.tX[i],), w=(tX1b[i],))
            else:
                S.op("dve", lambda e, i=i: e.tensor_copy(out=X1b[:, i, :], in_=self.X[:, i, :]), r=(self.tX[i],), w=(tX1b[i],))

        n = 0
        plg, tplg = self.PSBv[0], self.tPSB[0]
        for i in range(NT):
            for g in range(4):
                pb = n % 2
                n += 1
                ps, tps = self.PSF[pb], self.tPSF[pb]
                for j in range(4):
                    kc = g * 4 + j
                    S.op("pe", lambda e, kc=kc, j=j, ps=ps, i=i: e.transpose(
                        ps[:, j * 128:(j + 1) * 128], self.X[:, i, kc * 128:(kc + 1) * 128], self.c_ident[:]),
                        r=(self.tX[i], self.tC), w=(tps,))
                xt, txt = xT4[pb], txT4[pb]
                if pb:
                    S.op("act", lambda e, xt=xt, ps=ps: e.copy(out=xt, in_=ps[:]), r=(tps,), w=(txt,))
                else:
                    S.op("dve", lambda e, xt=xt, ps=ps: e.tensor_copy(out=xt, in_=ps[:]), r=(tps,), w=(txt,))
                for j in range(4):
                    kc = g * 4 + j
                    S.op("pe", lambda e, kc=kc, j=j, xt=xt: e.matmul(plg[:, 0:36], lhsT=xt[:, j * 128:(j + 1) * 128], rhs=wr[:, kc, :],
                                                                      start=(kc == 0), stop=(kc == NKC - 1)),
                         r=(txt, tc_), w=(tplg,))
            dv = lambda fn, r, w: S.op("dve", fn, r=r, w=w)
            dv(lambda e: e.tensor_tensor(out=lg, in0=plg[:, 0:36], in1=rb, op=ALU.add), (tplg, tc_), (tr,))
            gl, el = lg[:, 0:4], lg[:, 4:36]
            gmax, ngmax, sume, pgrp, m1, m2, dd, g1, g2 = (sm[:, k:k + 1] for k in range(9))
            dv(lambda e: e.tensor_reduce(out=gmax, in_=gl, axis=AX.X, op=ALU.max), (tr,), (tr,))
            dv(lambda e: e.tensor_scalar(out=gsel, in0=gl, scalar1=gmax, scalar2=None, op0=ALU.is_equal), (tr,), (tr,))
            dv(lambda e: e.tensor_scalar(out=ngmax, in0=gmax, scalar1=-1.0, scalar2=None, op0=ALU.mult), (tr,), (tr,))
            S.op("act", lambda e: e.activation(out=junk4, in_=gl, func=AF.Exp, bias=ngmax, scale=1.0, accum_out=sume), r=(tr,), w=(tr,))
            dv(lambda e: e.reciprocal(out=pgrp, in_=sume), (tr,), (tr,))
            dv(lambda e: e.tensor_scalar(out=pen, in0=gsel, scalar1=-1.0, scalar2=1e30, op0=ALU.add, op1=ALU.mult), (tr,), (tr,))
            for g in range(4):
                dv(lambda e, g=g: e.tensor_scalar(out=elm[:, g * 8:(g + 1) * 8], in0=el[:, g * 8:(g + 1) * 8], scalar1=pen[:, g:g + 1],
                                                  scalar2=None, op0=ALU.add), (tr,), (tr,))
            dv(lambda e: e.tensor_reduce(out=m1, in_=elm, axis=AX.X, op=ALU.max), (tr,), (tr,))
            dv(lambda e: e.tensor_scalar(out=oh1, in0=elm, scalar1=m1, scalar2=None, op0=ALU.is_equal), (tr,), (tr,))
            dv(lambda e: e.scalar_tensor_tensor(out=elm2, in0=oh1, scalar=-1e30, in1=elm, op0=ALU.mult, op1=ALU.add), (tr,), (tr,))
            dv(lambda e: e.tensor_reduce(out=m2, in_=elm2, axis=AX.X, op=ALU.max), (tr,), (tr,))
            dv(lambda e: e.tensor_scalar(out=oh2, in0=elm2, scalar1=m2, scalar2=None, op0=ALU.is_equal), (tr,), (tr,))
            dv(lambda e: e.tensor_tensor(out=dd, in0=m2, in1=m1, op=ALU.subtract), (tr,), (tr,))
            S.op("act", lambda e: e.activation(out=dd, in_=dd, func=AF.Exp), r=(tr,), w=(tr,))
            dv(lambda e: e.tensor_scalar(out=g1, in0=dd, scalar1=1.0, scalar2=None, op0=ALU.add), (tr,), (tr,))
            dv(lambda e: e.reciprocal(out=g1, in_=g1), (tr,), (tr,))
            dv(lambda e: e.tensor_tensor(out=g2, in0=dd, in1=g1, op=ALU.mult), (tr,), (tr,))
            dv(lambda e: e.tensor_tensor(out=g1, in0=g1, in1=pgrp, op=ALU.mult), (tr,), (tr,))
            dv(lambda e: e.tensor_tensor(out=g2, in0=g2, in1=pgrp, op=ALU.mult), (tr,), (tr,))
            dv(lambda e, i=i: e.tensor_tensor(out=A[:, i, :], in0=oh1, in1=oh2, op=ALU.add), (tr,), (tA[i],))
            dv(lambda e, i=i: e.tensor_scalar(out=Gt[:, i, :], in0=oh1, scalar1=g1, scalar2=None, op0=ALU.mult), (tr,), (tA[i],))
            dv(lambda e, i=i: e.scalar_tensor_tensor(out=Gt[:, i, :], in0=oh2, scalar=g2, in1=Gt[:, i, :], op0=ALU.mult, op1=ALU.add),
               (tr, tA[i]), (tA[i],))
        pr, tpr = self.PSBv[1], self.tPSB[1]
        for i in range(NT):
            for j in range(i):
                S.op("pe", lambda e, j=j: e.matmul(pr[:, 0:32], lhsT=ones, rhs=A[:, j, :], start=(j == 0), stop=False), r=(tc_, tA[j]), w=(tpr,))
            S.op("pe", lambda e, i=i: e.matmul(pr[:, 0:32], lhsT=ustrict, rhs=A[:, i, :], start=(i == 0), stop=True), r=(tc_, tA[i]), w=(tpr,))
            S.op("dve", lambda e, i=i: e.scalar_tensor_tensor(out=VAL[:, i, :], in0=pr[:, 0:32], scalar=1.0, in1=A[:, i, :], op0=ALU.add, op1=ALU.mult),
                 r=(tpr, tA[i]), w=(tA[i],))
            S.op("dve", lambda e, i=i: e.tensor_scalar(out=VAL[:, i, :], in0=VAL[:, i, :], scalar1=-1.0, scalar2=None, op0=ALU.add),
                 r=(tA[i],), w=(tA[i],))
        for i in range(NT):
            if i % 2:
                S.op("act", lambda e, i=i: e.mul(out=self.X[:, i, :], in_=self.X[:, i, :], mul=ALPHA), r=(self.tX[i],), w=(self.tX[i],))
            else:
                S.op("dve", lambda e, i=i: e.tensor_scalar(out=self.X[:, i, :], in0=self.X[:, i, :], scalar1=ALPHA, scalar2=None, op0=ALU.mult),
                     r=(self.tX[i],), w=(self.tX[i],))
        wg_d, wu_d, wd_d = self.din("moe_w_gate"), self.din("moe_w_up"), self.din("moe_w_down")
        wts = {}

        def loads(ex):
            wg, twg = self.wload([(lambda b: b[:], wg_d[L, ex].rearrange("(kc p) n -> p kc n", p=128))])
            wu, twu = self.wload([(lambda b: b[:], wu_d[L, ex].rearrange("(kc p) n -> p kc n", p=128))])
            wdv = lambda b: b[:].rearrange("p k n -> p (k n)").rearrange("p (fc d) -> p fc d", fc=4)
            wd_, twd = self.wload([(wdv, wd_d[L, ex].rearrange("(fc p) d -> p fc d", p=128))])
            wts[ex] = (wg, twg, wu, twu, wdv(wd_), twd)

        def front(ex):
            st = sets[ex % 2]
            P, Pg, PgT, XS = st["P"], st["Pg"], st["PgT"], st["XS"]
            for i in range(NT):
                S.op("dve", lambda e, i=i: e.tensor_scalar(out=P[:, i, :], in0=iota, scalar1=VAL[:, i, ex:ex + 1], scalar2=None, op0=ALU.is_equal),
                     r=(tc_, tA[i]), w=(st["tP"][i],))
                S.op("dve", lambda e, i=i: e.tensor_scalar(out=Pg[:, i, :], in0=iota, scalar1=VAL[:, i, ex:ex + 1], scalar2=Gt[:, i, ex:ex + 1],
                                                           op0=ALU.is_equal, op1=ALU.mult), r=(tc_, tA[i]), w=(st["tPg"][i],))
            pt, tpt = self.PST[0], self.tPST[0]
            for i in range(NT):
                S.op("pe", lambda e, i=i: e.transpose(pt[:, i * 128:(i + 1) * 128], Pg[:, i, :], self.c_identb[:]), r=(st["tPg"][i], self.tC), w=(tpt,))
            S.op("act", lambda e: e.copy(out=PgT, in_=pt[:].rearrange("p (i t) -> p i t", i=NT)), r=(tpt,), w=(st["tPgT"],))
            for g in range(4):
                ps, tps = self.PSF[g % 2], self.tPSF[g % 2]
                for j in range(4):
                    kc = g * 4 + j
                    for i in range(NT):
                        S.op("pe", lambda e, kc=kc, j=j, i=i, ps=ps: e.matmul(ps[:, j * CAP:(j + 1) * CAP], lhsT=X1b[:, i, kc * 128:(kc + 1) * 128],
                                                                               rhs=P[:, i, :], start=(i == 0), stop=(i == NT - 1)),
                             r=(tX1b[i], st["tP"][i]), w=(tps,))
                dst = XS[:, g * 4:(g + 1) * 4, :]
                src = ps[:, 0:4 * CAP].rearrange("p (j c) -> p j c", j=4)
                if g % 2:
                    S.op("act", lambda e, dst=dst, src=src: e.copy(out=dst, in_=src), r=(tps,), w=(st["tXS"][g],))
                else:
                    S.op("dve", lambda e, dst=dst, src=src: e.tensor_copy(out=dst, in_=src), r=(tps,), w=(st["tXS"][g],))

        def back(ex):
            st = sets[ex % 2]
            PgT, XS = st["PgT"], st["XS"]
            wg, twg, wu, twu, wd, twd = wts[ex]
            phg, tphg = self.PSBv[0], self.tPSB[0]
            phu, tphu = self.PSBv[1], self.tPSB[1]
            for (pw, tpw, wt, twt) in ((phg, tphg, wg, twg), (phu, tphu, wu, twu)):
                for fc in range(4):
                    for kc in range(NKC):
                        S.op("pe", lambda e, fc=fc, kc=kc, pw=pw, wt=wt: e.matmul(pw[:, fc * CAP:(fc + 1) * CAP], lhsT=wt[:, kc, fc * 128:(fc + 1) * 128],
                                                                                   rhs=XS[:, kc, :], start=(kc == 0), stop=(kc == NKC - 1)),
                             r=(twt, st["tXS"][kc // 4]), w=(tpw,))
            S.op("act", lambda e: e.activation(out=hsg, in_=phg[:, 0:4 * CAP], func=AF.Exp, scale=-1.0), r=(tphg,), w=(thsg,))
            S.op("dve", lambda e: e.tensor_scalar(out=hsg, in0=hsg, scalar1=1.0, scalar2=None, op0=ALU.add), r=(thsg,), w=(thsg,))
            S.op("dve", lambda e: e.reciprocal(out=hsg, in_=hsg), r=(thsg,), w=(thsg,))
            S.op("dve", lambda e: e.tensor_tensor(out=hsg, in0=hsg, in1=phg[:, 0:4 * CAP], op=ALU.mult), r=(thsg, tphg), w=(thsg,))
            S.op("dve", lambda e: e.tensor_tensor(out=HT[:].rearrange("p k c -> p (k c)"), in0=hsg, in1=phu[:, 0:4 * CAP], op=ALU.mult),
                 r=(thsg, tphu), w=(tHT,))
            for nn in range(4):
                py, tpy = self.PSBv[nn], self.tPSB[nn]
                for fc in range(4):
                    S.op("pe", lambda e, fc=fc, nn=nn, py=py: e.matmul(py[0:CAP, :], lhsT=HT[:, fc, :], rhs=wd[:, fc, nn * 512:(nn + 1) * 512],
                                                                        start=(fc == 0), stop=(fc == 3)), r=(tHT, twd), w=(tpy,))
                if nn % 2:
                    S.op("act", lambda e, nn=nn, py=py: e.copy(out=Y[0:CAP, nn * 512:(nn + 1) * 512], in_=py[0:CAP, :]), r=(tpy,), w=(tY[nn],))
                else:
                    S.op("dve", lambda e, nn=nn, py=py: e.tensor_copy(out=Y[0:CAP, nn * 512:(nn + 1) * 512], in_=py[0:CAP, :]), r=(tpy,), w=(tY[nn],))
            ncp = 0
            for i in range(NT):
                for nn in range(4):
                    pcb, tpcb = self.PSBv[ncp % 4], self.tPSB[ncp % 4]
                    ncp += 1
                    S.op("pe", lambda e, i=i, nn=nn, pcb=pcb: e.matmul(pcb[:], lhsT=PgT[0:CAP, i, :], rhs=Y[0:CAP, nn * 512:(nn + 1) * 512], start=True, stop=True),
                         r=(st["tPgT"], tY[nn]), w=(tpcb,))
                    xs = self.X[:, i, nn * 512:(nn + 1) * 512]
                    S.op("dve", lambda e, xs=xs, pcb=pcb: e.tensor_tensor(out=xs, in0=xs, in1=pcb[:], op=ALU.add), r=(tpcb, tXq[i][nn]), w=(tXq[i][nn],))

        self.barrier()
        tXq = [[T(f"Xq{i}_{nn}") for nn in range(4)] for i in range(NT)]
        recF, recB, recL = [], [], []
        for ex in range(NE):
            S.rec = []
            loads(ex)
            recL.append(S.rec)
            assert len(S.rec) == 3
            S.rec = []
            front(ex)
            recF.append(S.rec)
            S.rec = []
            back(ex)
            recB.append(S.rec)
            S.rec = None
        for ent in recL[0] + recF[0]:
            S.emit(ent)
        for ex in range(NE):
            b = recB[ex]
            fnx = recF[ex + 1] if ex + 1 < NE else []
            lnx = recL[ex + 1] if ex + 1 < NE else []
            nb_, nf_ = len(b), len(fnx)
            fi = 0
            for bi, ent in enumerate(b):
                S.emit(ent)
                if lnx and bi == 63:
                    S.emit(lnx[0])
                if lnx and bi == 127:
                    S.emit(lnx[1])
                if lnx and bi == 127 + 5 + 20:
                    S.emit(lnx[2])
                tgt = ((bi + 1) * nf_) // nb_
                while fi < tgt:
                    S.emit(fnx[fi])
                    fi += 1
            while fi < nf_:
                S.emit(fnx[fi])
                fi += 1
        self.barrier()

    def kv_alloc(self):
        self.KT2 = self.arb(4 * 1152).rearrange("p (h t) -> p h t", h=4)
        self.V = self.arb(9 * 256).rearrange("p (b c) -> p b c", b=9)
        self.tKV = T("KV")
        self.att_base = self.ar_off

    def save_halo(self):
        self.S.dma("sp", self.d_halo[:, :], self.X[:, NT - 1, :], r=(self.tX[NT - 1],))

    def kv_compute(self):
        S = self.S
        self.ar_off = self.att_base
        Xh = self.arf(D)
        XTh = self.arb(NKC * 128).rearrange("p (k t) -> p k t", k=NKC)
        tXh, tXTh = T("Xh"), T("XTh")
        S.dma("sp", Xh, self.d_halo[:, :], w=(tXh,))
        for g in range(4):
            ps, tps = self.PSF[g % 2], self.tPSF[g % 2]
            for j in range(4):
                kc = g * 4 + j
                S.op("pe", lambda e, kc=kc, j=j, ps=ps: e.transpose(ps[:, j * 128:(j + 1) * 128], Xh[:, kc * 128:(kc + 1) * 128], self.c_ident[:]),
                     r=(tXh, self.tC), w=(tps,))
            S.op("dve", lambda e, g=g, ps=ps: e.tensor_copy(out=XTh[:, g * 4:(g + 1) * 4, :], in_=ps[:].rearrange("p (j t) -> p j t", j=4)),
                 r=(tps,), w=(tXTh,))
        wkv = self.din("b_w_kv")
        bufv, tbv = self.wload([(lambda b: b[:], wkv.rearrange("(kc p) n -> p kc n", p=128))])
        wkd_v = lambda b: b[:].rearrange("p k (h f) -> p k h f", h=4)
        pieces = []
        for hk in range(4):
            for dup in range(2):
                pieces.append((lambda b, hk=hk, dup=dup: wkd_v(b)[:, :, hk, dup * 64:(dup + 1) * 64],
                               wkv[:, hk * 64:(hk + 1) * 64].rearrange("(kc p) n -> p kc n", p=128)))
        bufk_, tbk = self.wload(pieces)
        bufk = wkd_v(bufk_)
        n = 0
        for b in range(9):
            ps, tps = self.PSF[n % 2], self.tPSF[n % 2]
            n += 1
            for kc in range(NKC):
                lhsT = XTh[:, kc, :] if b == 0 else self.XT[:, kc, (b - 1) * 128:b * 128]
                rt = (tXTh,) if b == 0 else (self.tXT[b - 1],)
                S.op("pe", lambda e, kc=kc, ps=ps, lhsT=lhsT: e.matmul(ps[:, 0:256], lhsT=lhsT, rhs=bufv[:, kc, 256:512], start=(kc == 0), stop=(kc == NKC - 1)),
                     r=rt + (tbv,), w=(tps,))
            S.op("dve", lambda e, b=b, ps=ps: e.tensor_copy(out=self.V[:, b, :], in_=ps[:, 0:256]), r=(tps,), w=(self.tKV,))
        for hk in range(4):
            for part in range(3):
                ps, tps = self.PSF[n % 2], self.tPSF[n % 2]
                n += 1
                if part == 0:
                    rhs_fn, ncol, rt, dst = (lambda kc: XTh[:, kc, :]), 128, (tXTh,), self.KT2[:, hk, 0:128]
                else:
                    lo = (part - 1) * 512
                    rhs_fn, ncol, rt = (lambda kc, lo=lo: self.XT[:, kc, lo:lo + 512]), 512, tuple(self.tXT[(part - 1) * 4:(part - 1) * 4 + 4])
                    dst = self.KT2[:, hk, 128 + lo:128 + lo + 512]
                for kc in range(NKC):
                    S.op("pe", lambda e, kc=kc, ps=ps, hk=hk, rhs_fn=rhs_fn, ncol=ncol: e.matmul(ps[:, 0:ncol], lhsT=bufk[:, kc, hk, :], rhs=rhs_fn(kc),
                                                                                                 start=(kc == 0), stop=(kc == NKC - 1)),
                         r=rt + (tbk,), w=(tps,))
                S.op("act", lambda e, ps=ps, ncol=ncol, dst=dst: e.copy(out=dst, in_=ps[:, 0:ncol]), r=(tps,), w=(self.tKV,))
        self.barrier()

    def attention(self, j_layer):
        S = self.S
        self.barrier()
        self.ar_off = self.att_base
        f = self.arf
        biasm = f(1024).rearrange("p (h k) -> p h k", h=4)
        PB = self.arb(1024).rearrange("p (h k) -> p h k", h=4)
        PTs = self.arb(1024).rearrange("p (a q) -> p a q", a=8)
        QTg = self.arb(2048).rearrange("p (c t) -> p c t", c=2)
        OB = self.arb(256)
        SS2 = f(512).rearrange("p (h k) -> p h k", h=2)
        tSS = T("SS2")
        sm = f(32)
        sink, rm, negm, rs, es, rinv = (sm[:, 4 * k:4 * k + 4] for k in range(6))
        tb, tPB, tPTs, tQ, tOB, tsm = T("biasm"), T("PB"), T("PTs"), T("QTg"), T("OB"), T("asm")
        wq = self.din("b_w_q")
        btab = self.din("bias_tab")
        sinks = self.din("b_sinks")
        PSS3 = self.PSS[:].rearrange("p (h k) -> p h k", h=4)
        tS = (self.tPSB[0], self.tPSB[1])
        n = 0
        for hk in range(4):
            bufq, tbq = self.wload([(lambda b: b[:], wq[j_layer, :, hk * 512:(hk + 1) * 512].rearrange("(kc p) n -> p kc n", p=128))])
            for hh in range(2):
                h0 = 8 * hk + 4 * hh
                c0 = h0 // 2
                for cc in range(2):
                    for half in range(2):
                        ps, tps = self.PSF[n % 2], self.tPSF[n % 2]
                        n += 1
                        for kc in range(NKC):
                            S.op("pe", lambda e, kc=kc, ps=ps, cc=cc, half=half, hh=hh, bufq=bufq: e.matmul(
                                ps[:], lhsT=bufq[:, kc, hh * 256 + cc * 128:hh * 256 + (cc + 1) * 128], rhs=self.XT[:, kc, half * 512:(half + 1) * 512],
                                start=(kc == 0), stop=(kc == NKC - 1)), r=tuple(self.tXT[half * 4:half * 4 + 4]) + (tbq,), w=(tps,))
                        S.op("act", lambda e, ps=ps, cc=cc, half=half: e.copy(out=QTg[:, cc, half * 512:(half + 1) * 512], in_=ps[:]), r=(tps,), w=(tQ,))
                pos = lambda j: (j % 2) * 2 + j // 2
                for j in range(4):
                    S.dma("sp", biasm[:, pos(j), :], btab[h0 + j], w=(tb,))
                    S.dma("sp", sink[:, pos(j):pos(j) + 1], sinks[j_layer:j_layer + 1, h0 + j:h0 + j + 1].partition_broadcast(128), w=(tsm,))
                for j in range(4):
                    S.op("dve", lambda e, j=j: e.tensor_tensor(out=biasm[:, j, :], in0=biasm[:, j, :], in1=self.c_maskc[:], op=ALU.add), r=(tb, self.tC), w=(tb,))
                for nb in range(NT):
                    if os.environ.get("ATT_STOP") == "q":
                        continue
                    qs = slice(nb * 128, (nb + 1) * 128)
                    for j in range(4):
                        p, cc = j % 2, j // 2
                        S.op("pe", lambda e, j=j, p=p, cc=cc, qs=qs, nb=nb, hk=hk: e.matmul(
                            self.PSS[:, pos(j) * 256:(pos(j) + 1) * 256], lhsT=QTg[p * 64:(p + 1) * 64, cc, qs],
                            rhs=self.KT2[p * 64:(p + 1) * 64, hk, nb * 128:nb * 128 + 256], start=True, stop=True),
                            r=(tQ, self.tKV), w=(self.tPSB[j % 2],))
                    for bk in range(2):
                        j0 = 2 * bk
                        pbank = self.PSS[:, bk * 512:(bk + 1) * 512].rearrange("p (h k) -> p h k", h=2)
                        S.op("dve", lambda e, pbank=pbank, j0=j0: e.scalar_tensor_tensor(out=SS2, in0=pbank, scalar=0.125, in1=biasm[:, j0:j0 + 2, :],
                                                                                         op0=ALU.mult, op1=ALU.add), r=(tb, self.tPSB[bk]), w=(tSS,))
                        if nb == 0:
                            for jj in range(2):
                                S.op("dve", lambda e, jj=jj: e.tensor_tensor(out=SS2[:, jj, 0:128], in0=SS2[:, jj, 0:128], in1=self.c_halo[:], op=ALU.add),
                                     r=(self.tC, tSS), w=(tSS,))
                        S.op("dve", lambda e, j0=j0: e.tensor_reduce(out=rm[:, j0:j0 + 2], in_=SS2, axis=AX.X, op=ALU.max), r=(tSS,), w=(tsm,))
                        S.op("dve", lambda e, j0=j0: e.tensor_tensor(out=rm[:, j0:j0 + 2], in0=rm[:, j0:j0 + 2], in1=sink[:, j0:j0 + 2], op=ALU.max), r=(tsm,), w=(tsm,))
                        S.op("dve", lambda e, j0=j0: e.tensor_scalar(out=negm[:, j0:j0 + 2], in0=rm[:, j0:j0 + 2], scalar1=-1.0, scalar2=None, op0=ALU.mult), r=(tsm,), w=(tsm,))
                        for jj in range(2):
                            j = j0 + jj
                            S.op("act", lambda e, j=j, jj=jj: e.activation(out=PB[:, j, :], in_=SS2[:, jj, :], func=AF.Exp,
                                                                            bias=negm[:, j:j + 1], scale=1.0, accum_out=rs[:, j:j + 1]),
                                 r=(tSS, tsm), w=(tPB, tsm))
                    S.op("dve", lambda e: e.tensor_tensor(out=es, in0=sink, in1=negm, op=ALU.add), r=(tsm,), w=(tsm,))
                    S.op("act", lambda e: e.activation(out=es, in_=es, func=AF.Exp), r=(tsm,), w=(tsm,))
                    S.op("dve", lambda e: e.tensor_tensor(out=rinv, in0=rs, in1=es, op=ALU.add), r=(tsm,), w=(tsm,))
                    S.op("dve", lambda e: e.reciprocal(out=rinv, in_=rinv), r=(tsm,), w=(tsm,))
                    pt, tpt = self.PST[0], self.tPST[0]
                    for j in range(4):
                        for kb in range(2):
                            a = j * 2 + kb
                            S.op("pe", lambda e, j=j, kb=kb, a=a: e.transpose(pt[:, a * 128:(a + 1) * 128], PB[:, j, kb * 128:(kb + 1) * 128], self.c_identb[:]),
                                 r=(tPB, self.tC), w=(tpt,))
                    S.op("act", lambda e: e.copy(out=PTs, in_=pt[:].rearrange("p (a q) -> p a q", a=8)), r=(tpt,), w=(tPTs,))
                    po, tpo = self.PSBv[2], self.tPSB[2]
                    for j in range(4):
                        for kb in range(2):
                            S.op("pe", lambda e, j=j, kb=kb, nb=nb, hk=hk: e.matmul(po[:, j * 64:(j + 1) * 64], lhsT=PTs[:, pos(j) * 2 + kb, :],
                                                                                    rhs=self.V[:, nb + kb, hk * 64:(hk + 1) * 64],
                                                                                    start=(kb == 0), stop=(kb == 1)),
                                 r=(tPTs, self.tKV), w=(tpo,))
                    for j in range(4):
                        S.op("dve", lambda e, j=j: e.tensor_scalar(out=OB[:, j * 64:(j + 1) * 64], in0=po[:, j * 64:(j + 1) * 64], scalar1=rinv[:, pos(j):pos(j) + 1],
                                                                   scalar2=None, op0=ALU.mult), r=(tpo, tsm), w=(tOB,))
                    pt1, tpt1 = self.PST[1], self.tPST[1]
                    for cc in range(2):
                        S.op("pe", lambda e, cc=cc: e.transpose(pt1[:, cc * 128:(cc + 1) * 128], OB[:, cc * 128:(cc + 1) * 128], self.c_identb[:]),
                             r=(tOB, self.tC), w=(tpt1,))
                    S.op("dve", lambda e, c0=c0, qs=qs: e.tensor_copy(out=self.OT[:, c0:c0 + 2, qs], in_=pt1[:, 0:256].rearrange("p (c q) -> p c q", c=2)),
                         r=(tpt1,), w=(self.tOT[nb],))
        self.barrier()

    def _program(self):
        S = self.S
        self.c_eps = {}
        for eps in (LN_EPS, RMS_EPS):
            t = self.sb(f"eps{len(self.c_eps)}", [128, 1])
            self.c_eps[eps] = t
            S.op("dve", lambda e, t=t, eps=eps: e.memset(t[:], eps), w=(self.tC,))
        self.load_consts()
        stop = self.debug_stop
        if stop is not None and stop != "hg1":
            self.load_x(self.din("xm"))
        if stop in ("xt", "xt0"):
            self.make_xt()
        if stop == "xt":
            self.layernorm(0)
            self.store_out()
            return
        if stop == "hg1":
            self.load_x(self.din("xp"))
            self.make_xt()
            self.hgrn(1, "zero")
            self.barrier()
            self.load_x(self.din("xm"))
            self.make_xt()
            self.hgrn(1, "load")
            self.barrier()
            self.out_proj(self.din("a_w_out")[1])
            self.layernorm(2)
            self.store_out()
            return
        if stop == "att":
            self.phase()
            self.kv_alloc()
            self.load_x(self.din("xp"))
            self.save_halo()
            self.barrier()
            self.load_x(self.din("xm"))
            self.make_xt()
            self.kv_compute()
            if os.environ.get("ATT_STOP") == "kv":
                self.store_out()
                return
            self.attention(0)
            if os.environ.get("ATT_STOP"):
                self.store_out()
                return
            self.out_proj(self.din("b_w_out")[0])
            self.layernorm(4)
            self.store_out()
            return
        if stop == "moe":
            self.moe(0)
            self.layernorm(1)
            self.store_out()
            return
        if stop == "xt0":
            self.barrier()
            self.store_out()
            return
        def hgrn_layer(l, mode):
            self.make_xt()
            self.hgrn(l, mode)
            self.barrier()
            self.out_proj(self.din("a_w_out")[l])
            self.layernorm(2 * l)
            self.moe(l)
            self.layernorm(2 * l + 1)

        self.load_x(self.din("xp"))
        hgrn_layer(0, "zero")
        hgrn_layer(1, "zero")
        self.save_halo()
        self.barrier()
        self.load_x(self.din("xm"))
        hgrn_layer(0, "load")
        hgrn_layer(1, "load")
        self.make_xt()
        self.phase()
        self.kv_alloc()
        self.kv_compute()
        for j in range(2):
            if j:
                self.make_xt()
            self.attention(j)
            self.out_proj(self.din("b_w_out")[j])
            self.layernorm(4 + 2 * j)
            self.moe(2 + j)
            self.layernorm(5 + 2 * j)
        self.store_out()

    def store_out(self):
        S = self.S
        for i in range(NT):
            S.dma("sp", self.y[i * 128:(i + 1) * 128, :], self.X[:, i, :], r=(self.tX[i],))
        S.eng["sp"].wait_ge(S.sem["sp_d"], S.cnt["sp_d"])


_CACHE = {}


def _prep_inputs(inputs):
    c = host_consts()
    bucket = c.pop("_bucket")
    rel_bias = np.asarray(inputs["rel_bias"], np.float32)
    bias_tab = np.ascontiguousarray(rel_bias[bucket].transpose(2, 0, 1))
    shared = {k: np.ascontiguousarray(np.asarray(inputs[k], np.float32)) for k in
              ["a_w_in", "a_lower_bound", "a_norm_g", "a_w_out", "b_w_kv", "b_w_q", "b_sinks", "b_w_out",
               "moe_w_rg", "moe_b_rg", "moe_w_re", "moe_b_re", "moe_w_gate", "moe_w_up", "moe_w_down", "ln_g", "ln_b"]}
    shared["bias_tab"] = bias_tab
    shared.update(c)
    x = np.asarray(inputs["x"], np.float32)
    in_maps = []
    for core in range(8):
        b, half = core // 2, core % 2
        m = dict(shared)
        m["xp"] = np.ascontiguousarray(x[b, 0:SEG])
        m["xm"] = np.ascontiguousarray(x[b, half * SEG:(half + 1) * SEG])
        m["flag"] = np.full((128, 1), float(half), np.float32)
        m["halo_mask"] = np.full((128, 128), 0.0 if half else NEG, np.float32)
        in_maps.append(m)
    return in_maps


def kernel(**inputs):
    if "nc" not in _CACHE:
        p = Prog()
        _CACHE["nc"] = p.build()
        _CACHE["used"] = set(p.in_aps.keys())
    nc = _CACHE["nc"]
    in_maps = [{k: v for k, v in m.items() if k in _CACHE["used"]} for m in _prep_inputs(inputs)]
    res = run_bass_kernel_spmd(nc, in_maps, core_ids=list(range(8)))
    out = np.zeros((4, 2 * SEG, D), np.float32)
    for core in range(8):
        b, half = core // 2, core % 2
        out[b, half * SEG:(half + 1) * SEG] = res.results[core]["y"]
    return out
```

```python
import math
import os
HGL = int(os.environ.get('HGL', '99'))
HGH = int(os.environ.get('HGH', '16'))
HGV = int(os.environ.get('HGV', '3'))
from contextlib import ExitStack

import numpy as np
import concourse.bass as bass
import concourse.mybir as mybir
from concourse.bass_utils import run_bass_kernel_spmd

F32 = mybir.dt.float32
BF16 = mybir.dt.bfloat16
ALU = mybir.AluOpType
AF = mybir.ActivationFunctionType
AX = mybir.AxisListType

D = 2048
NT = 8
SEG = NT * 128
NKC = 16
CAP = 128
NE = 32
ALPHA = 8 ** 0.25
LN_EPS = 1e-5
RMS_EPS = 1e-6
NEG = -1e30
SAME_ENGINE_SYNC = True


class T:
    __slots__ = ("name", "w", "r", "excl")

    def __init__(self, name, excl=False):
        self.name = name
        self.w = None
        self.r = {}
        self.excl = excl


class Sched:
    def __init__(self, nc, es):
        self.nc = nc
        self.eng = {"pe": nc.tensor, "act": nc.scalar, "dve": nc.vector, "pool": nc.gpsimd, "sp": nc.sync}
        self.sem = {}
        self.cnt = {}
        for k in ["pe", "act", "dve", "pool", "sp", "pool_d", "sp_d"]:
            self.sem[k] = es.enter_context(nc.semaphore("sem_" + k))
            self.cnt[k] = 0
        self.waited = {k: {} for k in self.eng}
        self.n_ins = 0
        self.rec = None

    def _deps(self, e, r, w):
        deps = {}
        for t in r:
            if t.w is not None:
                k, v = t.w
                deps[k] = max(deps.get(k, 0), v)
        for t in w:
            if t.w is not None:
                k, v = t.w
                deps[k] = max(deps.get(k, 0), v)
            for k, v in t.r.items():
                deps[k] = max(deps.get(k, 0), v)
        for k, v in deps.items():
            if k == e and (e == "pe" or not SAME_ENGINE_SYNC):
                continue
            if self.waited[e].get(k, 0) < v:
                self.eng[e].wait_ge(self.sem[k], v)
                self.waited[e][k] = v

    def op(self, e, fn, r=(), w=()):
        if self.rec is not None:
            self.rec.append(("op", e, fn, tuple(r), tuple(w)))
            return
        xr = tuple(t for t in r if t.excl)
        if xr:
            r = tuple(t for t in r if not t.excl)
            w = tuple(w) + xr
        self._deps(e, r, w)
        ins = fn(self.eng[e])
        ins.then_inc(self.sem[e], 1)
        self.cnt[e] += 1
        self.n_ins += 1
        v = self.cnt[e]
        for t in r:
            t.r[e] = v
        for t in w:
            t.w = (e, v)
            t.r = {}

    def emit(self, ent):
        if ent[0] == "op":
            self.op(ent[1], ent[2], r=ent[3], w=ent[4])
        else:
            self.dma(ent[1], ent[2], ent[3], r=ent[4], w=ent[5])

    def dma(self, q, out, in_, r=(), w=()):
        if self.rec is not None:
            self.rec.append(("dma", q, out, in_, tuple(r), tuple(w)))
            return
        self._deps(q, r, w)
        dk = q + "_d"
        self.eng[q].dma_start(out=out, in_=in_).then_inc(self.sem[dk], 16)
        self.cnt[dk] += 16
        self.n_ins += 1
        v = self.cnt[dk]
        for t in r:
            t.r[dk] = v
        for t in w:
            t.w = (dk, v)
            t.r = {}

    def wait_all(self, e, tiles):
        self._deps(e, tiles, tiles)


def _t5_bucket_np(dist):
    n = np.clip(dist, 0, 127)
    max_exact = 16
    large = max_exact + (np.log(np.maximum(n, max_exact).astype(np.float32) / np.float32(max_exact))
                         / np.float32(math.log(128 / max_exact)) * np.float32(32 - max_exact)).astype(np.int32)
    large = np.minimum(large, 31)
    return np.where(n < max_exact, n, large)


def host_consts():
    c = {}
    t = np.arange(128)
    ch = t // 64
    same = ch[:, None] == ch[None, :]
    mid = ch * 64 + 31
    L1 = same * ((t[:, None] <= t[None, :]).astype(np.float32) - (t[:, None] <= mid[None, :]).astype(np.float32))
    L2 = same * (t[:, None] > t[None, :]).astype(np.float32)
    sel = np.zeros((128, 4), np.float32)
    sel[:, 0] = t <= 31
    sel[:, 1] = t < 64
    sel[:, 2] = (t >= 64) & (t <= 95)
    sel[:, 3] = t >= 64
    maskT = (same & (t[:, None] <= t[None, :])).astype(np.float32)
    ustrict = (t[:, None] < t[None, :]).astype(np.float32)
    c["c_ident"] = np.eye(128, dtype=np.float32)
    c["c_L1"] = L1.astype(np.float32)
    c["c_L2"] = L2.astype(np.float32)
    c["c_sel"] = sel
    c["c_maskT"] = maskT
    c["c_ustrict"] = ustrict
    c["c_ones"] = np.ones((128, 128), np.float32)
    c["c_iota"] = np.tile(np.arange(CAP, dtype=np.float32)[None, :], (128, 1))
    qi = np.arange(128)[:, None]
    kj = np.arange(256)[None, :]
    dist = qi + 128 - kj
    inwin = (dist >= 0) & (dist < 128)
    c["c_maskc"] = np.where(inwin, 0.0, NEG).astype(np.float32)
    c["_bucket"] = _t5_bucket_np(dist)
    return c


class Prog:
    def __init__(self, debug_stop=None):
        self.debug_stop = debug_stop
        self.nc = bass.Bass("TRN2", target_bir_lowering=False)
        self.es = ExitStack()
        self.in_shapes = {}
        self.in_aps = {}

    def dram_in(self, name, shape):
        self.in_shapes[name] = list(shape)
        return None

    def din(self, name):
        if name not in self.in_aps:
            self.in_aps[name] = self.nc.dram_tensor(name, self.in_shapes[name], F32, kind="ExternalInput").ap()
        return self.in_aps[name]

    def sb(self, name, shape, dt=F32):
        return self.es.enter_context(self.nc.sbuf_tensor("s_" + name, list(shape), dt))

    def build(self):
        nc = self.nc
        es = self.es
        with es:
            self.S = Sched(nc, es)
            self._declare()
            self._program()
        return nc

    def _declare(self):
        nc = self.nc
        di = self.dram_in
        di("xp", [SEG, D]); di("xm", [SEG, D])
        di("a_w_in", [2, D, 4 * D]); di("a_lower_bound", [2, D]); di("a_norm_g", [2, D]); di("a_w_out", [2, D, D])
        di("b_w_kv", [D, 512]); di("b_w_q", [2, D, D]); di("b_sinks", [2, 32]); di("b_w_out", [2, D, D])
        di("bias_tab", [32, 128, 256])
        di("moe_w_rg", [4, D, 4]); di("moe_b_rg", [4, 4]); di("moe_w_re", [4, D, 32]); di("moe_b_re", [4, 32])
        di("moe_w_gate", [4, NE, D, 512]); di("moe_w_up", [4, NE, D, 512]); di("moe_w_down", [4, NE, 512, D])
        di("ln_g", [8, D]); di("ln_b", [8, D]); di("flag", [128, 1]); di("halo_mask", [128, 128])
        for k, shp in [("c_ident", [128, 128]), ("c_L1", [128, 128]), ("c_L2", [128, 128]), ("c_sel", [128, 4]),
                       ("c_maskT", [128, 128]), ("c_ustrict", [128, 128]), ("c_ones", [128, 128]),
                       ("c_iota", [128, CAP]), ("c_maskc", [128, 256])]:
            di(k, shp)
        self.y = nc.dram_tensor("y", [SEG, D], F32, kind="ExternalOutput").ap()
        self.d_state = [nc.dram_tensor(f"d_state{l}", [128, 16 * 128], F32, kind="Internal").ap() for l in range(2)]
        self.d_halo = nc.dram_tensor("d_halo", [128, D], F32, kind="Internal").ap()

        sb = self.sb
        self.X = sb("X", [128, NT, D])
        self.XTraw = sb("XTraw", [128, 8192])
        self.XT = self.XTraw[:].bitcast(BF16).rearrange("p (k t) -> p k t", k=NKC)
        self.OT = sb("OT", [128, NKC, SEG], BF16)
        self.tX = [T(f"X{i}") for i in range(NT)]
        self.tXT = [T(f"XT{i}") for i in range(NT)]
        self.tOT = [T(f"OT{i}") for i in range(NT)]
        self.NB = 3
        self.WB = [sb(f"WB{i}", [128, NKC, 512], BF16) for i in range(self.NB)]
        self.tWB = [T(f"WB{i}") for i in range(self.NB)]
        self.wb_next = 0
        self.c_ident = sb("c_ident", [128, 128])
        self.c_identb = sb("c_identb", [128, 128], BF16)
        self.c_maskc = sb("c_maskc", [128, 256])
        self.c_flag = sb("c_flag", [128, 1])
        self.c_halo = sb("c_halo", [128, 128])
        self.tC = T("consts")
        self.lnst = sb("lnst", [128, 8])
        self.tlnst = T("lnst")
        self.ARN = 7300
        self.AR = sb("arena", [128, self.ARN])
        self.ar_off = 0
        self.PSS = self.es.enter_context(nc.psum_tensor("pss", [128, 1024], F32))
        self.PSG = [self.es.enter_context(nc.psum_tensor(f"psg{i}", [128, 512], F32)) for i in range(2)]
        self.PSBv = [self.PSS[:, 0:512], self.PSS[:, 512:1024], self.PSG[0][:], self.PSG[1][:]]
        self.PSF = [self.es.enter_context(nc.psum_tensor(f"psf{i}", [128, 512], F32)) for i in range(2)]
        self.PST = [self.es.enter_context(nc.psum_tensor(f"pst{i}", [128, 1024], BF16)) for i in range(2)]
        self.tPSB = [T(f"psb{i}", True) for i in range(4)]
        self.tPSF = [T(f"psf{i}", True) for i in range(2)]
        self.tPST = [T(f"pst{i}", True) for i in range(2)]

    def barrier(self):
        S = self.S
        for e in S.eng:
            for k in S.sem:
                if k == e:
                    continue
                if S.cnt[k] > S.waited[e].get(k, 0):
                    S.eng[e].wait_ge(S.sem[k], S.cnt[k])
                    S.waited[e][k] = S.cnt[k]

    def phase(self):
        self.barrier()
        self.ar_off = 0

    def arf(self, cols, shape=None):
        o = self.ar_off
        self.ar_off += cols
        assert self.ar_off <= self.ARN, f"arena overflow {self.ar_off}"
        v = self.AR[:, o:o + cols]
        return v

    def arb(self, cols):
        assert cols % 2 == 0
        return self.arf(cols // 2).bitcast(BF16)

    def wload(self, pieces):
        i = self.wb_next
        self.wb_next = (i + 1) % getattr(self, "NB_active", self.NB)
        buf, t = self.WB[i], self.tWB[i]
        for dst_fn, src in pieces:
            self.S.dma("pool", dst_fn(buf), src, r=(), w=(t,))
        return buf, t

    def load_consts(self):
        S = self.S
        for k in ["c_ident", "c_maskc"]:
            S.dma("sp", getattr(self, k)[:], self.din(k)[:, :], w=(self.tC,))
        S.dma("sp", self.c_flag[:], self.din("flag")[:, :], w=(self.tC,))
        S.dma("sp", self.c_halo[:], self.din("halo_mask")[:, :], w=(self.tC,))
        S.op("dve", lambda e: e.tensor_copy(out=self.c_identb[:], in_=self.c_ident[:]), r=(self.tC,), w=(self.tC,))

    def load_x(self, src):
        for i in range(NT):
            self.S.dma("sp", self.X[:, i, :], src[i * 128:(i + 1) * 128, :], w=(self.tX[i],))

    def make_xt(self):
        S = self.S
        n = 0
        for i in range(NT):
            for g in range(4):
                pb = n % 2
                n += 1
                ps, tps = self.PSF[pb], self.tPSF[pb]
                for j in range(4):
                    kc = g * 4 + j
                    S.op("pe", lambda e, kc=kc, j=j, ps=ps, i=i: e.transpose(
                        ps[:, j * 128:(j + 1) * 128], self.X[:, i, kc * 128:(kc + 1) * 128], self.c_ident[:]),
                        r=(self.tX[i], self.tC), w=(tps,))
                dst = self.XT[:, g * 4:(g + 1) * 4, i * 128:(i + 1) * 128]
                src = ps[:].rearrange("p (j t) -> p j t", j=4)
                if n % 2:
                    S.op("act", lambda e, dst=dst, src=src: e.copy(out=dst, in_=src), r=(tps,), w=(self.tXT[i],))
                else:
                    S.op("dve", lambda e, dst=dst, src=src: e.tensor_copy(out=dst, in_=src), r=(tps,), w=(self.tXT[i],))

    def rsqrt_small(self, out, in_, scale, eps, rt, wt):
        S = self.S
        S.op("act", lambda e: e.activation(out=out, in_=in_, func=AF.Ln, bias=self.epsb(eps), scale=scale), r=rt, w=wt)
        S.op("act", lambda e: e.activation(out=out, in_=out, func=AF.Exp, scale=-0.5), r=wt, w=wt)

    def epsb(self, eps):
        return self.c_eps[eps][:]

    def layernorm(self, idx):
        S = self.S
        self.barrier()
        vg = self.XTraw[:, 0:D]
        vb = self.XTraw[:, D:2 * D]
        tv = T("lnvec")
        S.dma("sp", vg, self.din("ln_g")[idx:idx + 1, :].partition_broadcast(128), w=(tv,))
        S.dma("sp", vb, self.din("ln_b")[idx:idx + 1, :].partition_broadcast(128), w=(tv,))
        st, tst = self.lnst, self.tlnst
        junk = self.OT[:, 0:2, :]
        tj = T("lnjunk")
        for i in range(NT):
            xi = self.X[:, i, :]
            xi3 = xi.rearrange("p (a b) -> p a b", a=2)
            S.op("act", lambda e, xi3=xi3: e.activation(out=junk, in_=xi3, func=AF.Copy, accum_out=st[:, 0:1]),
                 r=(self.tX[i],), w=(tst, tj))
            S.op("act", lambda e, xi3=xi3: e.activation(out=junk, in_=xi3, func=AF.Square, accum_out=st[:, 1:2]),
                 r=(self.tX[i],), w=(tst, tj))
            S.op("dve", lambda e: e.tensor_scalar(out=st[:, 2:4], in0=st[:, 0:2], scalar1=1.0 / D, scalar2=None, op0=ALU.mult),
                 r=(tst,), w=(tst,))
            S.op("dve", lambda e: e.tensor_tensor(out=st[:, 4:5], in0=st[:, 2:3], in1=st[:, 2:3], op=ALU.mult), r=(tst,), w=(tst,))
            S.op("dve", lambda e: e.tensor_tensor(out=st[:, 5:6], in0=st[:, 3:4], in1=st[:, 4:5], op=ALU.subtract), r=(tst,), w=(tst,))
            self.rsqrt_small(st[:, 6:7], st[:, 5:6], 1.0, LN_EPS, (tst,), (tst,))
            S.op("dve", lambda e: e.scalar_tensor_tensor(out=st[:, 7:8], in0=st[:, 2:3], scalar=-1.0, in1=st[:, 6:7], op0=ALU.mult, op1=ALU.mult),
                 r=(tst,), w=(tst,))
            S.op("act", lambda e, xi=xi: e.activation(out=xi, in_=xi, func=AF.Identity, bias=st[:, 7:8], scale=st[:, 6:7]),
                 r=(tst, self.tX[i]), w=(self.tX[i],))
            S.op("dve", lambda e, xi=xi: e.tensor_tensor(out=xi, in0=xi, in1=vg, op=ALU.mult), r=(tv, self.tX[i]), w=(self.tX[i],))
            S.op("dve", lambda e, xi=xi: e.tensor_tensor(out=xi, in0=xi, in1=vb, op=ALU.add), r=(tv, self.tX[i]), w=(self.tX[i],))
        self.barrier()

    def out_proj(self, w_dram):
        S = self.S
        n = 0
        for nch in range(4):
            buf, tb = self.wload([(lambda b: b[:], w_dram[:, nch * 512:(nch + 1) * 512].rearrange("(kc p) n -> p kc n", p=128))])
            for i in range(NT):
                pb = n % 2
                n += 1
                ps, tps = self.PSF[pb], self.tPSF[pb]
                for kc in range(NKC):
                    S.op("pe", lambda e, kc=kc, ps=ps, i=i, buf=buf: e.matmul(
                        ps[:], lhsT=self.OT[:, kc, i * 128:(i + 1) * 128], rhs=buf[:, kc, :], start=(kc == 0), stop=(kc == NKC - 1)),
                        r=(self.tOT[i], tb), w=(tps,))
                xs = self.X[:, i, nch * 512:(nch + 1) * 512]
                S.op("dve", lambda e, xs=xs, ps=ps: e.scalar_tensor_tensor(out=xs, in0=xs, scalar=ALPHA, in1=ps[:], op0=ALU.mult, op1=ALU.add),
                     r=(tps, self.tX[i]), w=(self.tX[i],))

    def hgrn(self, l, init_mode):
        S = self.S
        self.phase()
        f = self.arf
        St = f(2048).rearrange("p (h v) -> p h v", h=16)
        tSt = [T(f"St{h}") for h in range(16)]
        cL1, cL2, cMT, csel = f(128), f(128), f(128), f(4)
        tc = T("hc")
        for dst, k in [(cL1, "c_L1"), (cL2, "c_L2"), (cMT, "c_maskT"), (csel, "c_sel")]:
            S.dma("sp", dst, self.din(k)[:, :], w=(tc,))
        if init_mode == "zero":
            S.op("dve", lambda e: e.memset(St, 0.0), w=tuple(tSt))
        else:
            S.dma("sp", St, self.d_state[l].rearrange("p (h v) -> p h v", h=16), w=tuple(tSt))
            S.op("dve", lambda e: e.tensor_scalar(out=St, in0=St, scalar1=self.c_flag[:, 0:1], scalar2=None, op0=ALU.mult),
                 r=tuple(tSt) + (self.tC,), w=tuple(tSt))
        a0, a1, lb, oml, ng = f(128), f(128), f(128), f(128), f(128)
        tv = T("hvec")
        wb2 = self.WB[2][:].rearrange("p k n -> p (k n)").bitcast(F32)
        wb2_off = [0]

        def f2(cols):
            o = wb2_off[0]
            wb2_off[0] += cols
            assert wb2_off[0] <= 4096
            return wb2[:, o:o + cols]

        def mk_set(ff, sfx):
            fb_ = lambda cols: ff(cols // 2).bitcast(BF16)
            ws = {}
            for k in ["e1", "t1", "fg", "kk", "logf", "eq", "ek", "ekl", "sg", "gg"]:
                ws[k] = ff(128)
            ws["ebT"] = ff(4)
            ws["ss"] = ff(4)
            for k in ["qt", "kt", "kh", "vv", "QF", "KTs", "sTm", "Sp0", "Sp1", "on", "junk"]:
                ws[k] = fb_(128)
            ws["QZ"] = fb_(256).rearrange("p (a b) -> p a b", a=2)
            ws["tw"] = {k: T("hw_" + k + sfx) for k in ["e1", "t1", "fg", "kk", "logf", "eq", "ek", "ekl", "ebT", "sg", "gg", "ss", "qt", "kt", "kh", "vv",
                                                      "QF", "KTs", "sTm", "Sp0", "Sp1", "on", "QZ", "junk"]}
            S.op("dve", lambda e, ws=ws: e.memset(ws["QZ"], 0.0), w=(ws["tw"]["QZ"],))
            return ws

        wsets = [mk_set(f, "a"), mk_set(f2, "b")]
        self.NB_active = 2
        self.wb_next = 0
        w_in = self.din("a_w_in")
        alb = self.din("a_lower_bound")
        ang = self.din("a_norm_g")
        for h in range(HGH):
            hs = slice(h * 128, (h + 1) * 128)
            buf, tb = self.wload([(lambda b, p=p: b[:, :, p * 128:(p + 1) * 128],
                                   w_in[l, :, p * D + h * 128:p * D + (h + 1) * 128].rearrange("(kc p) n -> p kc n", p=128))
                                  for p in range(4)])
            S.dma("sp", ng, ang[l:l + 1, hs].partition_broadcast(128), w=(tv,))
            if l == 0:
                S.op("dve", lambda e: e.memset(lb, 0.0), w=(tv,))
            else:
                S.dma("sp", a0, alb[0:1, hs].partition_broadcast(128), w=(tv,))
                S.dma("sp", a1, alb[1:2, hs].partition_broadcast(128), w=(tv,))
                S.op("dve", lambda e: e.tensor_tensor(out=lb, in0=a0, in1=a1, op=ALU.subtract), r=(tv,), w=(tv,))
                S.op("act", lambda e: e.activation(out=lb, in_=lb, func=AF.Exp), r=(tv,), w=(tv,))
                S.op("dve", lambda e: e.tensor_scalar(out=lb, in0=lb, scalar1=1.0, scalar2=None, op0=ALU.add), r=(tv,), w=(tv,))
                S.op("dve", lambda e: e.reciprocal(out=lb, in_=lb), r=(tv,), w=(tv,))
            S.op("dve", lambda e: e.tensor_scalar(out=oml, in0=lb, scalar1=-1.0, scalar2=1.0, op0=ALU.mult, op1=ALU.add), r=(tv,), w=(tv,))
            def tile_body(i, ws, h=h, buf=buf, tb=tb):
                e1, t1, fg, kk, logf, eq, ek, ekl, ebT, sg, gg, ss = (ws[k] for k in ['e1', 't1', 'fg', 'kk', 'logf', 'eq', 'ek', 'ekl', 'ebT', 'sg', 'gg', 'ss'])
                qt, kt, kh, vv, QF, KTs, sTm, Sp0, Sp1, on, junk, QZ = (ws[k] for k in ['qt', 'kt', 'kh', 'vv', 'QF', 'KTs', 'sTm', 'Sp0', 'Sp1', 'on', 'junk', 'QZ'])
                tw = ws['tw']
                ts = slice(i * 128, (i + 1) * 128)
                pp, tpp = self.PSF[i % 2], self.tPSF[i % 2]
                for kc in range(NKC):
                    S.op("pe", lambda e, kc=kc, pp=pp, ts=ts, buf=buf: e.matmul(pp[:], lhsT=self.XT[:, kc, ts], rhs=buf[:, kc, :],
                                                                                 start=(kc == 0), stop=(kc == NKC - 1)),
                         r=(self.tXT[i], tb), w=(tpp,))
                if HGL < 1:
                    return
                pq, pf, pi_, pg = pp[:, 0:128], pp[:, 128:256], pp[:, 256:384], pp[:, 384:512]
                S.op("act", lambda e: e.activation(out=e1, in_=pf, func=AF.Exp, scale=-1.0), r=(tpp,), w=(tw["e1"],))
                S.op("dve", lambda e: e.tensor_scalar(out=e1, in0=e1, scalar1=1.0, scalar2=None, op0=ALU.add), r=(tw["e1"],), w=(tw["e1"],))
                S.op("dve", lambda e: e.reciprocal(out=e1, in_=e1), r=(tw["e1"],), w=(tw["e1"],))
                S.op("dve", lambda e: e.tensor_tensor(out=t1, in0=e1, in1=oml, op=ALU.mult), r=(tw["e1"], tv), w=(tw["t1"],))
                S.op("dve", lambda e: e.tensor_tensor(out=fg, in0=t1, in1=lb, op=ALU.add), r=(tw["t1"], tv), w=(tw["fg"],))
                S.op("dve", lambda e: e.tensor_scalar(out=fg, in0=fg, scalar1=1e-30, scalar2=None, op0=ALU.max), r=(tw["fg"],), w=(tw["fg"],))
                S.op("act", lambda e: e.activation(out=logf, in_=fg, func=AF.Ln), r=(tw["fg"],), w=(tw["logf"],))
                S.op("dve", lambda e: e.tensor_tensor(out=kk, in0=oml, in1=t1, op=ALU.subtract), r=(tw["t1"], tv), w=(tw["kk"],))
                if HGL < 2:
                    return
                pc, tpc = self.PSBv[0], self.tPSB[0]
                S.op("pe", lambda e: e.matmul(pc[:, 0:128], lhsT=cL1, rhs=logf, start=True, stop=True), r=(tc, tw["logf"]), w=(tpc,))
                if HGV >= 2:
                    S.op("pe", lambda e: e.matmul(pc[:, 128:256], lhsT=cL2, rhs=logf, start=True, stop=True), r=(tc, tw["logf"]), w=(tpc,))
                if HGV >= 3:
                    S.op("pe", lambda e: e.matmul(pc[:, 256:260], lhsT=logf, rhs=csel, start=True, stop=True), r=(tc, tw["logf"]), w=(tpc,))
                if HGL < 3 or os.environ.get('HGX') == '1':
                    return
                S.op("act", lambda e: e.activation(out=eq, in_=pc[:, 0:128], func=AF.Exp), r=(tpc,), w=(tw["eq"],))
                S.op("act", lambda e: e.activation(out=ek, in_=pc[:, 0:128], func=AF.Exp, scale=-1.0), r=(tpc,), w=(tw["ek"],))
                S.op("act", lambda e: e.activation(out=ekl, in_=pc[:, 128:256], func=AF.Exp), r=(tpc,), w=(tw["ekl"],))
                S.op("act", lambda e: e.activation(out=ebT, in_=pc[:, 256:260], func=AF.Exp), r=(tpc,), w=(tw["ebT"],))
                if os.environ.get('HGX') == '2':
                    return
                S.op("dve", lambda e: e.tensor_tensor(out=qt, in0=pq, in1=eq, op=ALU.mult), r=(tpp, tw["eq"]), w=(tw["qt"],))
                S.op("dve", lambda e: e.tensor_tensor(out=kt, in0=kk, in1=ek, op=ALU.mult), r=(tw["kk"], tw["ek"]), w=(tw["kt"],))
                S.op("dve", lambda e: e.tensor_tensor(out=kh, in0=kk, in1=ekl, op=ALU.mult), r=(tw["kk"], tw["ekl"]), w=(tw["kh"],))
                if os.environ.get('HGX') == '3':
                    return
                S.op("act", lambda e: e.copy(out=vv, in_=pi_), r=(tpp,), w=(tw["vv"],))
                if os.environ.get('HGX') == '4':
                    return
                S.op("act", lambda e: e.activation(out=sg, in_=pg, func=AF.Exp, scale=-1.0), r=(tpp,), w=(tw["sg"],))
                S.op("dve", lambda e: e.tensor_scalar(out=sg, in0=sg, scalar1=1.0, scalar2=None, op0=ALU.add), r=(tw["sg"],), w=(tw["sg"],))
                S.op("dve", lambda e: e.reciprocal(out=sg, in_=sg), r=(tw["sg"],), w=(tw["sg"],))
                S.op("dve", lambda e: e.tensor_tensor(out=gg, in0=pg, in1=sg, op=ALU.mult), r=(tpp, tw["sg"]), w=(tw["gg"],))
                S.op("dve", lambda e: e.tensor_tensor(out=gg, in0=gg, in1=ng, op=ALU.mult), r=(tw["gg"], tv), w=(tw["gg"],))
                if HGL < 4:
                    return
                pt, tpt = self.PST[0], self.tPST[0]
                S.op("pe", lambda e: e.transpose(pt[:, 0:128], qt, self.c_identb[:]), r=(tw["qt"], self.tC), w=(tpt,))
                S.op("pe", lambda e: e.transpose(pt[:, 128:256], kt, self.c_identb[:]), r=(tw["kt"], self.tC), w=(tpt,))
                S.op("dve", lambda e: e.tensor_copy(out=QF, in_=pt[:, 0:128]), r=(tpt,), w=(tw["QF"],))
                S.op("dve", lambda e: e.tensor_copy(out=QZ[:, 0, 0:64], in_=pt[:, 0:64]), r=(tpt,), w=(tw["QZ"],))
                S.op("dve", lambda e: e.tensor_copy(out=QZ[:, 1, 64:128], in_=pt[:, 64:128]), r=(tpt,), w=(tw["QZ"],))
                S.op("act", lambda e: e.copy(out=KTs, in_=pt[:, 128:256]), r=(tpt,), w=(tw["KTs"],))
                if HGL < 5:
                    return
                psc, tpsc = self.PSBv[1], self.tPSB[1]
                S.op("pe", lambda e: e.matmul(psc[:, 0:128], lhsT=KTs, rhs=QF, start=True, stop=True), r=(tw["KTs"], tw["QF"]), w=(tpsc,))
                S.op("dve", lambda e: e.tensor_tensor(out=sTm, in0=psc[:, 0:128], in1=cMT, op=ALU.mult), r=(tpsc, tc), w=(tw["sTm"],))
                if HGL < 6:
                    return
                pds, tpds = self.PSBv[2], self.tPSB[2]
                Sh = St[:, h, :]
                S.op("dve", lambda e: e.tensor_scalar(out=Sp0, in0=Sh, scalar1=ebT[:, 0:1], scalar2=None, op0=ALU.mult),
                     r=(tSt[h], tw["ebT"]), w=(tw["Sp0"],))
                S.op("pe", lambda e: e.matmul(pds[:, 0:128], lhsT=kh[0:64, :], rhs=vv[0:64, :], start=True, stop=True),
                     r=(tw["kh"], tw["vv"]), w=(tpds,))
                S.op("dve", lambda e: e.scalar_tensor_tensor(out=Sh, in0=Sh, scalar=ebT[:, 1:2], in1=pds[:, 0:128], op0=ALU.mult, op1=ALU.add),
                     r=(tSt[h], tw["ebT"], tpds), w=(tSt[h],))
                S.op("dve", lambda e: e.tensor_scalar(out=Sp1, in0=Sh, scalar1=ebT[:, 2:3], scalar2=None, op0=ALU.mult),
                     r=(tSt[h], tw["ebT"]), w=(tw["Sp1"],))
                S.op("pe", lambda e: e.matmul(pds[:, 128:256], lhsT=kh[64:128, :], rhs=vv[64:128, :], start=True, stop=True),
                     r=(tw["kh"], tw["vv"]), w=(tpds,))
                S.op("dve", lambda e: e.scalar_tensor_tensor(out=Sh, in0=Sh, scalar=ebT[:, 3:4], in1=pds[:, 128:256], op0=ALU.mult, op1=ALU.add),
                     r=(tSt[h], tw["ebT"], tpds), w=(tSt[h],))
                if HGL < 7:
                    return
                po, tpo = self.PSBv[3], self.tPSB[3]
                S.op("pe", lambda e: e.matmul(po[:, 0:128], lhsT=sTm, rhs=vv, start=True, stop=False), r=(tw["sTm"], tw["vv"]), w=(tpo,))
                S.op("pe", lambda e: e.matmul(po[:, 0:128], lhsT=QZ[:, 0, :], rhs=Sp0, start=False, stop=False), r=(tw["QZ"], tw["Sp0"]), w=(tpo,))
                S.op("pe", lambda e: e.matmul(po[:, 0:128], lhsT=QZ[:, 1, :], rhs=Sp1, start=False, stop=True), r=(tw["QZ"], tw["Sp1"]), w=(tpo,))
                if HGL < 8:
                    return
                S.op("act", lambda e: e.activation(out=junk, in_=po[:, 0:128], func=AF.Square, accum_out=ss[:, 0:1]), r=(tpo,), w=(tw["ss"], tw["junk"]))
                self.rsqrt_small(ss[:, 1:2], ss[:, 0:1], 1.0 / 128, RMS_EPS, (tw["ss"],), (tw["ss"],))
                S.op("dve", lambda e: e.scalar_tensor_tensor(out=on, in0=po[:, 0:128], scalar=ss[:, 1:2], in1=gg, op0=ALU.mult, op1=ALU.mult),
                     r=(tpo, tw["ss"], tw["gg"]), w=(tw["on"],))
                if HGL < 9:
                    return
                pt1, tpt1 = self.PST[1], self.tPST[1]
                S.op("pe", lambda e: e.transpose(pt1[:, 0:128], on, self.c_identb[:]), r=(tw["on"], self.tC), w=(tpt1,))
                S.op("act", lambda e, h=h, ts=ts: e.copy(out=self.OT[:, h, ts], in_=pt1[:, 0:128]), r=(tpt1,), w=(self.tOT[i],))

            LAG = 30
            recs = []
            for i in range(NT):
                rec = []
                real_op = S.op
                S.op = lambda e, fn, r=(), w=(), rec=rec: rec.append((e, fn, tuple(r), tuple(w)))
                try:
                    tile_body(i, wsets[i % 2])
                finally:
                    S.op = real_op
                recs.append(rec)
            active = []
            nxt = 0
            while nxt < NT or active:
                if nxt < NT and len(active) < 2 and (not active or active[-1][1] >= min(LAG, len(active[-1][0]))):
                    active.append([recs[nxt], 0])
                    nxt += 1
                for a in list(active):
                    if a[1] < len(a[0]):
                        e_, fn_, r_, w_ = a[0][a[1]]
                        S.op(e_, fn_, r=r_, w=w_)
                        a[1] += 1
                active = [a for a in active if a[1] < len(a[0])]
        S.dma("sp", self.d_state[l].rearrange("p (h v) -> p h v", h=16), St, r=tuple(tSt))
        self.barrier()
        self.NB_active = 3
        self.wb_next = 0

    def moe(self, L):
        S = self.S
        self.barrier()
        X1b = self.OT[:].rearrange("p k t -> p (k t)").rearrange("p (i d) -> p i d", i=NT)
        tX1b = [T(f"x1b{i}") for i in range(NT)]
        off = [0]

        def f(cols):
            o = off[0]
            off[0] += cols
            assert off[0] <= 8192, off[0]
            return self.XTraw[:, o:o + cols]

        def fb(cols):
            return f(cols // 2).bitcast(BF16)

        wr = f(NKC * 36).rearrange("p (k n) -> p k n", k=NKC)
        rb = f(36)
        ustrict, ones, iota = f(128), f(128), f(CAP)
        xT4 = [f(512), f(512)]
        A = f(NT * 32).rearrange("p (i e) -> p i e", i=NT)
        Gt = f(NT * 32).rearrange("p (i e) -> p i e", i=NT)
        VAL = f(NT * 32).rearrange("p (i e) -> p i e", i=NT)
        lg, gsel, pen, elm, elm2, oh1, oh2 = f(36), f(4), f(4), f(32), f(32), f(32), f(32)
        sm = f(16)
        junk4 = f(4)
        aoff = [3456]

        def fb2(cols):
            o = aoff[0]
            aoff[0] += cols // 2
            assert aoff[0] <= self.ARN
            return self.AR[:, o:o + cols // 2].bitcast(BF16)

        sets = []
        for fbx, sfx in ((fb, "a"), (fb2, "b")):
            sets.append(dict(
                P=fbx(NT * CAP).rearrange("p (i c) -> p i c", i=NT),
                Pg=fbx(NT * CAP).rearrange("p (i c) -> p i c", i=NT),
                PgT=fbx(NT * 128).rearrange("p (i t) -> p i t", i=NT),
                XS=fbx(NKC * CAP).rearrange("p (k c) -> p k c", k=NKC),
                tP=[T("P" + sfx + str(i)) for i in range(NT)], tPg=[T("Pg" + sfx + str(i)) for i in range(NT)],
                tPgT=T("PgT" + sfx), tXS=[T("XS" + sfx + str(g)) for g in range(4)]))
        HT = fb(4 * CAP).rearrange("p (k c) -> p k c", k=4)
        hsg = f(4 * CAP)
        Y = fb(D)
        tc_ = T("mc")
        tr = T("mrt")
        tA = [T(f"mA{i}") for i in range(NT)]
        txT4 = [T("xT4a"), T("xT4b")]
        tHT, thsg = T("HT"), T("hsg")
        tY = [T(f"Y{k}") for k in range(4)]

        S.dma("sp", wr[:, :, 0:4], self.din("moe_w_rg")[L].rearrange("(k p) n -> p k n", p=128), w=(tc_,))
        S.dma("sp", wr[:, :, 4:36], self.din("moe_w_re")[L].rearrange("(k p) n -> p k n", p=128), w=(tc_,))
        S.dma("sp", rb[:, 0:4], self.din("moe_b_rg")[L:L + 1, :].partition_broadcast(128), w=(tc_,))
        S.dma("sp", rb[:, 4:36], self.din("moe_b_re")[L:L + 1, :].partition_broadcast(128), w=(tc_,))
        S.dma("sp", ustrict, self.din("c_ustrict")[:, :], w=(tc_,))
        S.dma("sp", ones, self.din("c_ones")[:, :], w=(tc_,))
        S.dma("sp", iota, self.din("c_iota")[:, :], w=(tc_,))

        for i in range(NT):
            if i % 2:
                S.op("act", lambda e, i=i: e.copy(out=X1b[:, i, :], in_=self.X[:, i, :]), r=(self.tX[i],), w=(tX1b[i],))
            else:
                S.op("dve", lambda e, i=i: e.tensor_copy(out=X1b[:, i, :], in_=self.X[:, i, :]), r=(self.tX[i],), w=(tX1b[i],))

        n = 0
        plg, tplg = self.PSBv[0], self.tPSB[0]
        for i in range(NT):
            for g in range(4):
                pb = n % 2
                n += 1
                ps, tps = self.PSF[pb], self.tPSF[pb]
                for j in range(4):
                    kc = g * 4 + j
                    S.op("pe", lambda e, kc=kc, j=j, ps=ps, i=i: e.transpose(
                        ps[:, j * 128:(j + 1) * 128], self.X[:, i, kc * 128:(kc + 1) * 128], self.c_ident[:]),
                        r=(self.tX[i], self.tC), w=(tps,))
                xt, txt = xT4[pb], txT4[pb]
                if pb:
                    S.op("act", lambda e, xt=xt, ps=ps: e.copy(out=xt, in_=ps[:]), r=(tps,), w=(txt,))
                else:
                    S.op("dve", lambda e, xt=xt, ps=ps: e.tensor_copy(out=xt, in_=ps[:]), r=(tps,), w=(txt,))
                for j in range(4):
                    kc = g * 4 + j
                    S.op("pe", lambda e, kc=kc, j=j, xt=xt: e.matmul(plg[:, 0:36], lhsT=xt[:, j * 128:(j + 1) * 128], rhs=wr[:, kc, :],
                                                                      start=(kc == 0), stop=(kc == NKC - 1)),
                         r=(txt, tc_), w=(tplg,))
            dv = lambda fn, r, w: S.op("dve", fn, r=r, w=w)
            dv(lambda e: e.tensor_tensor(out=lg, in0=plg[:, 0:36], in1=rb, op=ALU.add), (tplg, tc_), (tr,))
            gl, el = lg[:, 0:4], lg[:, 4:36]
            gmax, ngmax, sume, pgrp, m1, m2, dd, g1, g2 = (sm[:, k:k + 1] for k in range(9))
            dv(lambda e: e.tensor_reduce(out=gmax, in_=gl, axis=AX.X, op=ALU.max), (tr,), (tr,))
            dv(lambda e: e.tensor_scalar(out=gsel, in0=gl, scalar1=gmax, scalar2=None, op0=ALU.is_equal), (tr,), (tr,))
            dv(lambda e: e.tensor_scalar(out=ngmax, in0=gmax, scalar1=-1.0, scalar2=None, op0=ALU.mult), (tr,), (tr,))
            S.op("act", lambda e: e.activation(out=junk4, in_=gl, func=AF.Exp, bias=ngmax, scale=1.0, accum_out=sume), r=(tr,), w=(tr,))
            dv(lambda e: e.reciprocal(out=pgrp, in_=sume), (tr,), (tr,))
            dv(lambda e: e.tensor_scalar(out=pen, in0=gsel, scalar1=-1.0, scalar2=1e30, op0=ALU.add, op1=ALU.mult), (tr,), (tr,))
            for g in range(4):
                dv(lambda e, g=g: e.tensor_scalar(out=elm[:, g * 8:(g + 1) * 8], in0=el[:, g * 8:(g + 1) * 8], scalar1=pen[:, g:g + 1],
                                                  scalar2=None, op0=ALU.add), (tr,), (tr,))
            dv(lambda e: e.tensor_reduce(out=m1, in_=elm, axis=AX.X, op=ALU.max), (tr,), (tr,))
            dv(lambda e: e.tensor_scalar(out=oh1, in0=elm, scalar1=m1, scalar2=None, op0=ALU.is_equal), (tr,), (tr,))
            dv(lambda e: e.scalar_tensor_tensor(out=elm2, in0=oh1, scalar=-1e30, in1=elm, op0=ALU.mult, op1=ALU.add), (tr,), (tr,))
            dv(lambda e: e.tensor_reduce(out=m2, in_=elm2, axis=AX.X, op=ALU.max), (tr,), (tr,))
            dv(lambda e: e.tensor_scalar(out=oh2, in0=elm2, scalar1=m2, scalar2=None, op0=ALU.is_equal), (tr,), (tr,))
            dv(lambda e: e.tensor_tensor(out=dd, in0=m2, in1=m1, op=ALU.subtract), (tr,), (tr,))
            S.op("act", lambda e: e.activation(out=dd, in_=dd, func=AF.Exp), r=(tr,), w=(tr,))
            dv(lambda e: e.tensor_scalar(out=g1, in0=dd, scalar1=1.0, scalar2=None, op0=ALU.add), (tr,), (tr,))
            dv(lambda e: e.reciprocal(out=g1, in_=g1), (tr,), (tr,))
            dv(lambda e: e.tensor_tensor(out=g2, in0=dd, in1=g1, op=ALU.mult), (tr,), (tr,))
            dv(lambda e: e.tensor_tensor(out=g1, in0=g1, in1=pgrp, op=ALU.mult), (tr,), (tr,))
            dv(lambda e: e.tensor_tensor(out=g2, in0=g2, in1=pgrp, op=ALU.mult), (tr,), (tr,))
            dv(lambda e, i=i: e.tensor_tensor(out=A[:, i, :], in0=oh1, in1=oh2, op=ALU.add), (tr,), (tA[i],))
            dv(lambda e, i=i: e.tensor_scalar(out=Gt[:, i, :], in0=oh1, scalar1=g1, scalar2=None, op0=ALU.mult), (tr,), (tA[i],))
            dv(lambda e, i=i: e.scalar_tensor_tensor(out=Gt[:, i, :], in0=oh2, scalar=g2, in1=Gt[:, i, :], op0=ALU.mult, op1=ALU.add),
               (tr, tA[i]), (tA[i],))
        pr, tpr = self.PSBv[1], self.tPSB[1]
        for i in range(NT):
            for j in range(i):
                S.op("pe", lambda e, j=j: e.matmul(pr[:, 0:32], lhsT=ones, rhs=A[:, j, :], start=(j == 0), stop=False), r=(tc_, tA[j]), w=(tpr,))
            S.op("pe", lambda e, i=i: e.matmul(pr[:, 0:32], lhsT=ustrict, rhs=A[:, i, :], start=(i == 0), stop=True), r=(tc_, tA[i]), w=(tpr,))
            S.op("dve", lambda e, i=i: e.scalar_tensor_tensor(out=VAL[:, i, :], in0=pr[:, 0:32], scalar=1.0, in1=A[:, i, :], op0=ALU.add, op1=ALU.mult),
                 r=(tpr, tA[i]), w=(tA[i],))
            S.op("dve", lambda e, i=i: e.tensor_scalar(out=VAL[:, i, :], in0=VAL[:, i, :], scalar1=-1.0, scalar2=None, op0=ALU.add),
                 r=(tA[i],), w=(tA[i],))
        for i in range(NT):
            if i % 2:
                S.op("act", lambda e, i=i: e.mul(out=self.X[:, i, :], in_=self.X[:, i, :], mul=ALPHA), r=(self.tX[i],), w=(self.tX[i],))
            else:
                S.op("dve", lambda e, i=i: e.tensor_scalar(out=self.X[:, i, :], in0=self.X[:, i, :], scalar1=ALPHA, scalar2=None, op0=ALU.mult),
                     r=(self.tX[i],), w=(self.tX[i],))
        wg_d, wu_d, wd_d = self.din("moe_w_gate"), self.din("moe_w_up"), self.din("moe_w_down")
        wts = {}

        def loads(ex):
            wg, twg = self.wload([(lambda b: b[:], wg_d[L, ex].rearrange("(kc p) n -> p kc n", p=128))])
            wu, twu = self.wload([(lambda b: b[:], wu_d[L, ex].rearrange("(kc p) n -> p kc n", p=128))])
            wdv = lambda b: b[:].rearrange("p k n -> p (k n)").rearrange("p (fc d) -> p fc d", fc=4)
            wd_, twd = self.wload([(wdv, wd_d[L, ex].rearrange("(fc p) d -> p fc d", p=128))])
            wts[ex] = (wg, twg, wu, twu, wdv(wd_), twd)

        def front(ex):
            st = sets[ex % 2]
            P, Pg, PgT, XS = st["P"], st["Pg"], st["PgT"], st["XS"]
            for i in range(NT):
                S.op("dve", lambda e, i=i: e.tensor_scalar(out=P[:, i, :], in0=iota, scalar1=VAL[:, i, ex:ex + 1], scalar2=None, op0=ALU.is_equal),
                     r=(tc_, tA[i]), w=(st["tP"][i],))
                S.op("dve", lambda e, i=i: e.tensor_scalar(out=Pg[:, i, :], in0=iota, scalar1=VAL[:, i, ex:ex + 1], scalar2=Gt[:, i, ex:ex + 1],
                                                           op0=ALU.is_equal, op1=ALU.mult), r=(tc_, tA[i]), w=(st["tPg"][i],))
            pt, tpt = self.PST[0], self.tPST[0]
            for i in range(NT):
                S.op("pe", lambda e, i=i: e.transpose(pt[:, i * 128:(i + 1) * 128], Pg[:, i, :], self.c_identb[:]), r=(st["tPg"][i], self.tC), w=(tpt,))
            S.op("act", lambda e: e.copy(out=PgT, in_=pt[:].rearrange("p (i t) -> p i t", i=NT)), r=(tpt,), w=(st["tPgT"],))
            for g in range(4):
                ps, tps = self.PSF[g % 2], self.tPSF[g % 2]
                for j in range(4):
                    kc = g * 4 + j
                    for i in range(NT):
                        S.op("pe", lambda e, kc=kc, j=j, i=i, ps=ps: e.matmul(ps[:, j * CAP:(j + 1) * CAP], lhsT=X1b[:, i, kc * 128:(kc + 1) * 128],
                                                                               rhs=P[:, i, :], start=(i == 0), stop=(i == NT - 1)),
                             r=(tX1b[i], st["tP"][i]), w=(tps,))
                dst = XS[:, g * 4:(g + 1) * 4, :]
                src = ps[:, 0:4 * CAP].rearrange("p (j c) -> p j c", j=4)
                if g % 2:
                    S.op("act", lambda e, dst=dst, src=src: e.copy(out=dst, in_=src), r=(tps,), w=(st["tXS"][g],))
                else:
                    S.op("dve", lambda e, dst=dst, src=src: e.tensor_copy(out=dst, in_=src), r=(tps,), w=(st["tXS"][g],))

        def back(ex):
            st = sets[ex % 2]
            PgT, XS = st["PgT"], st["XS"]
            wg, twg, wu, twu, wd, twd = wts[ex]
            phg, tphg = self.PSBv[0], self.tPSB[0]
            phu, tphu = self.PSBv[1], self.tPSB[1]
            for (pw, tpw, wt, twt) in ((phg, tphg, wg, twg), (phu, tphu, wu, twu)):
                for fc in range(4):
                    for kc in range(NKC):
                        S.op("pe", lambda e, fc=fc, kc=kc, pw=pw, wt=wt: e.matmul(pw[:, fc * CAP:(fc + 1) * CAP], lhsT=wt[:, kc, fc * 128:(fc + 1) * 128],
                                                                                   rhs=XS[:, kc, :], start=(kc == 0), stop=(kc == NKC - 1)),
                             r=(twt, st["tXS"][kc // 4]), w=(tpw,))
            S.op("act", lambda e: e.activation(out=hsg, in_=phg[:, 0:4 * CAP], func=AF.Exp, scale=-1.0), r=(tphg,), w=(thsg,))
            S.op("dve", lambda e: e.tensor_scalar(out=hsg, in0=hsg, scalar1=1.0, scalar2=None, op0=ALU.add), r=(thsg,), w=(thsg,))
            S.op("dve", lambda e: e.reciprocal(out=hsg, in_=hsg), r=(thsg,), w=(thsg,))
            S.op("dve", lambda e: e.tensor_tensor(out=hsg, in0=hsg, in1=phg[:, 0:4 * CAP], op=ALU.mult), r=(thsg, tphg), w=(thsg,))
            S.op("dve", lambda e: e.tensor_tensor(out=HT[:].rearrange("p k c -> p (k c)"), in0=hsg, in1=phu[:, 0:4 * CAP], op=ALU.mult),
                 r=(thsg, tphu), w=(tHT,))
            for nn in range(4):
                py, tpy = self.PSBv[nn], self.tPSB[nn]
                for fc in range(4):
                    S.op("pe", lambda e, fc=fc, nn=nn, py=py: e.matmul(py[0:CAP, :], lhsT=HT[:, fc, :], rhs=wd[:, fc, nn * 512:(nn + 1) * 512],
                                                                        start=(fc == 0), stop=(fc == 3)), r=(tHT, twd), w=(tpy,))
                if nn % 2:
                    S.op("act", lambda e, nn=nn, py=py: e.copy(out=Y[0:CAP, nn * 512:(nn + 1) * 512], in_=py[0:CAP, :]), r=(tpy,), w=(tY[nn],))
                else:
                    S.op("dve", lambda e, nn=nn, py=py: e.tensor_copy(out=Y[0:CAP, nn * 512:(nn + 1) * 512], in_=py[0:CAP, :]), r=(tpy,), w=(tY[nn],))
            ncp = 0
            for i in range(NT):
                for nn in range(4):
                    pcb, tpcb = self.PSBv[ncp % 4], self.tPSB[ncp % 4]
                    ncp += 1
                    S.op("pe", lambda e, i=i, nn=nn, pcb=pcb: e.matmul(pcb[:], lhsT=PgT[0:CAP, i, :], rhs=Y[0:CAP, nn * 512:(nn + 1) * 512], start=True, stop=True),
                         r=(st["tPgT"], tY[nn]), w=(tpcb,))
                    xs = self.X[:, i, nn * 512:(nn + 1) * 512]
                    S.op("dve", lambda e, xs=xs, pcb=pcb: e.tensor_tensor(out=xs, in0=xs, in1=pcb[:], op=ALU.add), r=(tpcb, tXq[i][nn]), w=(tXq[i][nn],))

        self.barrier()
        tXq = [[T(f"Xq{i}_{nn}") for nn in range(4)] for i in range(NT)]
        recF, recB, recL = [], [], []
        for ex in range(NE):
            S.rec = []
            loads(ex)
            recL.append(S.rec)
            assert len(S.rec) == 3
            S.rec = []
            front(ex)
            recF.append(S.rec)
            S.rec = []
            back(ex)
            recB.append(S.rec)
            S.rec = None
        for ent in recL[0] + recF[0]:
            S.emit(ent)
        for ex in range(NE):
            b = recB[ex]
            fnx = recF[ex + 1] if ex + 1 < NE else []
            lnx = recL[ex + 1] if ex + 1 < NE else []
            nb_, nf_ = len(b), len(fnx)
            fi = 0
            for bi, ent in enumerate(b):
                S.emit(ent)
                if lnx and bi == 63:
                    S.emit(lnx[0])
                if lnx and bi == 127:
                    S.emit(lnx[1])
                if lnx and bi == 127 + 5 + 20:
                    S.emit(lnx[2])
                tgt = ((bi + 1) * nf_) // nb_
                while fi < tgt:
                    S.emit(fnx[fi])
                    fi += 1
            while fi < nf_:
                S.emit(fnx[fi])
                fi += 1
        self.barrier()

    def kv_alloc(self):
        self.KT2 = self.arb(4 * 1152).rearrange("p (h t) -> p h t", h=4)
        self.V = self.arb(9 * 256).rearrange("p (b c) -> p b c", b=9)
        self.tKV = T("KV")
        self.att_base = self.ar_off

    def save_halo(self):
        self.S.dma("sp", self.d_halo[:, :], self.X[:, NT - 1, :], r=(self.tX[NT - 1],))

    def kv_compute(self):
        S = self.S
        self.ar_off = self.att_base
        Xh = self.arf(D)
        XTh = self.arb(NKC * 128).rearrange("p (k t) -> p k t", k=NKC)
        tXh, tXTh = T("Xh"), T("XTh")
        S.dma("sp", Xh, self.d_halo[:, :], w=(tXh,))
        for g in range(4):
            ps, tps = self.PSF[g % 2], self.tPSF[g % 2]
            for j in range(4):
                kc = g * 4 + j
                S.op("pe", lambda e, kc=kc, j=j, ps=ps: e.transpose(ps[:, j * 128:(j + 1) * 128], Xh[:, kc * 128:(kc + 1) * 128], self.c_ident[:]),
                     r=(tXh, self.tC), w=(tps,))
            S.op("dve", lambda e, g=g, ps=ps: e.tensor_copy(out=XTh[:, g * 4:(g + 1) * 4, :], in_=ps[:].rearrange("p (j t) -> p j t", j=4)),
                 r=(tps,), w=(tXTh,))
        wkv = self.din("b_w_kv")
        bufv, tbv = self.wload([(lambda b: b[:], wkv.rearrange("(kc p) n -> p kc n", p=128))])
        wkd_v = lambda b: b[:].rearrange("p k (h f) -> p k h f", h=4)
        pieces = []
        for hk in range(4):
            for dup in range(2):
                pieces.append((lambda b, hk=hk, dup=dup: wkd_v(b)[:, :, hk, dup * 64:(dup + 1) * 64],
                               wkv[:, hk * 64:(hk + 1) * 64].rearrange("(kc p) n -> p kc n", p=128)))
        bufk_, tbk = self.wload(pieces)
        bufk = wkd_v(bufk_)
        n = 0
        for b in range(9):
            ps, tps = self.PSF[n % 2], self.tPSF[n % 2]
            n += 1
            for kc in range(NKC):
                lhsT = XTh[:, kc, :] if b == 0 else self.XT[:, kc, (b - 1) * 128:b * 128]
                rt = (tXTh,) if b == 0 else (self.tXT[b - 1],)
                S.op("pe", lambda e, kc=kc, ps=ps, lhsT=lhsT: e.matmul(ps[:, 0:256], lhsT=lhsT, rhs=bufv[:, kc, 256:512], start=(kc == 0), stop=(kc == NKC - 1)),
                     r=rt + (tbv,), w=(tps,))
            S.op("dve", lambda e, b=b, ps=ps: e.tensor_copy(out=self.V[:, b, :], in_=ps[:, 0:256]), r=(tps,), w=(self.tKV,))
        for hk in range(4):
            for part in range(3):
                ps, tps = self.PSF[n % 2], self.tPSF[n % 2]
                n += 1
                if part == 0:
                    rhs_fn, ncol, rt, dst = (lambda kc: XTh[:, kc, :]), 128, (tXTh,), self.KT2[:, hk, 0:128]
                else:
                    lo = (part - 1) * 512
                    rhs_fn, ncol, rt = (lambda kc, lo=lo: self.XT[:, kc, lo:lo + 512]), 512, tuple(self.tXT[(part - 1) * 4:(part - 1) * 4 + 4])
                    dst = self.KT2[:, hk, 128 + lo:128 + lo + 512]
                for kc in range(NKC):
                    S.op("pe", lambda e, kc=kc, ps=ps, hk=hk, rhs_fn=rhs_fn, ncol=ncol: e.matmul(ps[:, 0:ncol], lhsT=bufk[:, kc, hk, :], rhs=rhs_fn(kc),
                                                                                                 start=(kc == 0), stop=(kc == NKC - 1)),
                         r=rt + (tbk,), w=(tps,))
                S.op("act", lambda e, ps=ps, ncol=ncol, dst=dst: e.copy(out=dst, in_=ps[:, 0:ncol]), r=(tps,), w=(self.tKV,))
        self.barrier()

    def attention(self, j_layer):
        S = self.S
        self.barrier()
        self.ar_off = self.att_base
        f = self.arf
        biasm = f(1024).rearrange("p (h k) -> p h k", h=4)
        PB = self.arb(1024).rearrange("p (h k) -> p h k", h=4)
        PTs = self.arb(1024).rearrange("p (a q) -> p a q", a=8)
        QTg = self.arb(2048).rearrange("p (c t) -> p c t", c=2)
        OB = self.arb(256)
        SS2 = f(512).rearrange("p (h k) -> p h k", h=2)
        tSS = T("SS2")
        sm = f(32)
        sink, rm, negm, rs, es, rinv = (sm[:, 4 * k:4 * k + 4] for k in range(6))
        tb, tPB, tPTs, tQ, tOB, tsm = T("biasm"), T("PB"), T("PTs"), T("QTg"), T("OB"), T("asm")
        wq = self.din("b_w_q")
        btab = self.din("bias_tab")
        sinks = self.din("b_sinks")
        PSS3 = self.PSS[:].rearrange("p (h k) -> p h k", h=4)
        tS = (self.tPSB[0], self.tPSB[1])
        n = 0
        for hk in range(4):
            bufq, tbq = self.wload([(lambda b: b[:], wq[j_layer, :, hk * 512:(hk + 1) * 512].rearrange("(kc p) n -> p kc n", p=128))])
            for hh in range(2):
                h0 = 8 * hk + 4 * hh
                c0 = h0 // 2
                for cc in range(2):
                    for half in range(2):
                        ps, tps = self.PSF[n % 2], self.tPSF[n % 2]
                        n += 1
                        for kc in range(NKC):
                            S.op("pe", lambda e, kc=kc, ps=ps, cc=cc, half=half, hh=hh, bufq=bufq: e.matmul(
                                ps[:], lhsT=bufq[:, kc, hh * 256 + cc * 128:hh * 256 + (cc + 1) * 128], rhs=self.XT[:, kc, half * 512:(half + 1) * 512],
                                start=(kc == 0), stop=(kc == NKC - 1)), r=tuple(self.tXT[half * 4:half * 4 + 4]) + (tbq,), w=(tps,))
                        S.op("act", lambda e, ps=ps, cc=cc, half=half: e.copy(out=QTg[:, cc, half * 512:(half + 1) * 512], in_=ps[:]), r=(tps,), w=(tQ,))
                pos = lambda j: (j % 2) * 2 + j // 2
                for j in range(4):
                    S.dma("sp", biasm[:, pos(j), :], btab[h0 + j], w=(tb,))
                    S.dma("sp", sink[:, pos(j):pos(j) + 1], sinks[j_layer:j_layer + 1, h0 + j:h0 + j + 1].partition_broadcast(128), w=(tsm,))
                for j in range(4):
                    S.op("dve", lambda e, j=j: e.tensor_tensor(out=biasm[:, j, :], in0=biasm[:, j, :], in1=self.c_maskc[:], op=ALU.add), r=(tb, self.tC), w=(tb,))
                for nb in range(NT):
                    if os.environ.get("ATT_STOP") == "q":
                        continue
                    qs = slice(nb * 128, (nb + 1) * 128)
                    for j in range(4):
                        p, cc = j % 2, j // 2
                        S.op("pe", lambda e, j=j, p=p, cc=cc, qs=qs, nb=nb, hk=hk: e.matmul(
                            self.PSS[:, pos(j) * 256:(pos(j) + 1) * 256], lhsT=QTg[p * 64:(p + 1) * 64, cc, qs],
                            rhs=self.KT2[p * 64:(p + 1) * 64, hk, nb * 128:nb * 128 + 256], start=True, stop=True),
                            r=(tQ, self.tKV), w=(self.tPSB[j % 2],))
                    for bk in range(2):
                        j0 = 2 * bk
                        pbank = self.PSS[:, bk * 512:(bk + 1) * 512].rearrange("p (h k) -> p h k", h=2)
                        S.op("dve", lambda e, pbank=pbank, j0=j0: e.scalar_tensor_tensor(out=SS2, in0=pbank, scalar=0.125, in1=biasm[:, j0:j0 + 2, :],
                                                                                         op0=ALU.mult, op1=ALU.add), r=(tb, self.tPSB[bk]), w=(tSS,))
                        if nb == 0:
                            for jj in range(2):
                                S.op("dve", lambda e, jj=jj: e.tensor_tensor(out=SS2[:, jj, 0:128], in0=SS2[:, jj, 0:128], in1=self.c_halo[:], op=ALU.add),
                                     r=(self.tC, tSS), w=(tSS,))
                        S.op("dve", lambda e, j0=j0: e.tensor_reduce(out=rm[:, j0:j0 + 2], in_=SS2, axis=AX.X, op=ALU.max), r=(tSS,), w=(tsm,))
                        S.op("dve", lambda e, j0=j0: e.tensor_tensor(out=rm[:, j0:j0 + 2], in0=rm[:, j0:j0 + 2], in1=sink[:, j0:j0 + 2], op=ALU.max), r=(tsm,), w=(tsm,))
                        S.op("dve", lambda e, j0=j0: e.tensor_scalar(out=negm[:, j0:j0 + 2], in0=rm[:, j0:j0 + 2], scalar1=-1.0, scalar2=None, op0=ALU.mult), r=(tsm,), w=(tsm,))
                        for jj in range(2):
                            j = j0 + jj
                            S.op("act", lambda e, j=j, jj=jj: e.activation(out=PB[:, j, :], in_=SS2[:, jj, :], func=AF.Exp,
                                                                            bias=negm[:, j:j + 1], scale=1.0, accum_out=rs[:, j:j + 1]),
                                 r=(tSS, tsm), w=(tPB, tsm))
                    S.op("dve", lambda e: e.tensor_tensor(out=es, in0=sink, in1=negm, op=ALU.add), r=(tsm,), w=(tsm,))
                    S.op("act", lambda e: e.activation(out=es, in_=es, func=AF.Exp), r=(tsm,), w=(tsm,))
                    S.op("dve", lambda e: e.tensor_tensor(out=rinv, in0=rs, in1=es, op=ALU.add), r=(tsm,), w=(tsm,))
                    S.op("dve", lambda e: e.reciprocal(out=rinv, in_=rinv), r=(tsm,), w=(tsm,))
                    pt, tpt = self.PST[0], self.tPST[0]
                    for j in range(4):
                        for kb in range(2):
                            a = j * 2 + kb
                            S.op("pe", lambda e, j=j, kb=kb, a=a: e.transpose(pt[:, a * 128:(a + 1) * 128], PB[:, j, kb * 128:(kb + 1) * 128], self.c_identb[:]),
                                 r=(tPB, self.tC), w=(tpt,))
                    S.op("act", lambda e: e.copy(out=PTs, in_=pt[:].rearrange("p (a q) -> p a q", a=8)), r=(tpt,), w=(tPTs,))
                    po, tpo = self.PSBv[2], self.tPSB[2]
                    for j in range(4):
                        for kb in range(2):
                            S.op("pe", lambda e, j=j, kb=kb, nb=nb, hk=hk: e.matmul(po[:, j * 64:(j + 1) * 64], lhsT=PTs[:, pos(j) * 2 + kb, :],
                                                                                    rhs=self.V[:, nb + kb, hk * 64:(hk + 1) * 64],
                                                                                    start=(kb == 0), stop=(kb == 1)),
                                 r=(tPTs, self.tKV), w=(tpo,))
                    for j in range(4):
                        S.op("dve", lambda e, j=j: e.tensor_scalar(out=OB[:, j * 64:(j + 1) * 64], in0=po[:, j * 64:(j + 1) * 64], scalar1=rinv[:, pos(j):pos(j) + 1],
                                                                   scalar2=None, op0=ALU.mult), r=(tpo, tsm), w=(tOB,))
                    pt1, tpt1 = self.PST[1], self.tPST[1]
                    for cc in range(2):
                        S.op("pe", lambda e, cc=cc: e.transpose(pt1[:, cc * 128:(cc + 1) * 128], OB[:, cc * 128:(cc + 1) * 128], self.c_identb[:]),
                             r=(tOB, self.tC), w=(tpt1,))
                    S.op("dve", lambda e, c0=c0, qs=qs: e.tensor_copy(out=self.OT[:, c0:c0 + 2, qs], in_=pt1[:, 0:256].rearrange("p (c q) -> p c q", c=2)),
                         r=(tpt1,), w=(self.tOT[nb],))
        self.barrier()

    def _program(self):
        S = self.S
        self.c_eps = {}
        for eps in (LN_EPS, RMS_EPS):
            t = self.sb(f"eps{len(self.c_eps)}", [128, 1])
            self.c_eps[eps] = t
            S.op("dve", lambda e, t=t, eps=eps: e.memset(t[:], eps), w=(self.tC,))
        self.load_consts()
        stop = self.debug_stop
        if stop is not None and stop != "hg1":
            self.load_x(self.din("xm"))
        if stop in ("xt", "xt0"):
            self.make_xt()
        if stop == "xt":
            self.layernorm(0)
            self.store_out()
            return
        if stop == "hg1":
            self.load_x(self.din("xp"))
            self.make_xt()
            self.hgrn(1, "zero")
            self.barrier()
            self.load_x(self.din("xm"))
            self.make_xt()
            self.hgrn(1, "load")
            self.barrier()
            self.out_proj(self.din("a_w_out")[1])
            self.layernorm(2)
            self.store_out()
            return
        if stop == "att":
            self.phase()
            self.kv_alloc()
            self.load_x(self.din("xp"))
            self.save_halo()
            self.barrier()
            self.load_x(self.din("xm"))
            self.make_xt()
            self.kv_compute()
            if os.environ.get("ATT_STOP") == "kv":
                self.store_out()
                return
            self.attention(0)
            if os.environ.get("ATT_STOP"):
                self.store_out()
                return
            self.out_proj(self.din("b_w_out")[0])
            self.layernorm(4)
            self.store_out()
            return
        if stop == "moe":
            self.moe(0)
            self.layernorm(1)
            self.store_out()
            return
        if stop == "xt0":
            self.barrier()
            self.store_out()
            return
        def hgrn_layer(l, mode):
            self.make_xt()
            self.hgrn(l, mode)
            self.barrier()
            self.out_proj(self.din("a_w_out")[l])
            self.layernorm(2 * l)
            self.moe(l)
            self.layernorm(2 * l + 1)

        self.load_x(self.din("xp"))
        hgrn_layer(0, "zero")
        hgrn_layer(1, "zero")
        self.save_halo()
        self.barrier()
        self.load_x(self.din("xm"))
        hgrn_layer(0, "load")
        hgrn_layer(1, "load")
        self.make_xt()
        self.phase()
        self.kv_alloc()
        self.kv_compute()
        for j in range(2):
            if j:
                self.make_xt()
            self.attention(j)
            self.out_proj(self.din("b_w_out")[j])
            self.layernorm(4 + 2 * j)
            self.moe(2 + j)
            self.layernorm(5 + 2 * j)
        self.store_out()

    def store_out(self):
        S = self.S
        for i in range(NT):
            S.dma("sp", self.y[i * 128:(i + 1) * 128, :], self.X[:, i, :], r=(self.tX[i],))
        S.eng["sp"].wait_ge(S.sem["sp_d"], S.cnt["sp_d"])


_CACHE = {}


def _prep_inputs(inputs):
    c = host_consts()
    bucket = c.pop("_bucket")
    rel_bias = np.asarray(inputs["rel_bias"], np.float32)
    bias_tab = np.ascontiguousarray(rel_bias[bucket].transpose(2, 0, 1))
    shared = {k: np.ascontiguousarray(np.asarray(inputs[k], np.float32)) for k in
              ["a_w_in", "a_lower_bound", "a_norm_g", "a_w_out", "b_w_kv", "b_w_q", "b_sinks", "b_w_out",
               "moe_w_rg", "moe_b_rg", "moe_w_re", "moe_b_re", "moe_w_gate", "moe_w_up", "moe_w_down", "ln_g", "ln_b"]}
    shared["bias_tab"] = bias_tab
    shared.update(c)
    x = np.asarray(inputs["x"], np.float32)
    in_maps = []
    for core in range(8):
        b, half = core // 2, core % 2
        m = dict(shared)
        m["xp"] = np.ascontiguousarray(x[b, 0:SEG])
        m["xm"] = np.ascontiguousarray(x[b, half * SEG:(half + 1) * SEG])
        m["flag"] = np.full((128, 1), float(half), np.float32)
        m["halo_mask"] = np.full((128, 128), 0.0 if half else NEG, np.float32)
        in_maps.append(m)
    return in_maps


def kernel(**inputs):
    if "nc" not in _CACHE:
        p = Prog()
        _CACHE["nc"] = p.build()
        _CACHE["used"] = set(p.in_aps.keys())
    nc = _CACHE["nc"]
    in_maps = [{k: v for k, v in m.items() if k in _CACHE["used"]} for m in _prep_inputs(inputs)]
    res = run_bass_kernel_spmd(nc, in_maps, core_ids=list(range(8)))
    out = np.zeros((4, 2 * SEG, D), np.float32)
    for core in range(8):
        b, half = core // 2, core % 2
        out[b, half * SEG:(half + 1) * SEG] = res.results[core]["y"]
    return out
```
